# Optimizing a Trainium2 kernel written in Bass

```python
import math
import jax, jax.numpy as jnp
from jax import lax
import numpy as np

D_MODEL = 2048
BATCH = 2
SEQ = 4096
DEPTH = 1
DEC_BATCH = 4
DEC_SEQ = 8192
PAST_LEN = 128

MIX_WIDTH = D_MODEL
ATTN_WIDTH = MIX_WIDTH // 2
LRU_WIDTH = MIX_WIDTH - ATTN_WIDTH
HEAD_DIM = 128
N_HEADS = ATTN_WIDTH // HEAD_DIM
N_KV_HEADS = 2
GQA_GROUP = N_HEADS // N_KV_HEADS
WINDOW = 128
BLOCK = 128
N_BUCKETS = 32
MAX_DISTANCE = 128
N_META = 16
LRU_BLOCKS = 8
LRU_BLOCK_W = LRU_WIDTH // LRU_BLOCKS
LRU_C = 8.0
CONV_WIDTH = 4
CONV_LEFT = 2
N_EXPERTS = 32
TOP_K = 4
D_FF = D_MODEL
SWIGLU_LIMIT = 7.0
SWIGLU_ALPHA = 1.702
EXPERT_BLOCK = 256
DN_ALPHA = (2.0 * DEPTH) ** 0.25
DN_BETA = (8.0 * DEPTH) ** -0.25
LN_EPS = 1e-5
NEG_INF = -1e30
Q_COLS = N_HEADS * HEAD_DIM
KV_COLS = N_KV_HEADS * HEAD_DIM
IN_COLS = Q_COLS + 2 * KV_COLS + 2 * LRU_WIDTH

kernel_name = 'hymba_swa_rglru_moe_encoder'


def layer_norm(x, g, b):
    xf = x.astype(jnp.float32)
    mu = jnp.mean(xf, axis=-1, keepdims=True)
    var = jnp.mean(jnp.square(xf - mu), axis=-1, keepdims=True)
    out = (xf - mu) * lax.rsqrt(var + LN_EPS) * g.astype(jnp.float32) + b.astype(jnp.float32)
    return out.astype(x.dtype)


def t5_bucket(rel):
    half = N_BUCKETS // 2
    exact = half // 2
    n = jnp.abs(rel)
    large = exact + (jnp.log(jnp.maximum(n, 1).astype(jnp.float32) / exact)
                     / math.log(MAX_DISTANCE / exact) * (half - exact)).astype(jnp.int32)
    large = jnp.minimum(large, half - 1)
    return jnp.where(rel > 0, half, 0) + jnp.where(n < exact, n, large)


def windowed_attention(q, k, v, rel_bias, sink):
    B, L = q.shape[0], q.shape[1]
    front = BLOCK - N_META
    nblk = -(-(front + L) // BLOCK)
    back = nblk * BLOCK - front - L
    qb = jnp.pad(q, ((0, 0), (front, back), (0, 0), (0, 0))).reshape(
        B, nblk, BLOCK, N_KV_HEADS, GQA_GROUP, HEAD_DIM)

    def band(t):
        tp = jnp.pad(t, ((0, 0), (front + BLOCK, back + BLOCK), (0, 0), (0, 0))).reshape(
            B, nblk + 2, BLOCK, N_KV_HEADS, HEAD_DIM)
        return jnp.concatenate([tp[:, :-2], tp[:, 1:-1], tp[:, 2:]], axis=2)

    kb, vb = band(k), band(v)
    km, vm = k[:, :N_META], v[:, :N_META]

    qi = jnp.arange(BLOCK)
    kj = jnp.arange(3 * BLOCK)
    rel_band = kj[None, :] - BLOCK - qi[:, None]
    kpos = (jnp.arange(nblk)[:, None] - 1) * BLOCK + kj[None, :]
    real = (kpos >= BLOCK) & (kpos < BLOCK + (L - N_META))
    band_mask = (jnp.abs(rel_band) <= WINDOW)[None] & real[:, None, :]
    band_bias = rel_bias[t5_bucket(rel_band)].transpose(2, 0, 1).reshape(
        N_KV_HEADS, GQA_GROUP, BLOCK, 3 * BLOCK)
    qpos = jnp.arange(nblk * BLOCK).reshape(nblk, BLOCK)
    rel_meta = (front + jnp.arange(N_META))[None, None, :] - qpos[:, :, None]
    meta_bias = rel_bias[t5_bucket(rel_meta)].transpose(0, 3, 1, 2).reshape(
        nblk, N_KV_HEADS, GQA_GROUP, BLOCK, N_META)

    scale = HEAD_DIM ** -0.5
    s_band = jnp.einsum('bnqhgd,bnkhd->bnhgqk', qb, kb).astype(jnp.float32) * scale \
        + band_bias.astype(jnp.float32)
    s_band = jnp.where(band_mask[None, :, None, None], s_band, NEG_INF)
    s_meta = jnp.einsum('bnqhgd,bmhd->bnhgqm', qb, km).astype(jnp.float32) * scale \
        + meta_bias.astype(jnp.float32)
    s_sink = jnp.broadcast_to(sink.astype(jnp.float32).reshape(N_KV_HEADS, GQA_GROUP, 1, 1),
                              s_meta.shape[:-1] + (1,))
    p = jax.nn.softmax(jnp.concatenate([s_band, s_meta, s_sink], axis=-1), axis=-1).astype(v.dtype)
    o = jnp.einsum('bnhgqk,bnkhd->bnqhgd', p[..., :3 * BLOCK], vb) \
        + jnp.einsum('bnhgqm,bmhd->bnqhgd', p[..., 3 * BLOCK:3 * BLOCK + N_META], vm)
    return o.reshape(B, nblk * BLOCK, N_HEADS * HEAD_DIM)[:, front:front + L]


def linear_scan(a, u, reverse):
    def combine(left, right):
        a1, b1 = left
        a2, b2 = right
        return a1 * a2, a2 * b1 + b2
    return lax.associative_scan(combine, (a, u), reverse=reverse, axis=1)[1]


def rg_lru_branch(xr, yg, conv_w, conv_b, wa, ba, wi, bi, lam):
    B, L = xr.shape[0], xr.shape[1]
    xp = jnp.pad(xr, ((0, 0), (CONV_LEFT, CONV_WIDTH - 1 - CONV_LEFT), (0, 0)))
    xc = conv_b + conv_w[0] * xp[:, 0:L]
    for t in range(1, CONV_WIDTH):
        xc = xc + conv_w[t] * xp[:, t:t + L]
    xblk = xc.reshape(B, L, LRU_BLOCKS, LRU_BLOCK_W)
    gate_a = jnp.einsum('blnc,zncj->zblnj', xblk, wa).reshape(2, B, L, LRU_WIDTH) + ba[:, None, None, :]
    gate_i = jnp.einsum('blnc,zncj->zblnj', xblk, wi).reshape(2, B, L, LRU_WIDTH) + bi[:, None, None, :]
    r = jax.nn.sigmoid(gate_a.astype(jnp.float32))
    i = jax.nn.sigmoid(gate_i.astype(jnp.float32))
    log_a = -LRU_C * r * jax.nn.softplus(-lam.astype(jnp.float32))[:, None, None, :]
    a = jnp.exp(log_a)
    u = jnp.sqrt(-jnp.expm1(2.0 * log_a)) * (i * xc.astype(jnp.float32)[None])
    h = linear_scan(a[0], u[0], False) + linear_scan(a[1], u[1], True)
    return (h * jax.nn.gelu(yg.astype(jnp.float32))).astype(xr.dtype)


def moe_ffn(h, router_w, router_b, w1, b1, w2, b2):
    T = h.shape[0]
    logits = (h @ router_w).astype(jnp.float32) + router_b.astype(jnp.float32)
    top_v, top_e = lax.top_k(logits, TOP_K)
    gates = jax.nn.softmax(top_v, axis=-1)
    flat_e = top_e.reshape(-1).astype(jnp.int32)
    flat_tok = jnp.repeat(jnp.arange(T, dtype=jnp.int32), TOP_K)
    flat_g = gates.reshape(-1)
    order = jnp.argsort(flat_e)
    se, stok, sg = flat_e[order], flat_tok[order], flat_g[order]
    counts = jnp.bincount(flat_e, length=N_EXPERTS).astype(jnp.int32)
    padded = (counts + EXPERT_BLOCK - 1) // EXPERT_BLOCK * EXPERT_BLOCK
    start = jnp.cumsum(counts) - counts
    pend = jnp.cumsum(padded)
    pstart = pend - padded
    dest = pstart[se] + jnp.arange(T * TOP_K, dtype=jnp.int32) - start[se]
    n_blocks = -(-(T * TOP_K + N_EXPERTS * (EXPERT_BLOCK - 1)) // EXPERT_BLOCK)
    R = n_blocks * EXPERT_BLOCK
    row_tok = jnp.full((R,), T, jnp.int32).at[dest].set(stok)
    row_g = jnp.zeros((R,), jnp.float32).at[dest].set(sg)
    blk_e = jnp.minimum(jnp.searchsorted(pend, jnp.arange(n_blocks, dtype=jnp.int32) * EXPERT_BLOCK,
                                         side='right'), N_EXPERTS - 1)
    h_ext = jnp.concatenate([h, jnp.zeros((1, h.shape[1]), h.dtype)], axis=0)

    def expert_block(args):
        e, tok, g = args
        u = h_ext[tok] @ w1[e] + b1[e]
        glu, lin = jnp.split(u, 2, axis=-1)
        glu = jnp.minimum(glu, SWIGLU_LIMIT)
        lin = jnp.clip(lin, -SWIGLU_LIMIT, SWIGLU_LIMIT)
        act = glu * jax.nn.sigmoid(SWIGLU_ALPHA * glu) * (lin + 1.0)
        return (act @ w2[e] + b2[e]) * g[:, None].astype(h.dtype)

    out = lax.map(expert_block, (blk_e, row_tok.reshape(n_blocks, EXPERT_BLOCK),
                                 row_g.reshape(n_blocks, EXPERT_BLOCK)))
    y = jnp.zeros((T + 1, h.shape[1]), h.dtype).at[row_tok].add(out.reshape(R, h.shape[1]))
    return y[:T]


def encode(x, meta_tokens, ln_in_g, ln_in_b, rel_bias, w_in, conv_w, conv_b, lru_wa, lru_ba,
           lru_wi, lru_bi, lru_lam, attn_sink, w_out, ln1_g, ln1_b, router_w, router_b,
           exp_w1, exp_b1, exp_w2, exp_b2, ln2_g, ln2_b):
    B = x.shape[0]
    h = jnp.concatenate([jnp.broadcast_to(meta_tokens[None], (B, N_META, D_MODEL)).astype(x.dtype), x], axis=1)
    h = layer_norm(h, ln_in_g, ln_in_b)
    L = h.shape[1]
    splits = [Q_COLS, Q_COLS + KV_COLS, Q_COLS + 2 * KV_COLS, Q_COLS + 2 * KV_COLS + LRU_WIDTH]
    for l in range(DEPTH):
        proj = h @ w_in[l]
        q, k, v, xr, yg = jnp.split(proj, splits, axis=-1)
        attn = windowed_attention(q.reshape(B, L, N_HEADS, HEAD_DIM),
                                  k.reshape(B, L, N_KV_HEADS, HEAD_DIM),
                                  v.reshape(B, L, N_KV_HEADS, HEAD_DIM), rel_bias, attn_sink[l])
        rec = rg_lru_branch(xr, yg, conv_w[l], conv_b[l], lru_wa[l], lru_ba[l], lru_wi[l], lru_bi[l], lru_lam[l])
        mix = jnp.concatenate([attn, rec], axis=-1) @ w_out[l]
        h = layer_norm(DN_ALPHA * h + mix, ln1_g[l], ln1_b[l])
        ffn = moe_ffn(h.reshape(B * L, D_MODEL), router_w[l], router_b[l], exp_w1[l], exp_b1[l],
                      exp_w2[l], exp_b2[l]).reshape(B, L, D_MODEL)
        h = layer_norm(DN_ALPHA * h + ffn, ln2_g[l], ln2_b[l])
    return h[:, N_META:]


def setup_inputs(seed: int = 0) -> dict:
    key = jax.random.key(seed)
    ks = jax.random.split(key, 28)
    f32 = jnp.float32
    nrm = lambda k, shape, s: jax.random.normal(k, shape, f32) * s
    a0 = jax.random.uniform(ks[10], (DEPTH, 2, LRU_WIDTH), f32, 0.9, 0.999) ** (1.0 / LRU_C)
    return {
        'x_prompt': nrm(ks[0], (BATCH, SEQ, D_MODEL), 1.0),
        'x_sample': nrm(ks[1], (DEC_BATCH, DEC_SEQ, D_MODEL), 1.0),
        'meta_tokens': nrm(ks[2], (N_META, D_MODEL), 1.0),
        'ln_in_g': 1.0 + nrm(ks[3], (D_MODEL,), 0.02),
        'ln_in_b': nrm(ks[4], (D_MODEL,), 0.02),
        'rel_bias': nrm(ks[5], (N_BUCKETS, N_HEADS), 0.5),
        'w_in': nrm(ks[6], (DEPTH, D_MODEL, IN_COLS), D_MODEL ** -0.5),
        'conv_w': nrm(ks[7], (DEPTH, CONV_WIDTH, LRU_WIDTH), CONV_WIDTH ** -0.5),
        'conv_b': nrm(ks[8], (DEPTH, LRU_WIDTH), 0.02),
        'lru_wa': nrm(ks[9], (DEPTH, 2, LRU_BLOCKS, LRU_BLOCK_W, LRU_BLOCK_W), LRU_BLOCK_W ** -0.5),
        'lru_ba': nrm(ks[11], (DEPTH, 2, LRU_WIDTH), 0.02),
        'lru_wi': nrm(ks[12], (DEPTH, 2, LRU_BLOCKS, LRU_BLOCK_W, LRU_BLOCK_W), LRU_BLOCK_W ** -0.5),
        'lru_bi': nrm(ks[13], (DEPTH, 2, LRU_WIDTH), 0.02),
        'lru_lam': jnp.log(a0) - jnp.log1p(-a0),
        'attn_sink': nrm(ks[14], (DEPTH, N_HEADS), 0.5),
        'w_out': nrm(ks[15], (DEPTH, MIX_WIDTH, D_MODEL), MIX_WIDTH ** -0.5 * DN_BETA),
        'ln1_g': 1.0 + nrm(ks[16], (DEPTH, D_MODEL), 0.02),
        'ln1_b': nrm(ks[17], (DEPTH, D_MODEL), 0.02),
        'router_w': nrm(ks[18], (DEPTH, D_MODEL, N_EXPERTS), D_MODEL ** -0.5),
        'router_b': nrm(ks[19], (DEPTH, N_EXPERTS), 0.01),
        'exp_w1': nrm(ks[20], (DEPTH, N_EXPERTS, D_MODEL, 2 * D_FF), D_MODEL ** -0.5),
        'exp_b1': nrm(ks[21], (DEPTH, N_EXPERTS, 2 * D_FF), 0.02),
        'exp_w2': nrm(ks[22], (DEPTH, N_EXPERTS, D_FF, D_MODEL), D_FF ** -0.5 * DN_BETA),
        'exp_b2': nrm(ks[23], (DEPTH, N_EXPERTS, D_MODEL), 0.02),
        'ln2_g': 1.0 + nrm(ks[24], (DEPTH, D_MODEL), 0.02),
        'ln2_b': nrm(ks[25], (DEPTH, D_MODEL), 0.02),
    }


def reference(x_prompt, x_sample, meta_tokens, ln_in_g, ln_in_b, rel_bias, w_in, conv_w, conv_b,
              lru_wa, lru_ba, lru_wi, lru_bi, lru_lam, attn_sink, w_out, ln1_g, ln1_b,
              router_w, router_b, exp_w1, exp_b1, exp_w2, exp_b2, ln2_g, ln2_b):
    params = (meta_tokens, ln_in_g, ln_in_b, rel_bias, w_in, conv_w, conv_b, lru_wa, lru_ba,
              lru_wi, lru_bi, lru_lam, attn_sink, w_out, ln1_g, ln1_b, router_w, router_b,
              exp_w1, exp_b1, exp_w2, exp_b2, ln2_g, ln2_b)
    y_prompt = encode(x_prompt, *params)
    y_sample = encode(x_sample, *params)
    return (y_prompt, y_sample)
```

```python
import numpy as np
import concourse.bass as bass
import concourse.mybir as mybir
from concourse.bass_utils import run_bass_kernel_spmd
from contextlib import ExitStack

F32 = mybir.dt.float32
BF16 = mybir.dt.bfloat16
I32 = mybir.dt.int32
AF = mybir.ActivationFunctionType
ALU = mybir.AluOpType

D = 2048
NCH = 16
NB = 65
T = NB * 128
NE = 32
CAP = 1280
NRT = CAP // 128
XROWS = NE * CAP
ALPHA = 2.0 ** 0.25
EPS = 1e-5
NEG = -30000.0
BIG = 1.0e6
ENGS = ["pe", "act", "dve", "pool", "sp"]
SEM_ROT = 30000


class Prog:
    def __init__(self, nc, stack):
        self.nc = nc
        self.stack = stack
        self.ops = {e: [] for e in ENGS}
        self.esem = {}
        self.ecnt = {e: 0 for e in ENGS}
        self.waited = {e: {} for e in ENGS}
        self.dsem = {}
        self.lastw = {}
        self.readers = {}
        self.nsem = 0
        self.nbar = 0
        self.bc = None

    def _newsem(self, name):
        self.nsem += 1
        return self.stack.enter_context(self.nc.semaphore(f"{name}_{self.nsem}"))

    def _resolve(self, tok):
        if tok[0] == "e":
            return tok[2], tok[3]
        ds = self.dsem[tok[1]]
        return ds[0], ds[1]

    def _collect(self, e, reads, writes):
        toks = []
        for k in reads:
            if k in self.lastw:
                toks.append(self.lastw[k])
        for k in writes:
            if k in self.lastw:
                toks.append(self.lastw[k])
            toks.extend(self.readers.get(k, ()))
        waits = []
        for t in toks:
            sem, v = self._resolve(t)
            sid = id(sem)
            if self.waited[e].get(sid, 0) >= v:
                continue
            self.waited[e][sid] = v
            waits.append((sem, v))
        return waits

    def _update(self, tok, reads, writes):
        for k in reads:
            self.readers.setdefault(k, []).append(tok)
        for k in writes:
            self.lastw[k] = tok
            self.readers[k] = []

    def op(self, e, fn, reads=(), writes=()):
        waits = self._collect(e, reads, writes)
        if e not in self.esem or self.ecnt[e] >= SEM_ROT:
            self.esem[e] = self._newsem("e" + e)
            self.ecnt[e] = 0
        sem = self.esem[e]
        self.ecnt[e] += 1
        tok = ("e", e, sem, self.ecnt[e])
        self.ops[e].append((waits, fn, (sem, 1)))
        self._update(tok, reads, writes)

    def dma(self, q, fn, reads=(), writes=(), key=None):
        waits = self._collect(q, reads, writes)
        if key not in self.dsem:
            self.dsem[key] = [self._newsem("d"), 0]
        ds = self.dsem[key]
        ds[1] += 16
        self.ops[q].append((waits, fn, (ds[0], 16)))
        self._update(("d", key), reads, writes)

    def barrier(self, marker_fn):
        self.nbar += 1
        n = self.nbar
        waits = []
        cands = [(s, v) for (s, v) in self.dsem.values()]
        for e in ENGS:
            if e != "sp" and e in self.esem:
                cands.append((self.esem[e], self.ecnt[e]))
        for s, v in cands:
            if self.waited["sp"].get(id(s), 0) < v:
                self.waited["sp"][id(s)] = v
                waits.append((s, v))
        self.ops["sp"].append((waits, None, None))
        self.dma("sp", marker_fn, writes=[f"bar{n}"], key="bar")
        for x in ("act", "dve", "pool", "pe"):
            self.ops[x].append((self._collect(x, [f"bar{n}"], []), None, None))

    def flush(self, final=False):
        nc = self.nc
        fin = []
        if final:
            for k, (s, v) in self.dsem.items():
                fin.append((s, v))
            for e in ENGS:
                if e in self.esem and e != "sp":
                    fin.append((self.esem[e], self.ecnt[e]))
        handles = {"pe": "tensor", "act": "scalar", "dve": "vector", "pool": "gpsimd", "sp": "sync"}
        with nc.Block() as block:
            for e in ENGS:
                ops = self.ops[e]
                last = final and (e == "sp")

                def body(eng, ops=ops, last=last, e=e):
                    if e == "pool" and self.bc is None:
                        r = eng.alloc_register("bcreg")
                        eng.reg_mov(r, XROWS - 1)
                        self.bc = eng.snap(r)
                    for waits, fn, inc in ops:
                        for s, v in waits:
                            eng.wait_ge(s, v)
                        if fn is not None:
                            ins = fn(eng)
                            ins.then_inc(inc[0], inc[1])
                    if last:
                        for s, v in fin:
                            eng.wait_ge(s, v)

                getattr(block, handles[e])(body)
        self.ops = {e: [] for e in ENGS}


class Ring:
    def __init__(self, items):
        self.items = items
        self.i = 0

    def next(self):
        it = self.items[self.i % len(self.items)]
        self.i += 1
        return it


def t5_bucket_np(rel):
    n = np.abs(rel)
    large = 8 + (np.log(np.maximum(n, 1).astype(np.float32) / 8) / np.log(128 / 8) * 8).astype(np.int32)
    large = np.minimum(large, 15)
    return np.where(rel > 0, 16, 0) + np.where(n < 8, n, large)


def static_tables():
    k = np.arange(128)[:, None]
    q = np.arange(128)[None, :]
    bk = np.zeros((4, 128, 128), np.float32)
    mk = np.zeros((4, 128, 128), np.float32)
    for vi, j in enumerate((-1, 0, 1)):
        rel = 128 * j + k - q
        bk[vi] = t5_bucket_np(rel)
        mk[vi] = np.where(np.abs(rel) <= 128, 0.0, NEG)
    rel = k - (128 + q)
    bk[3] = t5_bucket_np(rel)
    mk[3] = 0.0
    return bk, mk


def build(stop_after=99, dbg=()):
    nc = bass.Bass("TRN2", target_bir_lowering=False)

    def din(name, shape, dt=F32):
        return nc.dram_tensor(name, list(shape), dt, kind="ExternalInput").ap()

    def dscr(name, shape, dt):
        kind = "ExternalOutput" if name in dbg else "Internal"
        return nc.dram_tensor(name, list(shape), dt, kind=kind).ap()

    xs = din("xs", [T, D])
    valid_tm = din("valid_tm", [128, NB])
    invbig_tm = din("invbig_tm", [128, NB])
    kbias_tm = din("kbias_tm", [128, NB])
    vmask_fm = din("vmask_fm", [128, T])
    bk_tab = din("bk_tab", [4, 128, 128])
    mk_tab = din("mk_tab", [4, 128, 128])
    ut_tab = din("ut_tab", [128, 128])
    ecoff_tab = din("ecoff_tab", [128, NE])
    elim_tab = din("elim_tab", [128, NE])
    ident_tab = din("ident_tab", [128, 128])
    ln_in_g = din("ln_in_g", [D]); ln_in_b = din("ln_in_b", [D])
    rel_bias = din("rel_bias", [32, 8])
    w_in = din("w_in", [D, 3584])
    lru_par = din("lru_par", [128, 8, 11])
    lru_wa = din("lru_wa", [2, 8, 128, 128])
    lru_wi = din("lru_wi", [2, 8, 128, 128])
    attn_sink = din("attn_sink", [8])
    w_out = din("w_out", [D, D])
    ln1_g = din("ln1_g", [D]); ln1_b = din("ln1_b", [D])
    router_w = din("router_w", [D, NE]); router_b = din("router_b", [NE])
    exp_w1 = din("exp_w1", [NE, D, 2 * D]); exp_b1r = din("exp_b1r", [NE, 128, 32])
    exp_w2 = din("exp_w2", [NE, D, D]); exp_b2 = din("exp_b2", [NE, D])
    ln2_g = din("ln2_g", [D]); ln2_b = din("ln2_b", [D])
    y_out = nc.dram_tensor("y_out", [64 * 128, D], F32, kind="ExternalOutput").ap()

    WIN = dscr("WIN", [28, 128, 2048], BF16)
    H0 = dscr("H0", [T, D], F32)
    QT = dscr("QT", [8, 128, T], BF16)
    KT = dscr("KT", [2, 128, T], BF16)
    VV = dscr("VV", [T, 256], BF16)
    XR = dscr("XR", [8, 128, T + 4], F32)
    YG = dscr("YG", [8, 128, T], F32)
    HB = dscr("HB", [8, 128, T], F32)
    MIXT = dscr("MIXT", [16, 128, T], BF16)
    H1F = dscr("H1F", [T, D], F32)
    XE = dscr("XE", [XROWS, D], BF16)
    YE = dscr("YE", [XROWS, D], BF16)
    DBGM = dscr("DBGM", [T, D], F32)
    DBGL = dscr("DBGL", [T, 64], F32)

    with ExitStack() as gst:
        P = Prog(nc, gst)

        def sb(st, name, shape, dt):
            return st.enter_context(nc.sbuf_tensor(name, list(shape), dt))

        def ps(st, name, shape, dt):
            return st.enter_context(nc.psum_tensor(name, list(shape), dt))

        barsb = sb(gst, "barsb", [128, 16], F32)
        ident_b = sb(gst, "ident_b", [128, 128], BF16)
        ident_f = sb(gst, "ident_f", [128, 128], F32)
        valid_sb = sb(gst, "valid_sb", [128, NB], F32)
        invbig_sb = sb(gst, "invbig_sb", [128, NB], F32)
        kbias_sb = sb(gst, "kbias_sb", [128, NB], F32)
        DEST = sb(gst, "DEST", [128, 64, 4], I32)
        GATE = sb(gst, "GATE", [128, 64, 4], F32)
        eps_sb = sb(gst, "eps_sb", [128, 1], F32)

        bar_marker = lambda e: e.dma_start(out=barsb[0:1, 0:16], in_=valid_tm[0:1, 0:16])
        P.op("pool", lambda e: e.memset(eps_sb[:], EPS), writes=["eps"])
        P.dma("sp", lambda e: e.dma_start(out=ident_f[:], in_=ident_tab), writes=["ident_f"], key="g0")
        P.dma("sp", lambda e: e.dma_start(out=valid_sb[:], in_=valid_tm), writes=["valid"], key="g0")
        P.dma("sp", lambda e: e.dma_start(out=invbig_sb[:], in_=invbig_tm), writes=["invbig"], key="g0")
        P.dma("sp", lambda e: e.dma_start(out=kbias_sb[:], in_=kbias_tm), writes=["kbias"], key="g0")
        P.op("dve", lambda e: e.tensor_copy(out=ident_b[:], in_=ident_f[:]), reads=["ident_f"], writes=["ident_b"])

        def layer_norm_tile(st_key, src, dst, dkey, Gt, Bt, gkey, valid_col, scr, tag):
            stats, mv, sd, rs, nmr = scr
            skeys = st_key if isinstance(st_key, list) else [st_key]
            for i in range(4):
                P.op("dve", lambda e, i=i: e.bn_stats(out=stats[:, i, :], in_=src[:, i * 512:(i + 1) * 512]),
                     reads=skeys, writes=[f"{tag}_stats{i}"])
            P.op("dve", lambda e: e.bn_aggr(out=mv[:], in_=stats[:].rearrange("p a b -> p (a b)")),
                 reads=[f"{tag}_stats{i}" for i in range(4)], writes=[f"{tag}_mv"])
            P.op("act", lambda e: e.activation(out=sd[:], in_=mv[:, 1:2], func=AF.Sqrt, bias=eps_sb[:, 0:1], scale=1.0),
                 reads=[f"{tag}_mv", "eps"], writes=[f"{tag}_sd"])
            P.op("dve", lambda e: e.reciprocal(out=rs[:], in_=sd[:]), reads=[f"{tag}_sd"], writes=[f"{tag}_rs"])
            if valid_col is not None:
                P.op("dve", lambda e: e.tensor_tensor(out=rs[:], in0=rs[:], in1=valid_col, op=ALU.mult),
                     reads=[f"{tag}_rs", "valid"], writes=[f"{tag}_rs"])
            P.op("dve", lambda e: e.scalar_tensor_tensor(out=nmr[:], in0=mv[:, 0:1], scalar=-1.0, in1=rs[:], op0=ALU.mult, op1=ALU.mult),
                 reads=[f"{tag}_mv", f"{tag}_rs"], writes=[f"{tag}_nmr"])
            P.op("act", lambda e: e.activation(out=dst, in_=src, func=AF.Identity, scale=rs[:, 0:1], bias=nmr[:, 0:1]),
                 reads=skeys + [f"{tag}_rs", f"{tag}_nmr"], writes=[dkey])
            P.op("dve", lambda e: e.tensor_tensor(out=dst, in0=dst, in1=Gt[:], op=ALU.mult),
                 reads=[dkey, gkey], writes=[dkey])
            if valid_col is not None:
                P.op("dve", lambda e: e.scalar_tensor_tensor(out=dst, in0=Bt[:], scalar=valid_col, in1=dst, op0=ALU.mult, op1=ALU.add),
                     reads=[dkey, gkey, "valid"], writes=[dkey])
            else:
                P.op("dve", lambda e: e.tensor_tensor(out=dst, in0=dst, in1=Bt[:], op=ALU.add),
                     reads=[dkey, gkey], writes=[dkey])

        with ExitStack() as st:
            wst = [sb(st, f"p0_wst{i}", [128, 16, 256], F32) for i in range(2)]
            wbf = [sb(st, f"p0_wbf{i}", [128, 16, 256], BF16) for i in range(2)]
            for j2 in range(14):
                i = j2 % 2
                P.dma("sp", lambda e, i=i, j2=j2: e.dma_start(out=wst[i][:], in_=w_in[:, j2 * 256:(j2 + 1) * 256].rearrange("(c p) n -> p c n", p=128)),
                      writes=[f"p0_wst{i}"], key=f"p0_l{i}")
                eng = "dve" if i == 0 else "act"
                if eng == "dve":
                    P.op("dve", lambda e, i=i: e.tensor_copy(out=wbf[i][:], in_=wst[i][:]), reads=[f"p0_wst{i}"], writes=[f"p0_wbf{i}"])
                else:
                    P.op("act", lambda e, i=i: e.copy(out=wbf[i][:], in_=wst[i][:]), reads=[f"p0_wst{i}"], writes=[f"p0_wbf{i}"])
                for h in range(2):
                    P.dma("sp", lambda e, i=i, j2=j2, h=h: e.dma_start(out=WIN[2 * j2 + h].rearrange("p (c n) -> p c n", c=16), in_=wbf[i][:, :, h * 128:(h + 1) * 128]),
                          reads=[f"p0_wbf{i}"], writes=["WIN"], key=f"p0_s{i}")
            zt = sb(st, "p0_zt", [128, 8, 2], F32)
            P.op("dve", lambda e: e.memset(zt[:], 0.0), writes=["p0_zt"])
            P.dma("sp", lambda e: e.dma_start(out=XR[:, :, 0:2].rearrange("c p n -> p c n"), in_=zt[:]), reads=["p0_zt"], writes=["XRhalo"], key="p0_z")
            P.dma("sp", lambda e: e.dma_start(out=XR[:, :, T + 2:T + 4].rearrange("c p n -> p c n"), in_=zt[:]), reads=["p0_zt"], writes=["XRhalo"], key="p0_z")
            P.barrier(bar_marker)
            P.flush()

        tiles = [(0, 128)] + [(128 + 512 * i, 512) for i in range(16)]
        with ExitStack() as st:
            Gt = sb(st, "p1_G", [128, D], F32)
            Bt = sb(st, "p1_B", [128, D], F32)
            P.dma("sp", lambda e: e.dma_start(out=Gt[:], in_=ln_in_g.partition_broadcast(128)), writes=["p1_GB"], key="p1_gb")
            P.dma("sp", lambda e: e.dma_start(out=Bt[:], in_=ln_in_b.partition_broadcast(128)), writes=["p1_GB"], key="p1_gb")
            xt_r = Ring([(sb(st, f"p1_xt{i}", [128, D], F32), f"p1_xt{i}") for i in range(2)])
            h0_r = Ring([(sb(st, f"p1_h0{i}", [128, D], F32), f"p1_h0{i}") for i in range(2)])
            hb_r = Ring([(sb(st, f"p1_hb{i}", [128, D], BF16), f"p1_hb{i}") for i in range(2)])
            hT_r = Ring([(sb(st, f"p1_hT{i}", [128, 16, 512], BF16), f"p1_hT{i}") for i in range(2)])
            wc_r = Ring([(sb(st, f"p1_wc{i}", [128, 16, 128], BF16), f"p1_wc{i}") for i in range(4)])
            sf_r = Ring([(sb(st, f"p1_sf{i}", [128, 512], F32), f"p1_sf{i}") for i in range(3)])
            sh_r = Ring([(sb(st, f"p1_sh{i}", [128, 512], BF16), f"p1_sh{i}") for i in range(3)])
            sv_r = Ring([(sb(st, f"p1_sv{i}", [128, 256], BF16), f"p1_sv{i}") for i in range(2)])
            lnscr = (sb(st, "p1_stats", [128, 4, 6], F32), sb(st, "p1_mv", [128, 2], F32), sb(st, "p1_sd", [128, 1], F32),
                     sb(st, "p1_rs", [128, 1], F32), sb(st, "p1_nmr", [128, 1], F32))
            tp_r = Ring([(ps(st, f"p1_tp{i}", [128, 1024], BF16), f"p1_tp{i}") for i in range(2)])
            pj_r = Ring([(ps(st, f"p1_pj{i}", [128, 512], F32), f"p1_pj{i}") for i in range(4)])
            QSCALE = 128.0 ** -0.5

            for (s0, w) in tiles:
                nbk = w // 128
                hT, hTk = hT_r.next()
                for bi in range(nbk):
                    b = s0 // 128 + bi
                    xt, xk = xt_r.next()
                    h0t, h0k = h0_r.next()
                    hbt, hbk = hb_r.next()
                    P.dma("sp", lambda e, xt=xt, b=b: e.dma_start(out=xt[:], in_=xs[b * 128:(b + 1) * 128, :]), writes=[xk], key=xk)
                    stats, mv, sd, rs, nmr = lnscr
                    layer_norm_tile(xk, xt[:], h0t[:], h0k, Gt, Bt, "p1_GB", valid_sb[:, b:b + 1], lnscr, "p1ln")
                    P.op("pool", lambda e, hbt=hbt, h0t=h0t: e.tensor_copy(out=hbt[:], in_=h0t[:]), reads=[h0k], writes=[hbk])
                    P.dma("sp", lambda e, h0t=h0t, b=b: e.dma_start(out=H0[b * 128:(b + 1) * 128, :], in_=h0t[:]), reads=[h0k], writes=["H0"], key=h0k + "s")
                    for half in range(2):
                        tp, tpk = tp_r.next()

                        def tr(e, tp=tp, hbt=hbt, half=half):
                            ins = None
                            for c8 in range(8):
                                c = half * 8 + c8
                                ins = e.transpose(out=tp[:, c8 * 128:(c8 + 1) * 128], in_=hbt[:, c * 128:(c + 1) * 128], identity=ident_b[:])
                            return ins
                        P.op("pe", tr, reads=[hbk, "ident_b"], writes=[tpk])
                        if half == 0:
                            P.op("act", lambda e, tp=tp, hT=hT, bi=bi: e.copy(out=hT[:, 0:8, bi * 128:(bi + 1) * 128], in_=tp[:].rearrange("p (c n) -> p c n", c=8)),
                                 reads=[tpk], writes=[hTk + f"_{bi}a"])
                        else:
                            P.op("dve", lambda e, tp=tp, hT=hT, bi=bi: e.tensor_copy(out=hT[:, 8:16, bi * 128:(bi + 1) * 128], in_=tp[:].rearrange("p (c n) -> p c n", c=8)),
                                 reads=[tpk], writes=[hTk + f"_{bi}b"])
                hT_keys = [hTk + f"_{bi}{x}" for bi in range(nbk) for x in "ab"]
                for bi in range(nbk):
                    b = s0 // 128 + bi
                    pj, pjk = pj_r.next()
                    wcs = []
                    for h in range(2):
                        wc, wck = wc_r.next()
                        P.dma("sp", lambda e, wc=wc, h=h: e.dma_start(out=wc[:], in_=WIN[10 + h].rearrange("p (c n) -> p c n", c=16)), reads=["WIN"], writes=[wck], key=wck)
                        wcs.append((wc, wck))

                    def vmm(e, pj=pj, hT=hT, bi=bi, wcs=wcs):
                        ins = None
                        for h in range(2):
                            for c in range(16):
                                ins = e.matmul(pj[:, h * 128:(h + 1) * 128], lhsT=hT[:, c, bi * 128:(bi + 1) * 128], rhs=wcs[h][0][:, c, :], start=(c == 0), stop=(c == 15))
                        return ins
                    P.op("pe", vmm, reads=hT_keys + [wcs[0][1], wcs[1][1]], writes=[pjk])
                    sv, svk = sv_r.next()
                    P.op("act", lambda e, sv=sv, pj=pj: e.copy(out=sv[:], in_=pj[:, 0:256]), reads=[pjk], writes=[svk])
                    P.dma("sp", lambda e, sv=sv, b=b: e.dma_start(out=VV[b * 128:(b + 1) * 128, :], in_=sv[:]), reads=[svk], writes=["VV"], key=svk + "s")
                chunks = list(range(8, 10)) + list(range(12, 20))
                if s0 > 0:
                    chunks = list(range(0, 10)) + list(range(12, 28))
                for j in chunks:
                    wc, wck = wc_r.next()
                    P.dma("sp", lambda e, wc=wc, j=j: e.dma_start(out=wc[:], in_=WIN[j].rearrange("p (c n) -> p c n", c=16)), reads=["WIN"], writes=[wck], key=wck)
                    pj, pjk = pj_r.next()

                    def pmm(e, pj=pj, hT=hT, wc=wc, w=w):
                        ins = None
                        for c in range(16):
                            ins = e.matmul(pj[:, 0:w], lhsT=wc[:, c, :], rhs=hT[:, c, 0:w], start=(c == 0), stop=(c == 15))
                        return ins
                    P.op("pe", pmm, reads=hT_keys + [wck], writes=[pjk])
                    if j < 8:
                        sh, shk = sh_r.next()
                        P.op("act", lambda e, sh=sh, pj=pj, w=w: e.activation(out=sh[:, 0:w], in_=pj[:, 0:w], func=AF.Copy, scale=QSCALE), reads=[pjk], writes=[shk])
                        P.dma("sp", lambda e, sh=sh, j=j, s0=s0, w=w: e.dma_start(out=QT[j, :, s0:s0 + w], in_=sh[:, 0:w]), reads=[shk], writes=["QT"], key=shk + "s")
                    elif j < 10:
                        sh, shk = sh_r.next()
                        P.op("dve", lambda e, sh=sh, pj=pj, w=w: e.tensor_copy(out=sh[:, 0:w], in_=pj[:, 0:w]), reads=[pjk], writes=[shk])
                        P.dma("sp", lambda e, sh=sh, j=j, s0=s0, w=w: e.dma_start(out=KT[j - 8, :, s0:s0 + w], in_=sh[:, 0:w]), reads=[shk], writes=["KT"], key=shk + "s")
                    elif j < 20:
                        sf, sfk = sf_r.next()
                        P.op("dve", lambda e, sf=sf, pj=pj, w=w: e.tensor_copy(out=sf[:, 0:w], in_=pj[:, 0:w]), reads=[pjk], writes=[sfk])
                        P.dma("sp", lambda e, sf=sf, j=j, s0=s0, w=w: e.dma_start(out=XR[j - 12, :, 2 + s0:2 + s0 + w], in_=sf[:, 0:w]), reads=[sfk], writes=["XR"], key=sfk + "s")
                    else:
                        sf, sfk = sf_r.next()
                        P.op("act", lambda e, sf=sf, pj=pj, w=w: e.activation(out=sf[:, 0:w], in_=pj[:, 0:w], func=AF.Gelu), reads=[pjk], writes=[sfk])
                        P.dma("sp", lambda e, sf=sf, j=j, s0=s0, w=w: e.dma_start(out=YG[j - 20, :, s0:s0 + w], in_=sf[:, 0:w]), reads=[sfk], writes=["YG"], key=sfk + "s")
            P.barrier(bar_marker)
            P.flush()
        if stop_after <= 1:
            P.flush(final=True)
            return nc

        for z in (1, 0):
            with ExitStack() as st:
                par = sb(st, f"l{z}_par", [128, 8, 11], F32)
                one_c = sb(st, f"l{z}_one", [128, 1], F32)
                nksp = sb(st, f"l{z}_nksp", [128, 8], F32)
                tmp = [sb(st, f"l{z}_tmp{i}", [128, 8], F32) for i in range(8)]
                wst = sb(st, f"l{z}_wst", [128, 2, 8, 128], F32)
                WA = sb(st, f"l{z}_WA", [128, 8, 128], BF16)
                WI = sb(st, f"l{z}_WI", [128, 8, 128], BF16)
                carry = sb(st, f"l{z}_carry", [128, 8], F32)
                P.dma("sp", lambda e: e.dma_start(out=par[:], in_=lru_par), writes=["l_par"], key="l_par")
                P.op("pool", lambda e: e.memset(one_c[:], 1.0), writes=["l_one"])
                P.op("pool", lambda e: e.memset(carry[:], 0.0), writes=[f"carry{c}" for c in range(8)])
                for (src, dst, nm) in ((lru_wa, WA, "wa"), (lru_wi, WI, "wi")):
                    P.dma("sp", lambda e, src=src: e.dma_start(out=wst[:], in_=src.rearrange("z n c j -> c z n j")), writes=["l_wst"], key="l_wst")
                    P.op("dve", lambda e, dst=dst: e.tensor_copy(out=dst[:], in_=wst[:, z, :, :]), reads=["l_wst"], writes=["l_" + nm])
                lam = par[:, :, 9 + z]
                t_abs, t_e, t_u, t_ln, t_ser, t_msk, t_mx, t_q = [t[:] for t in tmp]
                P.op("dve", lambda e: e.tensor_scalar(out=t_abs, in0=lam, scalar1=-1.0, scalar2=None, op0=ALU.mult), reads=["l_par"], writes=["lt_abs"])
                P.op("dve", lambda e: e.tensor_tensor(out=t_abs, in0=t_abs, in1=lam, op=ALU.max), reads=["l_par", "lt_abs"], writes=["lt_abs"])
                P.op("act", lambda e: e.activation(out=t_e, in_=t_abs, func=AF.Exp, scale=-1.0), reads=["lt_abs"], writes=["lt_e"])
                P.op("dve", lambda e: e.tensor_scalar(out=t_u, in0=t_e, scalar1=1.0, scalar2=None, op0=ALU.add), reads=["lt_e"], writes=["lt_u"])
                P.op("act", lambda e: e.activation(out=t_ln, in_=t_u, func=AF.Ln), reads=["lt_u"], writes=["lt_ln"])
                P.op("dve", lambda e: e.tensor_scalar(out=t_ser, in0=t_e, scalar1=-0.2, scalar2=0.25, op0=ALU.mult, op1=ALU.add), reads=["lt_e"], writes=["lt_ser"])
                for cst in (1.0 / 3.0, 0.5, 1.0):
                    P.op("dve", lambda e: e.tensor_tensor(out=t_ser, in0=t_ser, in1=t_e, op=ALU.mult), reads=["lt_ser", "lt_e"], writes=["lt_ser"])
                    P.op("dve", lambda e, cst=cst: e.tensor_scalar(out=t_ser, in0=t_ser, scalar1=-1.0, scalar2=cst, op0=ALU.mult, op1=ALU.add), reads=["lt_ser"], writes=["lt_ser"])
                P.op("dve", lambda e: e.tensor_tensor(out=t_ser, in0=t_ser, in1=t_e, op=ALU.mult), reads=["lt_ser", "lt_e"], writes=["lt_ser"])
                P.op("dve", lambda e: e.tensor_scalar(out=t_msk, in0=t_e, scalar1=0.1, scalar2=None, op0=ALU.is_lt), reads=["lt_e"], writes=["lt_msk"])
                P.op("dve", lambda e: e.tensor_tensor(out=t_q, in0=t_ser, in1=t_ln, op=ALU.subtract), reads=["lt_ser", "lt_ln"], writes=["lt_q"])
                P.op("dve", lambda e: e.tensor_tensor(out=t_q, in0=t_q, in1=t_msk, op=ALU.mult), reads=["lt_q", "lt_msk"], writes=["lt_q"])
                P.op("dve", lambda e: e.tensor_tensor(out=t_q, in0=t_q, in1=t_ln, op=ALU.add), reads=["lt_q", "lt_ln"], writes=["lt_q"])
                P.op("dve", lambda e: e.tensor_scalar(out=t_mx, in0=lam, scalar1=-1.0, scalar2=0.0, op0=ALU.mult, op1=ALU.max), reads=["l_par"], writes=["lt_mx"])
                P.op("dve", lambda e: e.tensor_tensor(out=t_q, in0=t_q, in1=t_mx, op=ALU.add), reads=["lt_q", "lt_mx"], writes=["lt_q"])
                P.op("dve", lambda e: e.tensor_scalar(out=nksp[:], in0=t_q, scalar1=-8.0, scalar2=None, op0=ALU.mult), reads=["lt_q"], writes=["l_nksp"])

                def ring(name, shape, dt, n):
                    return Ring([(sb(st, f"l{z}_{name}{i}", shape, dt), f"l_{name}{i}") for i in range(n)])
                xr_r = ring("xr", [128, 515], F32, 2)
                vm_r = ring("vm", [128, 512], F32, 2)
                xc_r = ring("xc", [128, 512], F32, 2)
                xcm_r = ring("xcm", [128, 512], F32, 2)
                xcb_r = ring("xcb", [128, 512], BF16, 2)
                r_r = ring("r", [128, 512], F32, 2)
                i_r = ring("i", [128, 512], F32, 2)
                a_r = ring("a", [128, 512], F32, 2)
                s_r = ring("s", [128, 512], F32, 2)
                u_r = ring("u", [128, 512], F32, 2)
                h_r = ring("h", [128, 512], F32, 2)
                hb_r2 = ring("hbt", [128, 512], F32, 2)
                yg_r = ring("ygt", [128, 512], F32, 2)
                rec_r = ring("rec", [128, 512], BF16, 2)
                pg_r = Ring([(ps(st, f"l{z}_pg{i}", [128, 512], F32), f"l_pg{i}") for i in range(4)])
                order = tiles if z == 0 else list(reversed(tiles[1:]))
                for (s0, w) in order:
                    vm, vmk = vm_r.next()
                    P.dma("sp", lambda e, vm=vm, s0=s0, w=w: e.dma_start(out=vm[:, 0:w], in_=vmask_fm[:, s0:s0 + w]), writes=[vmk], key=vmk)
                    for c in range(8):
                        xr_t, xrk = xr_r.next()
                        P.dma("sp", lambda e, xr_t=xr_t, c=c, s0=s0, w=w: e.dma_start(out=xr_t[:, 0:w + 3], in_=XR[c, :, s0:s0 + w + 3]), writes=[xrk], key=xrk)
                        xc, xck = xc_r.next()
                        P.op("pool", lambda e, xc=xc, xr_t=xr_t, c=c, w=w: e.tensor_scalar(out=xc[:, 0:w], in0=xr_t[:, 0:w], scalar1=par[:, c, 0:1], scalar2=par[:, c, 4:5], op0=ALU.mult, op1=ALU.add),
                             reads=[xrk, "l_par"], writes=[xck])
                        for j in (1, 2, 3):
                            P.op("dve", lambda e, xc=xc, xr_t=xr_t, c=c, w=w, j=j: e.scalar_tensor_tensor(out=xc[:, 0:w], in0=xr_t[:, j:j + w], scalar=par[:, c, j:j + 1], in1=xc[:, 0:w], op0=ALU.mult, op1=ALU.add),
                                 reads=[xrk, xck, "l_par"], writes=[xck])
                        xcm, xcmk = xcm_r.next()
                        P.op("pool", lambda e, xcm=xcm, xc=xc, vm=vm, w=w: e.tensor_tensor(out=xcm[:, 0:w], in0=xc[:, 0:w], in1=vm[:, 0:w], op=ALU.mult), reads=[xck, vmk], writes=[xcmk])
                        xcb, xcbk = xcb_r.next()
                        P.op("act", lambda e, xcb=xcb, xc=xc, w=w: e.copy(out=xcb[:, 0:w], in_=xc[:, 0:w]), reads=[xck], writes=[xcbk])
                        pga, pgak = pg_r.next()
                        pgi, pgik = pg_r.next()
                        P.op("pe", lambda e, pga=pga, xcb=xcb, c=c, w=w: e.matmul(pga[:, 0:w], lhsT=WA[:, c, :], rhs=xcb[:, 0:w], start=True, stop=True), reads=[xcbk, "l_wa"], writes=[pgak])
                        P.op("pe", lambda e, pgi=pgi, xcb=xcb, c=c, w=w: e.matmul(pgi[:, 0:w], lhsT=WI[:, c, :], rhs=xcb[:, 0:w], start=True, stop=True), reads=[xcbk, "l_wi"], writes=[pgik])
                        rt_, rk = r_r.next()
                        it_, ik = i_r.next()
                        P.op("act", lambda e, rt_=rt_, pga=pga, c=c, w=w: e.activation(out=rt_[:, 0:w], in_=pga[:, 0:w], func=AF.Sigmoid, bias=par[:, c, 5 + z:6 + z], scale=1.0), reads=[pgak, "l_par"], writes=[rk])
                        P.op("act", lambda e, it_=it_, pgi=pgi, c=c, w=w: e.activation(out=it_[:, 0:w], in_=pgi[:, 0:w], func=AF.Sigmoid, bias=par[:, c, 7 + z:8 + z], scale=1.0), reads=[pgik, "l_par"], writes=[ik])
                        at_, ak = a_r.next()
                        P.op("act", lambda e, at_=at_, rt_=rt_, c=c, w=w: e.activation(out=at_[:, 0:w], in_=rt_[:, 0:w], func=AF.Exp, scale=nksp[:, c:c + 1]), reads=[rk, "l_nksp"], writes=[ak])
                        st_, sk = s_r.next()
                        P.op("pool", lambda e, st_=st_, at_=at_, w=w: e.tensor_tensor(out=st_[:, 0:w], in0=at_[:, 0:w], in1=at_[:, 0:w], op=ALU.mult), reads=[ak], writes=[sk])
                        P.op("act", lambda e, st_=st_, w=w: e.activation(out=st_[:, 0:w], in_=st_[:, 0:w], func=AF.Sqrt, scale=-1.0, bias=one_c[:, 0:1]), reads=[sk, "l_one"], writes=[sk])
                        ut_, uk = u_r.next()
                        P.op("dve", lambda e, ut_=ut_, it_=it_, xcm=xcm, w=w: e.tensor_tensor(out=ut_[:, 0:w], in0=it_[:, 0:w], in1=xcm[:, 0:w], op=ALU.mult), reads=[ik, xcmk], writes=[uk])
                        P.op("dve", lambda e, ut_=ut_, st_=st_, w=w: e.tensor_tensor(out=ut_[:, 0:w], in0=ut_[:, 0:w], in1=st_[:, 0:w], op=ALU.mult), reads=[uk, sk], writes=[uk])
                        ht_, hk = h_r.next()
                        if z == 0:
                            P.op("dve", lambda e, ht_=ht_, at_=at_, ut_=ut_, c=c, w=w: e.tensor_tensor_scan(out=ht_[:, 0:w], data0=at_[:, 0:w], data1=ut_[:, 0:w], initial=carry[:, c:c + 1], op0=ALU.mult, op1=ALU.add),
                                 reads=[ak, uk, f"carry{c}"], writes=[hk])
                            P.op("act", lambda e, ht_=ht_, c=c, w=w: e.copy(out=carry[:, c:c + 1], in_=ht_[:, w - 1:w]), reads=[hk], writes=[f"carry{c}"])
                        else:
                            P.op("dve", lambda e, ht_=ht_, at_=at_, ut_=ut_, c=c, w=w: e.tensor_tensor_scan(out=ht_[:, 0:w][:, ::-1], data0=at_[:, 0:w][:, ::-1], data1=ut_[:, 0:w][:, ::-1], initial=carry[:, c:c + 1], op0=ALU.mult, op1=ALU.add),
                                 reads=[ak, uk, f"carry{c}"], writes=[hk])
                            P.op("act", lambda e, ht_=ht_, c=c: e.copy(out=carry[:, c:c + 1], in_=ht_[:, 0:1]), reads=[hk], writes=[f"carry{c}"])
                        if z == 1:
                            P.dma("sp", lambda e, ht_=ht_, c=c, s0=s0, w=w: e.dma_start(out=HB[c, :, s0:s0 + w], in_=ht_[:, 0:w]), reads=[hk], writes=["HB"], key=hk + "s")
                        elif s0 > 0:
                            hbt, hbk2 = hb_r2.next()
                            ygt, ygk = yg_r.next()
                            P.dma("sp", lambda e, hbt=hbt, c=c, s0=s0, w=w: e.dma_start(out=hbt[:, 0:w], in_=HB[c, :, s0:s0 + w]), writes=[hbk2], key=hbk2)
                            P.dma("sp", lambda e, ygt=ygt, c=c, s0=s0, w=w: e.dma_start(out=ygt[:, 0:w], in_=YG[c, :, s0:s0 + w]), writes=[ygk], key=ygk)
                            P.op("pool", lambda e, hbt=hbt, ht_=ht_, w=w: e.tensor_tensor(out=hbt[:, 0:w], in0=hbt[:, 0:w], in1=ht_[:, 0:w], op=ALU.add), reads=[hbk2, hk], writes=[hbk2])
                            rc, rck = rec_r.next()
                            P.op("dve", lambda e, rc=rc, hbt=hbt, ygt=ygt, w=w: e.tensor_tensor(out=rc[:, 0:w], in0=hbt[:, 0:w], in1=ygt[:, 0:w], op=ALU.mult), reads=[hbk2, ygk], writes=[rck])
                            P.dma("sp", lambda e, rc=rc, c=c, s0=s0, w=w: e.dma_start(out=MIXT[8 + c, :, s0:s0 + w], in_=rc[:, 0:w]), reads=[rck], writes=["MIXT"], key=rck + "s")
                P.barrier(bar_marker)
                P.flush()
        if stop_after <= 2:
            P.flush(final=True)
            return nc

        with ExitStack() as st:
            rb_bc = sb(st, "a_rb", [128, 256], F32)
            sk_bc = sb(st, "a_sk", [128, 8], F32)
            bk_sb = sb(st, "a_bk", [128, 4, 128], F32)
            biasT = sb(st, "a_biasT", [128, 5, 8, 128], F32)
            oh = sb(st, "a_oh", [128, 128], F32)
            sinkt = sb(st, "a_sinkt", [128, 8, 128], F32)
            ones_b = sb(st, "a_ones", [128, 128], BF16)
            kmeta = sb(st, "a_kmeta", [128, 2, 128], BF16)
            vmeta = sb(st, "a_vmeta", [128, 256], BF16)
            P.dma("sp", lambda e: e.dma_start(out=rb_bc[:], in_=rel_bias.rearrange("a b -> (a b)").partition_broadcast(128)), writes=["a_rb"], key="a_c")
            P.dma("sp", lambda e: e.dma_start(out=sk_bc[:], in_=attn_sink.partition_broadcast(128)), writes=["a_sk"], key="a_c")
            P.dma("sp", lambda e: e.dma_start(out=bk_sb[:], in_=bk_tab.rearrange("v p n -> p v n")), writes=["a_bk"], key="a_c")
            for h in range(8):
                P.dma("sp", lambda e, h=h: e.dma_start(out=biasT[:, 0:4, h, :], in_=mk_tab.rearrange("v p n -> p v n")), writes=["a_biasT"], key="a_c")
            P.dma("sp", lambda e: e.dma_start(out=kmeta[:], in_=KT[:, :, 0:128].rearrange("g p n -> p g n")), writes=["a_kmeta"], key="a_c")
            P.dma("sp", lambda e: e.dma_start(out=vmeta[:], in_=VV[0:128, :]), writes=["a_vmeta"], key="a_c")
            P.op("pool", lambda e: e.memset(ones_b[:], 1.0), writes=["a_ones"])
            P.op("act", lambda e: e.activation(out=sk_bc[:], in_=sk_bc[:], func=AF.Exp), reads=["a_sk"], writes=["a_sk"])
            P.op("pool", lambda e: e.memset(sinkt[:], 0.0), writes=["a_sinkt"])
            P.op("pool", lambda e: e.memset(biasT[:, 4, :, :], 0.0), reads=["a_biasT"], writes=["a_biasT"])
            for h in range(8):
                P.op("dve", lambda e, h=h: e.tensor_scalar(out=sinkt[:, h, :], in0=sinkt[:, h, :], scalar1=sk_bc[:, h:h + 1], scalar2=None, op0=ALU.add), reads=["a_sinkt", "a_sk"], writes=["a_sinkt"])
                P.op("dve", lambda e, h=h: e.tensor_scalar(out=biasT[:, 4, h, :], in0=biasT[:, 4, h, :], scalar1=rb_bc[:, 15 * 8 + h:15 * 8 + h + 1], scalar2=None, op0=ALU.add), reads=["a_biasT", "a_rb"], writes=["a_biasT"])
            bk_np, mk_np = static_tables()
            for var in range(4):
                present = sorted(set(int(v) for v in np.unique(bk_np[var][mk_np[var] == 0.0])))
                for bkt in present:
                    P.op("dve", lambda e, var=var, bkt=bkt: e.tensor_scalar(out=oh[:], in0=bk_sb[:, var, :], scalar1=float(bkt), scalar2=None, op0=ALU.is_equal), reads=["a_bk", "a_oh"], writes=["a_oh"])
                    for h in range(8):
                        P.op("dve", lambda e, var=var, bkt=bkt, h=h: e.scalar_tensor_tensor(out=biasT[:, var, h, :], in0=oh[:], scalar=rb_bc[:, bkt * 8 + h:bkt * 8 + h + 1], in1=biasT[:, var, h, :], op0=ALU.mult, op1=ALU.add),
                             reads=["a_oh", "a_rb", "a_biasT"], writes=["a_biasT"])
            q_r = Ring([(sb(st, f"a_q{i}", [128, 8, 512], BF16), f"a_q{i}") for i in range(2)])
            kw_r = Ring([(sb(st, f"a_kw{i}", [128, 2, 768], BF16), f"a_kw{i}") for i in range(2)])
            vw_r = Ring([(sb(st, f"a_vw{i}", [128, 6, 256], BF16), f"a_vw{i}") for i in range(2)])
            ao_r = Ring([(sb(st, f"a_ao{i}", [128, 8, 512], BF16), f"a_ao{i}") for i in range(2)])
            ss_r = Ring([(sb(st, f"a_ss{i}", [128, 512], F32), f"a_ss{i}") for i in range(3)])
            pt_r = Ring([(sb(st, f"a_pt{i}", [128, 512], BF16), f"a_pt{i}") for i in range(8)])
            dn_r = Ring([(sb(st, f"a_dn{i}", [128, 512], F32), f"a_dn{i}") for i in range(2)])
            psS = Ring([(ps(st, f"a_pS{i}", [128, 512], F32), f"a_pS{i}") for i in range(3)])
            psO = Ring([(ps(st, f"a_pO{i}", [128, 512], F32), f"a_pO{i}") for i in range(2)])
            psD = Ring([(ps(st, f"a_pD{i}", [128, 512], F32), f"a_pD{i}") for i in range(2)])
            for (s0, w) in tiles[1:]:
                qt, qk = q_r.next()
                kw, kwk = kw_r.next()
                vw, vwk = vw_r.next()
                ao, aok = ao_r.next()
                hi = min(T, s0 + w + 128)
                ncol = hi - (s0 - 128)
                P.dma("sp", lambda e, qt=qt, s0=s0, w=w: e.dma_start(out=qt[:], in_=QT[:, :, s0:s0 + w].rearrange("c p n -> p c n")), writes=[qk], key=qk)
                P.dma("sp", lambda e, kw=kw, s0=s0, ncol=ncol: e.dma_start(out=kw[:, :, 0:ncol], in_=KT[:, :, s0 - 128:s0 - 128 + ncol].rearrange("g p n -> p g n")), writes=[kwk], key=kwk)
                P.dma("sp", lambda e, vw=vw, s0=s0, ncol=ncol: e.dma_start(out=vw[:, 0:ncol // 128, :], in_=VV[s0 - 128:s0 - 128 + ncol, :].rearrange("(b p) n -> p b n", p=128)), writes=[vwk], key=vwk)
                for bi in range(4):
                    n = s0 // 128 + bi
                    for g in range(2):
                        kbs = [("meta", None, 3 if n == 1 else 4, 0)]
                        for j in (-1, 0, 1):
                            kb = n + j
                            if 1 <= kb <= 64:
                                kbs.append(("band", bi + 1 + j, j + 1, kb))
                        pts = []
                        for (kind, wi_, var, kbcol) in kbs:
                            pS, pSk = psS.next()
                            if kind == "meta":
                                kap = kmeta[:, g, :]
                                kkeys = ["a_kmeta"]
                            else:
                                kap = kw[:, g, wi_ * 128:(wi_ + 1) * 128]
                                kkeys = [kwk]
                            P.op("pe", lambda e, pS=pS, kap=kap, qt=qt, g=g, bi=bi: e.matmul(pS[:], lhsT=kap, rhs=qt[:, 4 * g:4 * g + 4, bi * 128:(bi + 1) * 128], start=True, stop=True),
                                 reads=kkeys + [qk], writes=[pSk])
                            ss, ssk = ss_r.next()
                            P.op("dve", lambda e, ss=ss, pS=pS, var=var, g=g: e.tensor_tensor(out=ss[:].rearrange("p (h n) -> p h n", h=4), in0=pS[:].rearrange("p (h n) -> p h n", h=4), in1=biasT[:, var, 4 * g:4 * g + 4, :], op=ALU.add),
                                 reads=[pSk, "a_biasT"], writes=[ssk])
                            pt, ptk = pt_r.next()
                            P.op("act", lambda e, pt=pt, ss=ss, kbcol=kbcol: e.activation(out=pt[:], in_=ss[:], func=AF.Exp, bias=kbias_sb[:, kbcol:kbcol + 1], scale=1.0), reads=[ssk, "kbias"], writes=[ptk])
                            if kind == "meta":
                                vap = vmeta[:, g * 128:(g + 1) * 128]
                                vkeys = ["a_vmeta"]
                            else:
                                vap = vw[:, wi_, g * 128:(g + 1) * 128]
                                vkeys = [vwk]
                            pts.append((pt, ptk, vap, vkeys))
                        pO, pOk = psO.next()
                        pD, pDk = psD.next()

                        def omm(e, pO=pO, pts=pts):
                            ins = None
                            for ii, (pt, ptk, vap, vkeys) in enumerate(pts):
                                ins = e.matmul(pO[:], lhsT=vap, rhs=pt[:], start=(ii == 0), stop=(ii == len(pts) - 1))
                            return ins

                        def dmm(e, pD=pD, pts=pts):
                            ins = None
                            for ii, (pt, ptk, vap, vkeys) in enumerate(pts):
                                ins = e.matmul(pD[:], lhsT=ones_b[:], rhs=pt[:], start=(ii == 0), stop=(ii == len(pts) - 1))
                            return ins
                        allk = [x[1] for x in pts] + [k for x in pts for k in x[3]]
                        P.op("pe", omm, reads=allk, writes=[pOk])
                        P.op("pe", dmm, reads=allk + ["a_ones"], writes=[pDk])
                        dn, dnk = dn_r.next()
                        P.op("dve", lambda e, dn=dn, pD=pD, g=g: e.tensor_tensor(out=dn[:].rearrange("p (h n) -> p h n", h=4), in0=pD[:].rearrange("p (h n) -> p h n", h=4), in1=sinkt[:, 4 * g:4 * g + 4, :], op=ALU.add),
                             reads=[pDk, "a_sinkt"], writes=[dnk])
                        P.op("dve", lambda e, dn=dn: e.reciprocal(out=dn[:], in_=dn[:]), reads=[dnk], writes=[dnk])
                        P.op("dve", lambda e, ao=ao, pO=pO, dn=dn, g=g, bi=bi: e.tensor_tensor(out=ao[:, 4 * g:4 * g + 4, bi * 128:(bi + 1) * 128], in0=pO[:].rearrange("p (h n) -> p h n", h=4), in1=dn[:].rearrange("p (h n) -> p h n", h=4), op=ALU.mult),
                             reads=[pOk, dnk], writes=[aok + f"_{bi}{g}"])
                P.dma("sp", lambda e, ao=ao, s0=s0, w=w: e.dma_start(out=MIXT[0:8, :, s0:s0 + w].rearrange("c p n -> p c n"), in_=ao[:]),
                      reads=[aok + f"_{bi}{g}" for bi in range(4) for g in range(2)], writes=["MIXT"], key=aok + "s")
            P.barrier(bar_marker)
            P.flush()
        if stop_after <= 3:
            P.flush(final=True)
            return nc

        with ExitStack() as st:
            wo = sb(st, "d_wo", [128, 16, 2048], BF16)
            wst_r = Ring([(sb(st, f"d_wst{i}", [128, 16, 128], F32), f"d_wst{i}") for i in range(1)])
            for j in range(16):
                wstt, wk = wst_r.next()
                P.dma("sp", lambda e, wstt=wstt, j=j: e.dma_start(out=wstt[:], in_=w_out[:, j * 128:(j + 1) * 128].rearrange("(c p) n -> p c n", p=128)), writes=[wk], key=wk)
                if j % 2 == 0:
                    P.op("dve", lambda e, wstt=wstt, j=j: e.tensor_copy(out=wo[:, :, j * 128:(j + 1) * 128], in_=wstt[:]), reads=[wk], writes=[f"d_wo{j}"])
                else:
                    P.op("act", lambda e, wstt=wstt, j=j: e.copy(out=wo[:, :, j * 128:(j + 1) * 128], in_=wstt[:]), reads=[wk], writes=[f"d_wo{j}"])
            wo_keys = [f"d_wo{j}" for j in range(16)]
            G1 = sb(st, "d_G1", [128, D], F32)
            B1 = sb(st, "d_B1", [128, D], F32)
            P.dma("sp", lambda e: e.dma_start(out=G1[:], in_=ln1_g.partition_broadcast(128)), writes=["d_GB"], key="d_c")
            P.dma("sp", lambda e: e.dma_start(out=B1[:], in_=ln1_b.partition_broadcast(128)), writes=["d_GB"], key="d_c")
            rw = sb(st, "d_rw", [128, 16, NE], F32)
            rbb = sb(st, "d_rbb", [128, NE], F32)
            UT = sb(st, "d_UT", [128, 128], F32)
            ones_f = sb(st, "d_ones", [128, 128], F32)
            base = sb(st, "d_base", [128, NE], F32)
            elim = sb(st, "d_elim", [128, NE], F32)
            P.dma("sp", lambda e: e.dma_start(out=rw[:], in_=router_w.rearrange("(c p) n -> p c n", p=128)), writes=["d_rw"], key="d_c")
            P.dma("sp", lambda e: e.dma_start(out=rbb[:], in_=router_b.partition_broadcast(128)), writes=["d_rbb"], key="d_c")
            P.dma("sp", lambda e: e.dma_start(out=UT[:], in_=ut_tab), writes=["d_UT"], key="d_c")
            P.dma("sp", lambda e: e.dma_start(out=base[:], in_=ecoff_tab), writes=["d_base"], key="d_c")
            P.dma("sp", lambda e: e.dma_start(out=elim[:], in_=elim_tab), writes=["d_elim"], key="d_c")
            P.op("pool", lambda e: e.memset(ones_f[:], 1.0), writes=["d_ones"])
            mx_r = Ring([(sb(st, f"d_mx{i}", [128, 16, 512], BF16), f"d_mx{i}") for i in range(2)])
            h0_r = Ring([(sb(st, f"d_h0{i}", [128, D], F32), f"d_h0{i}") for i in range(1)])
            r1_r = Ring([(sb(st, f"d_r1{i}", [128, D], F32), f"d_r1{i}") for i in range(1)])
            h1_r = Ring([(sb(st, f"d_h1{i}", [128, D], F32), f"d_h1{i}") for i in range(2)])
            h1b_r = Ring([(sb(st, f"d_h1b{i}", [128, D], BF16), f"d_h1b{i}") for i in range(2)])
            h1T_r = Ring([(sb(st, f"d_h1T{i}", [128, 16, 128], F32), f"d_h1T{i}") for i in range(1)])
            lnscr = (sb(st, "d_stats", [128, 4, 6], F32), sb(st, "d_mv", [128, 2], F32), sb(st, "d_sd", [128, 1], F32),
                     sb(st, "d_rs", [128, 1], F32), sb(st, "d_nmr", [128, 1], F32))
            lg = sb(st, "d_lg", [128, NE], F32)
            m8 = sb(st, "d_m8", [128, 8], F32)
            sel = sb(st, "d_sel", [128, NE], F32)
            dd = sb(st, "d_dd", [128, NE], F32)
            junk = sb(st, "d_junk", [128, NE], F32)
            nv0 = sb(st, "d_nv0", [128, 1], F32)
            ex4 = sb(st, "d_ex4", [128, 4], F32)
            den1 = sb(st, "d_den1", [128, 1], F32)
            dk = sb(st, "d_dk", [128, 4], F32)
            lk = sb(st, "d_lk", [128, 4], F32)
            ov = sb(st, "d_ov", [128, 4], F32)
            nov = sb(st, "d_nov", [128, 4], F32)
            pw_r = Ring([(ps(st, f"d_pw{i}", [128, 512], F32), f"d_pw{i}") for i in range(4)])
            pt_r = Ring([(ps(st, f"d_pt{i}", [128, 512], F32), f"d_pt{i}") for i in range(2)])
            pl = ps(st, "d_pl", [128, 512], F32)
            pc = ps(st, "d_pc", [128, 512], F32)
            for (s0, w) in tiles[1:]:
                mx, mxk = mx_r.next()
                P.dma("sp", lambda e, mx=mx, s0=s0, w=w: e.dma_start(out=mx[:], in_=MIXT[:, :, s0:s0 + w].rearrange("c p n -> p c n")), writes=[mxk], key=mxk)
                for bi in range(4):
                    n = s0 // 128 + bi
                    h0t, h0k = h0_r.next()
                    P.dma("sp", lambda e, h0t=h0t, n=n: e.dma_start(out=h0t[:], in_=H0[n * 128:(n + 1) * 128, :]), writes=[h0k], key=h0k)
                    r1, r1k = r1_r.next()
                    for ng in range(4):
                        pw, pwk = pw_r.next()

                        def wmm(e, pw=pw, mx=mx, bi=bi, ng=ng):
                            ins = None
                            for c in range(16):
                                ins = e.matmul(pw[:], lhsT=mx[:, c, bi * 128:(bi + 1) * 128], rhs=wo[:, c, ng * 512:(ng + 1) * 512], start=(c == 0), stop=(c == 15))
                            return ins
                        P.op("pe", wmm, reads=[mxk] + wo_keys, writes=[pwk])
                        P.op("dve", lambda e, r1=r1, h0t=h0t, pw=pw, ng=ng: e.scalar_tensor_tensor(out=r1[:, ng * 512:(ng + 1) * 512], in0=h0t[:, ng * 512:(ng + 1) * 512], scalar=ALPHA, in1=pw[:], op0=ALU.mult, op1=ALU.add),
                             reads=[h0k, pwk], writes=[r1k + f"_{ng}"])
                    r1keys = [r1k + f"_{ng}" for ng in range(4)]
                    h1, h1k = h1_r.next()
                    layer_norm_tile(r1keys, r1[:], h1[:], h1k, G1, B1, "d_GB", None, lnscr, "dln")
                    if "DBGM" in dbg:
                        P.dma("sp", lambda e, r1=r1, n=n: e.dma_start(out=DBGM[n * 128:(n + 1) * 128, :], in_=r1[:]), reads=r1keys, key="dbgm")
                    P.dma("sp", lambda e, h1=h1, n=n: e.dma_start(out=H1F[n * 128:(n + 1) * 128, :], in_=h1[:]), reads=[h1k], writes=["H1F"], key=h1k + "s")
                    h1b, h1bk = h1b_r.next()
                    P.op("pool", lambda e, h1b=h1b, h1=h1: e.tensor_copy(out=h1b[:], in_=h1[:]), reads=[h1k], writes=[h1bk])
                    h1T, h1Tk = h1T_r.next()
                    for q4 in range(4):
                        ptp, ptk = pt_r.next()

                        def trf(e, ptp=ptp, h1=h1, q4=q4):
                            ins = None
                            for c4 in range(4):
                                c = q4 * 4 + c4
                                ins = e.transpose(out=ptp[:, c4 * 128:(c4 + 1) * 128], in_=h1[:, c * 128:(c + 1) * 128], identity=ident_f[:])
                            return ins
                        P.op("pe", trf, reads=[h1k, "ident_f"], writes=[ptk])
                        if q4 % 2 == 0:
                            P.op("act", lambda e, ptp=ptp, h1T=h1T, q4=q4: e.copy(out=h1T[:, q4 * 4:(q4 + 1) * 4, :], in_=ptp[:].rearrange("p (c n) -> p c n", c=4)), reads=[ptk], writes=[h1Tk + f"_{q4}"])
                        else:
                            P.op("dve", lambda e, ptp=ptp, h1T=h1T, q4=q4: e.tensor_copy(out=h1T[:, q4 * 4:(q4 + 1) * 4, :], in_=ptp[:].rearrange("p (c n) -> p c n", c=4)), reads=[ptk], writes=[h1Tk + f"_{q4}"])

                    def lmm(e, h1T=h1T):
                        ins = None
                        for c in range(16):
                            ins = e.matmul(pl[:, 0:NE], lhsT=h1T[:, c, :], rhs=rw[:, c, :], start=(c == 0), stop=(c == 15))
                        return ins
                    P.op("pe", lmm, reads=[h1Tk + f"_{q4}" for q4 in range(4)] + ["d_rw"], writes=["d_pl"])
                    P.op("dve", lambda e: e.tensor_tensor(out=lg[:], in0=pl[:, 0:NE], in1=rbb[:], op=ALU.add), reads=["d_pl", "d_rbb"], writes=["d_lg"])
                    P.op("dve", lambda e: e.max(out=m8[:], in_=lg[:]), reads=["d_lg"], writes=["d_m8"])
                    P.op("dve", lambda e, n=n: e.tensor_scalar(out=sel[:], in0=lg[:], scalar1=m8[:, 3:4], scalar2=valid_sb[:, n:n + 1], op0=ALU.is_ge, op1=ALU.mult), reads=["d_lg", "d_m8", "valid"], writes=["d_sel"])
                    P.op("pe", lambda e: e.matmul(pc[:, 0:NE], lhsT=UT[:], rhs=sel[:], start=True, stop=True), reads=["d_sel", "d_UT"], writes=["d_pc0"])
                    P.op("pe", lambda e: e.matmul(pc[:, 64:64 + NE], lhsT=ones_f[:], rhs=sel[:], start=True, stop=True), reads=["d_sel", "d_ones"], writes=["d_pc1"])
                    P.op("dve", lambda e: e.tensor_tensor(out=dd[:], in0=pc[:, 0:NE], in1=base[:], op=ALU.add), reads=["d_pc0", "d_base"], writes=["d_dd"])
                    P.op("dve", lambda e: e.tensor_tensor(out=base[:], in0=pc[:, 64:64 + NE], in1=base[:], op=ALU.add), reads=["d_pc1", "d_base"], writes=["d_base"])
                    P.op("dve", lambda e: e.tensor_scalar(out=nv0[:], in0=m8[:, 0:1], scalar1=-1.0, scalar2=None, op0=ALU.mult), reads=["d_m8"], writes=["d_nv0"])
                    P.op("act", lambda e: e.activation(out=ex4[:], in_=m8[:, 0:4], func=AF.Exp, bias=nv0[:, 0:1], scale=1.0, accum_out=den1[:]), reads=["d_m8", "d_nv0"], writes=["d_ex4", "d_den1"])
                    P.op("dve", lambda e: e.reciprocal(out=den1[:], in_=den1[:]), reads=["d_den1"], writes=["d_den1"])
                    P.op("dve", lambda e: e.tensor_scalar(out=ex4[:], in0=ex4[:], scalar1=den1[:, 0:1], scalar2=None, op0=ALU.mult), reads=["d_ex4", "d_den1"], writes=["d_ex4"])
                    for k in range(4):
                        P.op("dve", lambda e, k=k: e.scalar_tensor_tensor(out=junk[:], in0=lg[:], scalar=m8[:, k:k + 1], in1=dd[:], op0=ALU.is_equal, op1=ALU.mult, accum_out=dk[:, k:k + 1]),
                             reads=["d_lg", "d_m8", "d_dd", "d_junk"], writes=["d_junk", f"d_dk{k}"])
                        P.op("dve", lambda e, k=k: e.scalar_tensor_tensor(out=junk[:], in0=lg[:], scalar=m8[:, k:k + 1], in1=elim[:], op0=ALU.is_equal, op1=ALU.mult, accum_out=lk[:, k:k + 1]),
                             reads=["d_lg", "d_m8", "d_elim", "d_junk"], writes=["d_junk", f"d_lk{k}"])
                    dkk = [f"d_dk{k}" for k in range(4)]
                    lkk = [f"d_lk{k}" for k in range(4)]
                    P.op("dve", lambda e: e.tensor_tensor(out=ov[:], in0=dk[:], in1=lk[:], op=ALU.is_ge), reads=dkk + lkk, writes=["d_ov"])
                    P.op("dve", lambda e: e.tensor_scalar(out=nov[:], in0=ov[:], scalar1=-1.0, scalar2=1.0, op0=ALU.mult, op1=ALU.add), reads=["d_ov"], writes=["d_nov"])
                    P.op("dve", lambda e, n=n: e.tensor_tensor(out=GATE[:, n - 1, :], in0=ex4[:], in1=nov[:], op=ALU.mult), reads=["d_ex4", "d_nov"], writes=[f"GATE{n}"])
                    P.op("dve", lambda e: e.scalar_tensor_tensor(out=ov[:], in0=ov[:], scalar=BIG, in1=dk[:], op0=ALU.mult, op1=ALU.add), reads=["d_ov"] + dkk, writes=["d_ov"])
                    P.op("dve", lambda e, n=n: e.tensor_scalar(out=ov[:], in0=ov[:], scalar1=invbig_sb[:, n:n + 1], scalar2=None, op0=ALU.add), reads=["d_ov", "invbig"], writes=["d_ov"])
                    P.op("dve", lambda e, n=n: e.tensor_copy(out=DEST[:, n - 1, :], in_=ov[:]), reads=["d_ov"], writes=[f"DEST{n}"])
                    if "DBGL" in dbg:
                        P.dma("sp", lambda e, n=n: e.dma_start(out=DBGL[n * 128:(n + 1) * 128, 0:NE], in_=lg[:]), reads=["d_lg"], key="dbgl")
                        P.dma("sp", lambda e, n=n: e.dma_start(out=DBGL[n * 128:(n + 1) * 128, 32:36], in_=ov[:]), reads=["d_ov"], key="dbgl")
                        P.dma("sp", lambda e, n=n: e.dma_start(out=DBGL[n * 128:(n + 1) * 128, 36:40], in_=GATE[:, n - 1, :]), reads=[f"GATE{n}"], key="dbgl")
                    for k in range(4):
                        P.dma("pool", lambda e, h1b=h1b, n=n, k=k: e.indirect_dma_start(out=XE[:, :], out_offset=bass.IndirectOffsetOnAxis(ap=DEST[:, n - 1, k:k + 1], axis=0), in_=h1b[:, :], in_offset=None,
                                                                                   bounds_check=P.bc, oob_is_err=False),
                              reads=[h1bk, f"DEST{n}"], writes=["XE"], key=h1bk + "x")
            P.barrier(bar_marker)
            P.flush()
        if stop_after <= 4:
            P.flush(final=True)
            return nc

        with ExitStack() as st:
            XT = sb(st, "e_XT", [128, 16, CAP], BF16)
            AT = sb(st, "e_AT", [128, 16, CAP], BF16)
            ws_r = Ring([(sb(st, f"e_ws{i}", [128, 16, 256], F32), f"e_ws{i}") for i in range(3)])
            wb_r = Ring([(sb(st, f"e_wb{i}", [128, 16, 256], BF16), f"e_wb{i}") for i in range(3)])
            xr_r = Ring([(sb(st, f"e_xr{i}", [128, D], BF16), f"e_xr{i}") for i in range(2)])
            b1_r = Ring([(sb(st, f"e_b1{i}", [128, 32], F32), f"e_b1{i}") for i in range(2)])
            b2_r = Ring([(sb(st, f"e_b2{i}", [128, D], F32), f"e_b2{i}") for i in range(2)])
            glu_r = Ring([(sb(st, f"e_glu{i}", [128, 512], F32), f"e_glu{i}") for i in range(2)])
            sg_r = Ring([(sb(st, f"e_sg{i}", [128, 512], F32), f"e_sg{i}") for i in range(2)])
            l0_r = Ring([(sb(st, f"e_l0{i}", [128, 512], F32), f"e_l0{i}") for i in range(2)])
            yst_r = Ring([(sb(st, f"e_yst{i}", [128, 256], BF16), f"e_yst{i}") for i in range(3)])
            ptx_r = Ring([(ps(st, f"e_ptx{i}", [128, 1024], BF16), f"e_ptx{i}") for i in range(2)])
            pg_r = Ring([(ps(st, f"e_pg{i}", [128, 512], F32), f"e_pg{i}") for i in range(2)])
            pl_r = Ring([(ps(st, f"e_plin{i}", [128, 512], F32), f"e_plin{i}") for i in range(2)])
            py_r = Ring([(ps(st, f"e_py{i}", [128, 512], F32), f"e_py{i}") for i in range(2)])
            cast_i = [0]

            def cast(ws, wsk, wb, wbk):
                which = ("dve", "act", "dve", "act", "pool")[cast_i[0] % 5]
                cast_i[0] += 1
                if which == "act":
                    P.op("act", lambda e: e.copy(out=wb[:], in_=ws[:]), reads=[wsk], writes=[wbk])
                else:
                    P.op(which, lambda e: e.tensor_copy(out=wb[:], in_=ws[:]), reads=[wsk], writes=[wbk])

            rgs = [(0, 512), (512, 512), (1024, CAP - 1024)] if CAP > 1024 else [(0, 512), (512, CAP - 512)]
            for ex in range(NE):
                b1t, b1k = b1_r.next()
                b2t, b2k = b2_r.next()
                P.dma("sp", lambda e, b1t=b1t, ex=ex: e.dma_start(out=b1t[:], in_=exp_b1r[ex]), writes=[b1k], key=b1k)
                P.dma("sp", lambda e, b2t=b2t, ex=ex: e.dma_start(out=b2t[:], in_=exp_b2[ex].partition_broadcast(128)), writes=[b2k], key=b2k)
                for rt in range(NRT):
                    xrow, xrk = xr_r.next()
                    P.dma("sp", lambda e, xrow=xrow, ex=ex, rt=rt: e.dma_start(out=xrow[:], in_=XE[ex * CAP + rt * 128:ex * CAP + (rt + 1) * 128, :]), writes=[xrk], key=xrk)
                    for half in range(2):
                        tp, tpk = ptx_r.next()

                        def trx(e, tp=tp, xrow=xrow, half=half):
                            ins = None
                            for c8 in range(8):
                                c = half * 8 + c8
                                ins = e.transpose(out=tp[:, c8 * 128:(c8 + 1) * 128], in_=xrow[:, c * 128:(c + 1) * 128], identity=ident_b[:])
                            return ins
                        P.op("pe", trx, reads=[xrk, "ident_b"], writes=[tpk])
                        if half == 0:
                            P.op("act", lambda e, tp=tp, rt=rt: e.copy(out=XT[:, 0:8, rt * 128:(rt + 1) * 128], in_=tp[:].rearrange("p (c n) -> p c n", c=8)), reads=[tpk], writes=[f"e_XT{rt}a"])
                        else:
                            P.op("dve", lambda e, tp=tp, rt=rt: e.tensor_copy(out=XT[:, 8:16, rt * 128:(rt + 1) * 128], in_=tp[:].rearrange("p (c n) -> p c n", c=8)), reads=[tpk], writes=[f"e_XT{rt}b"])
                xt_keys = [f"e_XT{rt}{x}" for rt in range(NRT) for x in "ab"]
                for jp in range(8):
                    wsg, wsgk = ws_r.next()
                    wbg, wbgk = wb_r.next()
                    P.dma("sp", lambda e, wsg=wsg, ex=ex, jp=jp: e.dma_start(out=wsg[:], in_=exp_w1[ex, :, jp * 256:(jp + 1) * 256].rearrange("(c p) n -> p c n", p=128)), writes=[wsgk], key=wsgk)
                    cast(wsg, wsgk, wbg, wbgk)
                    wsl, wslk = ws_r.next()
                    wbl, wblk = wb_r.next()
                    P.dma("sp", lambda e, wsl=wsl, ex=ex, jp=jp: e.dma_start(out=wsl[:], in_=exp_w1[ex, :, D + jp * 256:D + (jp + 1) * 256].rearrange("(c p) n -> p c n", p=128)), writes=[wslk], key=wslk)
                    cast(wsl, wslk, wbl, wblk)
                    for jh in range(2):
                        j = jp * 2 + jh
                        for (r0, rw_) in rgs:
                            pg, pgk = pg_r.next()
                            pln, plk = pl_r.next()

                            def m1(e, pg=pg, wb=wbg, jh=jh, r0=r0, rw_=rw_):
                                ins = None
                                for c in range(16):
                                    ins = e.matmul(pg[:, 0:rw_], lhsT=wb[:, c, jh * 128:(jh + 1) * 128], rhs=XT[:, c, r0:r0 + rw_], start=(c == 0), stop=(c == 15))
                                return ins
                            xk_need = [f"e_XT{rt}{x}" for rt in range(r0 // 128, (r0 + rw_) // 128) for x in "ab"]
                            P.op("pe", m1, reads=xk_need + [wbgk], writes=[pgk])
                            P.op("pe", lambda e, pln=pln, wbl=wbl, jh=jh, r0=r0, rw_=rw_: m1(e, pg=pln, wb=wbl, jh=jh, r0=r0, rw_=rw_), reads=xk_need + [wblk], writes=[plk])
                            glu, gluk = glu_r.next()
                            sg, sgk = sg_r.next()
                            l0, l0k = l0_r.next()
                            P.op("dve", lambda e, glu=glu, pg=pg, b1t=b1t, j=j, rw_=rw_: e.tensor_scalar(out=glu[:, 0:rw_], in0=pg[:, 0:rw_], scalar1=b1t[:, j:j + 1], scalar2=7.0, op0=ALU.add, op1=ALU.min), reads=[pgk, b1k], writes=[gluk])
                            P.op("act", lambda e, sg=sg, glu=glu, rw_=rw_: e.activation(out=sg[:, 0:rw_], in_=glu[:, 0:rw_], func=AF.Sigmoid, scale=1.702), reads=[gluk], writes=[sgk])
                            P.op("act", lambda e, l0=l0, pln=pln, b1t=b1t, j=j, rw_=rw_: e.activation(out=l0[:, 0:rw_], in_=pln[:, 0:rw_], func=AF.Identity, bias=b1t[:, 16 + j:17 + j], scale=1.0), reads=[plk, b1k], writes=[l0k])
                            P.op("dve", lambda e, l0=l0, rw_=rw_: e.tensor_scalar(out=l0[:, 0:rw_], in0=l0[:, 0:rw_], scalar1=7.0, scalar2=-7.0, op0=ALU.min, op1=ALU.max), reads=[l0k], writes=[l0k])
                            P.op("pool", lambda e, sg=sg, glu=glu, rw_=rw_: e.tensor_tensor(out=sg[:, 0:rw_], in0=sg[:, 0:rw_], in1=glu[:, 0:rw_], op=ALU.mult), reads=[sgk, gluk], writes=[sgk])
                            P.op("dve", lambda e, l0=l0, sg=sg, j=j, r0=r0, rw_=rw_: e.scalar_tensor_tensor(out=AT[:, j, r0:r0 + rw_], in0=l0[:, 0:rw_], scalar=1.0, in1=sg[:, 0:rw_], op0=ALU.add, op1=ALU.mult),
                                 reads=[l0k, sgk], writes=[f"e_AT{j}_{r0}"])
                at_keys = [f"e_AT{j}_{r0}" for j in range(16) for (r0, _) in rgs]
                for ng in range(8):
                    ws2, ws2k = ws_r.next()
                    wb2, wb2k = wb_r.next()
                    P.dma("sp", lambda e, ws2=ws2, ex=ex, ng=ng: e.dma_start(out=ws2[:], in_=exp_w2[ex, :, ng * 256:(ng + 1) * 256].rearrange("(c p) n -> p c n", p=128)), writes=[ws2k], key=ws2k)
                    cast(ws2, ws2k, wb2, wb2k)
                    for rt in range(NRT):
                        py, pyk = py_r.next()

                        def m2(e, py=py, wb2=wb2, rt=rt):
                            ins = None
                            for c in range(16):
                                ins = e.matmul(py[:, 0:256], lhsT=AT[:, c, rt * 128:(rt + 1) * 128], rhs=wb2[:, c, :], start=(c == 0), stop=(c == 15))
                            return ins
                        P.op("pe", m2, reads=at_keys + [wb2k], writes=[pyk])
                        yst, ystk = yst_r.next()
                        P.op("dve", lambda e, yst=yst, py=py, b2t=b2t, ng=ng: e.tensor_tensor(out=yst[:], in0=py[:, 0:256], in1=b2t[:, ng * 256:(ng + 1) * 256], op=ALU.add), reads=[pyk, b2k], writes=[ystk])
                        P.dma("sp", lambda e, yst=yst, ex=ex, rt=rt, ng=ng: e.dma_start(out=YE[ex * CAP + rt * 128:ex * CAP + (rt + 1) * 128, ng * 256:(ng + 1) * 256], in_=yst[:]), reads=[ystk], writes=["YE"], key=ystk + "s")
            P.barrier(bar_marker)
            P.flush()
        if stop_after <= 5:
            P.flush(final=True)
            return nc

        with ExitStack() as st:
            G2 = sb(st, "f_G2", [128, D], F32)
            B2 = sb(st, "f_B2", [128, D], F32)
            P.dma("sp", lambda e: e.dma_start(out=G2[:], in_=ln2_g.partition_broadcast(128)), writes=["f_GB"], key="f_c")
            P.dma("sp", lambda e: e.dma_start(out=B2[:], in_=ln2_b.partition_broadcast(128)), writes=["f_GB"], key="f_c")
            yg_r = Ring([(sb(st, f"f_yg{i}", [128, D], BF16), f"f_yg{i}") for i in range(6)])
            h1_r = Ring([(sb(st, f"f_h1{i}", [128, D], F32), f"f_h1{i}") for i in range(2)])
            acc_r = Ring([(sb(st, f"f_acc{i}", [128, D], F32), f"f_acc{i}") for i in range(2)])
            out_r = Ring([(sb(st, f"f_out{i}", [128, D], F32), f"f_out{i}") for i in range(2)])
            lnscr = (sb(st, "f_stats", [128, 4, 6], F32), sb(st, "f_mv", [128, 2], F32), sb(st, "f_sd", [128, 1], F32),
                     sb(st, "f_rs", [128, 1], F32), sb(st, "f_nmr", [128, 1], F32))
            for i in range(6):
                P.op("pool", lambda e, i=i: e.memset(yg_r.items[i][0][:], 0.0), writes=[yg_r.items[i][1]])
            for n in range(1, 65):
                h1, h1k = h1_r.next()
                P.dma("sp", lambda e, h1=h1, n=n: e.dma_start(out=h1[:], in_=H1F[n * 128:(n + 1) * 128, :]), writes=[h1k], key=h1k)
                acc, acck = acc_r.next()
                for k in range(4):
                    yg, ygk = yg_r.next()
                    P.dma("pool", lambda e, yg=yg, n=n, k=k: e.indirect_dma_start(out=yg[:, :], out_offset=None, in_=YE[:, :], in_offset=bass.IndirectOffsetOnAxis(ap=DEST[:, n - 1, k:k + 1], axis=0),
                                                                               bounds_check=P.bc, oob_is_err=False),
                          reads=[f"DEST{n}"], writes=[ygk], key=ygk)
                    if k == 0:
                        P.op("dve", lambda e, acc=acc, h1=h1, yg=yg, n=n: e.tensor_scalar(out=acc[:], in0=yg[:], scalar1=GATE[:, n - 1, 0:1], scalar2=None, op0=ALU.mult), reads=[ygk, f"GATE{n}"], writes=[acck])
                    else:
                        P.op("dve", lambda e, acc=acc, yg=yg, n=n, k=k: e.scalar_tensor_tensor(out=acc[:], in0=yg[:], scalar=GATE[:, n - 1, k:k + 1], in1=acc[:], op0=ALU.mult, op1=ALU.add), reads=[ygk, f"GATE{n}", acck], writes=[acck])
                P.op("dve", lambda e, acc=acc, h1=h1: e.scalar_tensor_tensor(out=acc[:], in0=h1[:], scalar=ALPHA, in1=acc[:], op0=ALU.mult, op1=ALU.add), reads=[h1k, acck], writes=[acck])
                ot, otk = out_r.next()
                layer_norm_tile(acck, acc[:], ot[:], otk, G2, B2, "f_GB", None, lnscr, "fln")
                P.dma("sp", lambda e, ot=ot, n=n: e.dma_start(out=y_out[(n - 1) * 128:n * 128, :], in_=ot[:]), reads=[otk], writes=["y_out"], key=otk + "s")
            P.barrier(bar_marker)
            P.flush()
        P.flush(final=True)
    return nc


def host_prep(x_seq, L, meta_tokens):
    xs = np.zeros((T, D), np.float32)
    xs[112:128] = meta_tokens
    xs[128:128 + L] = x_seq
    valid = np.zeros(T, np.float32)
    valid[112:128 + L] = 1.0
    kb = np.full(T, NEG, np.float32)
    kb[112:128 + L] = 0.0
    tm = lambda a: np.ascontiguousarray(a.reshape(NB, 128).T)
    return {
        "xs": xs,
        "valid_tm": tm(valid),
        "invbig_tm": tm((1.0 - valid) * BIG),
        "kbias_tm": tm(kb),
        "vmask_fm": np.ascontiguousarray(np.broadcast_to(valid[None, :], (128, T))),
    }


def common_inputs(inp):
    bk, mk = static_tables()
    ut = (np.arange(128)[:, None] <= np.arange(128)[None, :]).astype(np.float32)
    ecoff = np.broadcast_to((np.arange(NE) * CAP - 1).astype(np.float32)[None, :], (128, NE))
    elim = np.broadcast_to(((np.arange(NE) + 1) * CAP).astype(np.float32)[None, :], (128, NE))
    c = {
        "bk_tab": bk, "mk_tab": mk, "ut_tab": ut,
        "ecoff_tab": np.ascontiguousarray(ecoff), "elim_tab": np.ascontiguousarray(elim),
        "ident_tab": np.eye(128, dtype=np.float32),
    }
    f = lambda a: np.ascontiguousarray(np.asarray(a, np.float32))
    c["ln_in_g"] = f(inp["ln_in_g"]); c["ln_in_b"] = f(inp["ln_in_b"])
    c["rel_bias"] = f(inp["rel_bias"])
    c["w_in"] = f(inp["w_in"][0])
    rows = [inp["conv_w"][0][j] for j in range(4)] + [inp["conv_b"][0], inp["lru_ba"][0][0], inp["lru_ba"][0][1],
                                                    inp["lru_bi"][0][0], inp["lru_bi"][0][1], inp["lru_lam"][0][0], inp["lru_lam"][0][1]]
    par = np.stack([np.asarray(r, np.float32) for r in rows], axis=0)
    c["lru_par"] = np.ascontiguousarray(par.reshape(11, 8, 128).transpose(2, 1, 0))
    c["lru_wa"] = f(inp["lru_wa"][0])
    c["lru_wi"] = f(inp["lru_wi"][0])
    c["attn_sink"] = f(inp["attn_sink"][0])
    c["w_out"] = f(inp["w_out"][0])
    c["ln1_g"] = f(inp["ln1_g"][0]); c["ln1_b"] = f(inp["ln1_b"][0])
    c["router_w"] = f(inp["router_w"][0]); c["router_b"] = f(inp["router_b"][0])
    c["exp_w1"] = f(inp["exp_w1"][0]); c["exp_b1r"] = np.ascontiguousarray(f(inp["exp_b1"][0]).reshape(NE, 32, 128).transpose(0, 2, 1))
    c["exp_w2"] = f(inp["exp_w2"][0]); c["exp_b2"] = f(inp["exp_b2"][0])
    c["ln2_g"] = f(inp["ln2_g"][0]); c["ln2_b"] = f(inp["ln2_b"][0])
    return c


def kernel(**inp):
    xp = np.asarray(inp["x_prompt"], np.float32)
    xsm = np.asarray(inp["x_sample"], np.float32)
    meta = np.asarray(inp["meta_tokens"], np.float32)
    common = common_inputs(inp)
    seqs = [(xsm[i], 8192) for i in range(4)] + [(xp[i], 4096) for i in range(2)] + [(xp[0], 4096), (xp[1], 4096)]
    in_maps = []
    for (xq, L) in seqs:
        m = dict(common)
        m.update(host_prep(xq, L, meta))
        in_maps.append(m)
    nc = build()
    res = run_bass_kernel_spmd(nc, in_maps, core_ids=list(range(8)))
    ys = [r["y_out"] for r in res.results]
    y_sample = np.stack([ys[i][:8192] for i in range(4)], axis=0).astype(np.float32)
    y_prompt = np.stack([ys[4 + i][:4096] for i in range(2)], axis=0).astype(np.float32)
    return (y_prompt, y_sample)
```

```python
import numpy as np
import concourse.bass as bass
import concourse.mybir as mybir
from concourse.bass_utils import run_bass_kernel_spmd
from contextlib import ExitStack

F32 = mybir.dt.float32
BF16 = mybir.dt.bfloat16
I32 = mybir.dt.int32
AF = mybir.ActivationFunctionType
ALU = mybir.AluOpType

D = 2048
NCH = 16
NB = 65
T = NB * 128
NE = 32
CAP = 1280
NRT = CAP // 128
XROWS = NE * CAP
ALPHA = 2.0 ** 0.25
EPS = 1e-5
NEG = -30000.0
BIG = 1.0e6
ENGS = ["pe", "act", "dve", "pool", "sp"]
SEM_ROT = 30000
DRAM_KEYS = {"H0", "QT", "KT", "VV", "XR", "YG", "HB", "MIXT", "H1F", "XE", "YE", "WIN", "XRhalo", "y_out"}


class Prog:
    def __init__(self, nc, stack):
        self.nc = nc
        self.stack = stack
        self.ops = {e: [] for e in ENGS}
        self.esem = {}
        self.ecnt = {e: 0 for e in ENGS}
        self.waited = {e: {} for e in ENGS}
        self.dsem = {}
        self.lastw = {}
        self.readers = {}
        self.nsem = 0
        self.nbar = 0
        self.bc = None

    def _newsem(self, name):
        self.nsem += 1
        return self.stack.enter_context(self.nc.semaphore(f"{name}_{self.nsem}"))

    def _resolve(self, tok):
        if tok[0] == "e":
            return tok[2], tok[3]
        ds = self.dsem[tok[1]]
        return ds[0], ds[1]

    def _collect(self, e, reads, writes):
        toks = []
        for k in reads:
            if k in self.lastw:
                toks.append(self.lastw[k])
        for k in writes:
            if k in self.lastw:
                toks.append(self.lastw[k])
            toks.extend(self.readers.get(k, ()))
        waits = []
        for t in toks:
            sem, v = self._resolve(t)
            sid = id(sem)
            if self.waited[e].get(sid, 0) >= v:
                continue
            self.waited[e][sid] = v
            waits.append((sem, v))
        return waits

    def _update(self, tok, reads, writes):
        for k in reads:
            self.readers.setdefault(k, []).append(tok)
        for k in writes:
            self.lastw[k] = tok
            self.readers[k] = []

    def op(self, e, fn, reads=(), writes=()):
        waits = self._collect(e, reads, writes)
        if e not in self.esem or self.ecnt[e] >= SEM_ROT:
            self.esem[e] = self._newsem("e" + e)
            self.ecnt[e] = 0
        sem = self.esem[e]
        self.ecnt[e] += 1
        tok = ("e", e, sem, self.ecnt[e])
        self.ops[e].append((waits, fn, (sem, 1)))
        self._update(tok, reads, writes)

    def dma(self, q, fn, reads=(), writes=(), key=None, allow_ww=False):
        for k in writes:
            if (not allow_ww) and k in RING_KEYS and k in self.lastw and self.lastw[k][0] == "d" and not self.readers.get(k):
                raise RuntimeError(f"ring slot {k} overwritten before any recorded reader")
        waits = self._collect(q, reads, writes)
        if key not in self.dsem:
            self.dsem[key] = [self._newsem("d"), 0]
        ds = self.dsem[key]
        ds[1] += 16
        self.ops[q].append((waits, fn, (ds[0], 16)))
        self._update(("d", key), reads, writes)

    def barrier(self, marker_fn):
        self.nbar += 1
        n = self.nbar
        waits = []
        cands = [(s, v) for (s, v) in self.dsem.values()]
        for e in ENGS:
            if e != "sp" and e in self.esem:
                cands.append((self.esem[e], self.ecnt[e]))
        for s, v in cands:
            if self.waited["sp"].get(id(s), 0) < v:
                self.waited["sp"][id(s)] = v
                waits.append((s, v))
        self.ops["sp"].append((waits, None, None))
        self.dma("sp", marker_fn, writes=[f"bar{n}"], key="bar")
        for x in ("act", "dve", "pool", "pe"):
            self.ops[x].append((self._collect(x, [f"bar{n}"], []), None, None))

    def flush(self, final=False):
        nc = self.nc
        fin = []
        if final:
            for k, (s, v) in self.dsem.items():
                fin.append((s, v))
            for e in ENGS:
                if e in self.esem and e != "sp":
                    fin.append((self.esem[e], self.ecnt[e]))
        handles = {"pe": "tensor", "act": "scalar", "dve": "vector", "pool": "gpsimd", "sp": "sync"}
        with nc.Block() as block:
            for e in ENGS:
                ops = self.ops[e]
                last = final and (e == "sp")

                def body(eng, ops=ops, last=last, e=e):
                    if e == "pool" and self.bc is None:
                        r = eng.alloc_register("bcreg")
                        eng.reg_mov(r, XROWS - 1)
                        self.bc = eng.snap(r)
                    for waits, fn, inc in ops:
                        for s, v in waits:
                            eng.wait_ge(s, v)
                        if fn is not None:
                            ins = fn(eng)
                            ins.then_inc(inc[0], inc[1])
                    if last:
                        for s, v in fin:
                            eng.wait_ge(s, v)

                getattr(block, handles[e])(body)
        self.ops = {e: [] for e in ENGS}


RING_KEYS = set()


class Ring:
    def __init__(self, items):
        self.items = items
        self.i = 0
        for it in items:
            RING_KEYS.add(it[1])

    def next(self):
        it = self.items[self.i % len(self.items)]
        self.i += 1
        return it


def t5_bucket_np(rel):
    n = np.abs(rel)
    large = 8 + (np.log(np.maximum(n, 1).astype(np.float32) / 8) / np.log(128 / 8) * 8).astype(np.int32)
    large = np.minimum(large, 15)
    return np.where(rel > 0, 16, 0) + np.where(n < 8, n, large)


def static_tables():
    k = np.arange(128)[:, None]
    q = np.arange(128)[None, :]
    bk = np.zeros((4, 128, 128), np.float32)
    mk = np.zeros((4, 128, 128), np.float32)
    for vi, j in enumerate((-1, 0, 1)):
        rel = 128 * j + k - q
        bk[vi] = t5_bucket_np(rel)
        mk[vi] = np.where(np.abs(rel) <= 128, 0.0, NEG)
    rel = k - (128 + q)
    bk[3] = t5_bucket_np(rel)
    mk[3] = 0.0
    return bk, mk


def build(stop_after=99, dbg=()):
    nc = bass.Bass("TRN2", target_bir_lowering=False)

    def din(name, shape, dt=F32):
        return nc.dram_tensor(name, list(shape), dt, kind="ExternalInput").ap()

    def dscr(name, shape, dt):
        kind = "ExternalOutput" if name in dbg else "Internal"
        return nc.dram_tensor(name, list(shape), dt, kind=kind).ap()

    xs = din("xs", [T, D])
    valid_tm = din("valid_tm", [128, NB])
    invbig_tm = din("invbig_tm", [128, NB])
    kbias_tm = din("kbias_tm", [128, NB])
    vmask_fm = din("vmask_fm", [128, T])
    bk_tab = din("bk_tab", [4, 128, 128])
    mk_tab = din("mk_tab", [4, 128, 128])
    ut_tab = din("ut_tab", [128, 128])
    ecoff_tab = din("ecoff_tab", [128, NE])
    elim_tab = din("elim_tab", [128, NE])
    ident_tab = din("ident_tab", [128, 128])
    ln_in_g = din("ln_in_g", [D]); ln_in_b = din("ln_in_b", [D])
    rel_bias = din("rel_bias", [32, 8])
    w_in = din("w_in", [D, 3584])
    lru_par = din("lru_par", [128, 8, 11])
    lru_wa = din("lru_wa", [2, 8, 128, 128])
    lru_wi = din("lru_wi", [2, 8, 128, 128])
    attn_sink = din("attn_sink", [8])
    w_out = din("w_out", [D, D])
    ln1_g = din("ln1_g", [D]); ln1_b = din("ln1_b", [D])
    router_w = din("router_w", [D, NE]); router_b = din("router_b", [NE])
    exp_w1 = din("exp_w1", [NE, D, 2 * D]); exp_b1r = din("exp_b1r", [NE, 128, 32])
    exp_w2 = din("exp_w2", [NE, D, D]); exp_b2 = din("exp_b2", [NE, D])
    ln2_g = din("ln2_g", [D]); ln2_b = din("ln2_b", [D])
    y_out = nc.dram_tensor("y_out", [64 * 128, D], F32, kind="ExternalOutput").ap()

    WIN = dscr("WIN", [28, 128, 2048], BF16)
    H0 = dscr("H0", [T, D], F32)
    QT = dscr("QT", [8, 128, T], BF16)
    KT = dscr("KT", [2, 128, T], BF16)
    VV = dscr("VV", [T, 256], BF16)
    XR = dscr("XR", [8, 128, T + 4], F32)
    YG = dscr("YG", [8, 128, T], F32)
    HB = dscr("HB", [8, 128, T], F32)
    MIXT = dscr("MIXT", [16, 128, T], BF16)
    H1F = dscr("H1F", [T, D], F32)
    XE = dscr("XE", [XROWS, D], BF16)
    YE = dscr("YE", [XROWS, D], BF16)
    DBGM = dscr("DBGM", [T, D], F32)
    DBGL = dscr("DBGL", [T, 64], F32)

    with ExitStack() as gst:
        P = Prog(nc, gst)

        def sb(st, name, shape, dt):
            return st.enter_context(nc.sbuf_tensor(name, list(shape), dt))

        def ps(st, name, shape, dt):
            return st.enter_context(nc.psum_tensor(name, list(shape), dt))

        barsb = sb(gst, "barsb", [128, 16], F32)
        ident_b = sb(gst, "ident_b", [128, 128], BF16)
        ident_f = sb(gst, "ident_f", [128, 128], F32)
        valid_sb = sb(gst, "valid_sb", [128, NB], F32)
        invbig_sb = sb(gst, "invbig_sb", [128, NB], F32)
        kbias_sb = sb(gst, "kbias_sb", [128, NB], F32)
        DEST = sb(gst, "DEST", [128, 64, 4], I32)
        GATE = sb(gst, "GATE", [128, 64, 4], F32)
        eps_sb = sb(gst, "eps_sb", [128, 1], F32)

        bar_marker = lambda e: e.dma_start(out=barsb[0:1, 0:16], in_=valid_tm[0:1, 0:16])
        P.op("pool", lambda e: e.memset(eps_sb[:], EPS), writes=["eps"])
        P.dma("sp", lambda e: e.dma_start(out=ident_f[:], in_=ident_tab), writes=["ident_f"], key="g0")
        P.dma("sp", lambda e: e.dma_start(out=valid_sb[:], in_=valid_tm), writes=["valid"], key="g0")
        P.dma("sp", lambda e: e.dma_start(out=invbig_sb[:], in_=invbig_tm), writes=["invbig"], key="g0")
        P.dma("sp", lambda e: e.dma_start(out=kbias_sb[:], in_=kbias_tm), writes=["kbias"], key="g0")
        P.op("dve", lambda e: e.tensor_copy(out=ident_b[:], in_=ident_f[:]), reads=["ident_f"], writes=["ident_b"])

        def layer_norm_tile(st_key, src, dst, dkey, Gt, Bt, gkey, valid_col, scr, tag):
            stats, mv, sd, rs, nmr = scr
            skeys = st_key if isinstance(st_key, list) else [st_key]
            for i in range(4):
                P.op("dve", lambda e, i=i: e.bn_stats(out=stats[:, i, :], in_=src[:, i * 512:(i + 1) * 512]),
                     reads=skeys, writes=[f"{tag}_stats{i}"])
            P.op("dve", lambda e: e.bn_aggr(out=mv[:], in_=stats[:].rearrange("p a b -> p (a b)")),
                 reads=[f"{tag}_stats{i}" for i in range(4)], writes=[f"{tag}_mv"])
            P.op("act", lambda e: e.activation(out=sd[:], in_=mv[:, 1:2], func=AF.Sqrt, bias=eps_sb[:, 0:1], scale=1.0),
                 reads=[f"{tag}_mv", "eps"], writes=[f"{tag}_sd"])
            P.op("dve", lambda e: e.reciprocal(out=rs[:], in_=sd[:]), reads=[f"{tag}_sd"], writes=[f"{tag}_rs"])
            if valid_col is not None:
                P.op("dve", lambda e: e.tensor_tensor(out=rs[:], in0=rs[:], in1=valid_col, op=ALU.mult),
                     reads=[f"{tag}_rs", "valid"], writes=[f"{tag}_rs"])
            P.op("dve", lambda e: e.scalar_tensor_tensor(out=nmr[:], in0=mv[:, 0:1], scalar=-1.0, in1=rs[:], op0=ALU.mult, op1=ALU.mult),
                 reads=[f"{tag}_mv", f"{tag}_rs"], writes=[f"{tag}_nmr"])
            P.op("act", lambda e: e.activation(out=dst, in_=src, func=AF.Identity, scale=rs[:, 0:1], bias=nmr[:, 0:1]),
                 reads=skeys + [f"{tag}_rs", f"{tag}_nmr"], writes=[dkey])
            P.op("dve", lambda e: e.tensor_tensor(out=dst, in0=dst, in1=Gt[:], op=ALU.mult),
                 reads=[dkey, gkey], writes=[dkey])
            if valid_col is not None:
                P.op("dve", lambda e: e.scalar_tensor_tensor(out=dst, in0=Bt[:], scalar=valid_col, in1=dst, op0=ALU.mult, op1=ALU.add),
                     reads=[dkey, gkey, "valid"], writes=[dkey])
            else:
                P.op("dve", lambda e: e.tensor_tensor(out=dst, in0=dst, in1=Bt[:], op=ALU.add),
                     reads=[dkey, gkey], writes=[dkey])

        with ExitStack() as st:
            wst = [sb(st, f"p0_wst{i}", [128, 16, 256], F32) for i in range(2)]
            wbf = [sb(st, f"p0_wbf{i}", [128, 16, 256], BF16) for i in range(2)]
            for j2 in range(14):
                i = j2 % 2
                P.dma("sp", lambda e, i=i, j2=j2: e.dma_start(out=wst[i][:], in_=w_in[:, j2 * 256:(j2 + 1) * 256].rearrange("(c p) n -> p c n", p=128)),
                      writes=[f"p0_wst{i}"], key=f"p0_l{i}")
                eng = "dve" if i == 0 else "act"
                if eng == "dve":
                    P.op("dve", lambda e, i=i: e.tensor_copy(out=wbf[i][:], in_=wst[i][:]), reads=[f"p0_wst{i}"], writes=[f"p0_wbf{i}"])
                else:
                    P.op("act", lambda e, i=i: e.copy(out=wbf[i][:], in_=wst[i][:]), reads=[f"p0_wst{i}"], writes=[f"p0_wbf{i}"])
                for h in range(2):
                    P.dma("sp", lambda e, i=i, j2=j2, h=h: e.dma_start(out=WIN[2 * j2 + h].rearrange("p (c n) -> p c n", c=16), in_=wbf[i][:, :, h * 128:(h + 1) * 128]),
                          reads=[f"p0_wbf{i}"], writes=["WIN"], key=f"p0_s{i}")
            zt = sb(st, "p0_zt", [128, 8, 2], F32)
            P.op("dve", lambda e: e.memset(zt[:], 0.0), writes=["p0_zt"])
            P.dma("sp", lambda e: e.dma_start(out=XR[:, :, 0:2].rearrange("c p n -> p c n"), in_=zt[:]), reads=["p0_zt"], writes=["XRhalo"], key="p0_z")
            P.dma("sp", lambda e: e.dma_start(out=XR[:, :, T + 2:T + 4].rearrange("c p n -> p c n"), in_=zt[:]), reads=["p0_zt"], writes=["XRhalo"], key="p0_z")
            P.barrier(bar_marker)
            P.flush()

        tiles = [(0, 128)] + [(128 + 512 * i, 512) for i in range(16)]
        with ExitStack() as st:
            Gt = sb(st, "p1_G", [128, D], F32)
            Bt = sb(st, "p1_B", [128, D], F32)
            P.dma("sp", lambda e: e.dma_start(out=Gt[:], in_=ln_in_g.partition_broadcast(128)), writes=["p1_GB"], key="p1_gb")
            P.dma("sp", lambda e: e.dma_start(out=Bt[:], in_=ln_in_b.partition_broadcast(128)), writes=["p1_GB"], key="p1_gb")
            xt_r = Ring([(sb(st, f"p1_xt{i}", [128, D], F32), f"p1_xt{i}") for i in range(4)])
            h0_r = Ring([(sb(st, f"p1_h0{i}", [128, D], F32), f"p1_h0{i}") for i in range(2)])
            hb_r = Ring([(sb(st, f"p1_hb{i}", [128, D], BF16), f"p1_hb{i}") for i in range(2)])
            hT_r = Ring([(sb(st, f"p1_hT{i}", [128, 16, 512], BF16), f"p1_hT{i}") for i in range(2)])
            wc_r = Ring([(sb(st, f"p1_wc{i}", [128, 16, 128], BF16), f"p1_wc{i}") for i in range(6)])
            sf_r = Ring([(sb(st, f"p1_sf{i}", [128, 512], F32), f"p1_sf{i}") for i in range(3)])
            sh_r = Ring([(sb(st, f"p1_sh{i}", [128, 512], BF16), f"p1_sh{i}") for i in range(3)])
            sv_r = Ring([(sb(st, f"p1_sv{i}", [128, 256], BF16), f"p1_sv{i}") for i in range(2)])
            lnscr = (sb(st, "p1_stats", [128, 4, 6], F32), sb(st, "p1_mv", [128, 2], F32), sb(st, "p1_sd", [128, 1], F32),
                     sb(st, "p1_rs", [128, 1], F32), sb(st, "p1_nmr", [128, 1], F32))
            tp_r = Ring([(ps(st, f"p1_tp{i}", [128, 1024], BF16), f"p1_tp{i}") for i in range(2)])
            pj_r = Ring([(ps(st, f"p1_pj{i}", [128, 512], F32), f"p1_pj{i}") for i in range(4)])
            QSCALE = 128.0 ** -0.5

            tstate = {}

            def t_xload(ti):
                if ti >= len(tiles):
                    return
                s0, w = tiles[ti]
                hT, hTk = hT_r.next()
                xs_l = []
                for bi in range(w // 128):
                    b = s0 // 128 + bi
                    xt, xk = xt_r.next()
                    P.dma("sp", lambda e, xt=xt, b=b: e.dma_start(out=xt[:], in_=xs[b * 128:(b + 1) * 128, :]), writes=[xk], key=xk)
                    xs_l.append((xt, xk))
                tstate[ti] = {"hT": hT, "hTk": hTk, "x": xs_l, "keys": []}

            def t_ln(ti, bi):
                if ti >= len(tiles):
                    return
                s0, w = tiles[ti]
                if bi >= w // 128:
                    return
                stt = tstate[ti]
                hT, hTk = stt["hT"], stt["hTk"]
                xt, xk = stt["x"][bi]
                b = s0 // 128 + bi
                h0t, h0k = h0_r.next()
                hbt, hbk = hb_r.next()
                layer_norm_tile(xk, xt[:], h0t[:], h0k, Gt, Bt, "p1_GB", valid_sb[:, b:b + 1], lnscr, "p1ln")
                P.op("pool", lambda e, hbt=hbt, h0t=h0t: e.tensor_copy(out=hbt[:], in_=h0t[:]), reads=[h0k], writes=[hbk])
                P.dma("sp", lambda e, h0t=h0t, b=b: e.dma_start(out=H0[b * 128:(b + 1) * 128, :], in_=h0t[:]), reads=[h0k], writes=["H0"], key=h0k + "s")
                for half in range(2):
                    tp, tpk = tp_r.next()

                    def tr(e, tp=tp, hbt=hbt, half=half):
                        ins = None
                        for c8 in range(8):
                            c = half * 8 + c8
                            ins = e.transpose(out=tp[:, c8 * 128:(c8 + 1) * 128], in_=hbt[:, c * 128:(c + 1) * 128], identity=ident_b[:])
                        return ins
                    P.op("pe", tr, reads=[hbk, "ident_b"], writes=[tpk])
                    if half == 0:
                        P.op("act", lambda e, tp=tp, hT=hT, bi=bi: e.copy(out=hT[:, 0:8, bi * 128:(bi + 1) * 128], in_=tp[:].rearrange("p (c n) -> p c n", c=8)),
                             reads=[tpk], writes=[hTk + f"_{bi}a"])
                    else:
                        P.op("dve", lambda e, tp=tp, hT=hT, bi=bi: e.tensor_copy(out=hT[:, 8:16, bi * 128:(bi + 1) * 128], in_=tp[:].rearrange("p (c n) -> p c n", c=8)),
                             reads=[tpk], writes=[hTk + f"_{bi}b"])
                    stt["keys"].append(hTk + f"_{bi}{'ab'[half]}")

            items = []
            for ti, (s0, w) in enumerate(tiles):
                for bi in range(w // 128):
                    items.append((ti, "v", bi))
                chunks = (list(range(8, 10)) + list(range(12, 20))) if s0 == 0 else (list(range(0, 10)) + list(range(12, 28)))
                for j in chunks:
                    items.append((ti, "f", j))
            istate = {}

            def i_load(i):
                if i >= len(items):
                    return
                ti, kind, p_ = items[i]
                ids = [10, 11] if kind == "v" else [p_]
                lst = []
                for cid in ids:
                    wc, wck = wc_r.next()
                    P.dma("sp", lambda e, wc=wc, cid=cid: e.dma_start(out=wc[:], in_=WIN[cid].rearrange("p (c n) -> p c n", c=16)), writes=[wck], key=wck)
                    lst.append((wc, wck))
                istate[i] = lst

            t_xload(0)
            t_ln(0, 0)
            t_xload(1)
            i_load(0)
            i_load(1)
            per_tile_count = {}
            for (ti, kind, p_) in items:
                per_tile_count[ti] = per_tile_count.get(ti, 0) + 1
            seen_in_tile = {}
            for i, (ti, kind, p_) in enumerate(items):
                i_load(i + 2)
                s0, w = tiles[ti]
                stt = tstate[ti]
                hT, hTk = stt["hT"], stt["hTk"]
                nbk = w // 128
                hT_keys = [hTk + f"_{bi}{x}" for bi in range(nbk) for x in "ab"]
                k_in = seen_in_tile.get(ti, 0)
                seen_in_tile[ti] = k_in + 1
                n_it = per_tile_count[ti]
                for bi_n in range(4):
                    if k_in == (bi_n * n_it) // 4:
                        if bi_n == 0 and ti + 2 < len(tiles) + 1:
                            pass
                        t_ln(ti + 1, bi_n)
                if k_in == n_it - 1:
                    t_xload(ti + 2)
                wcs = istate.pop(i)
                if kind == "v":
                    bi = p_
                    b = s0 // 128 + bi
                    pj, pjk = pj_r.next()

                    def vmm(e, pj=pj, hT=hT, bi=bi, wcs=wcs):
                        ins = None
                        for h in range(2):
                            for c in range(16):
                                ins = e.matmul(pj[:, h * 128:(h + 1) * 128], lhsT=hT[:, c, bi * 128:(bi + 1) * 128], rhs=wcs[h][0][:, c, :], start=(c == 0), stop=(c == 15))
                        return ins
                    P.op("pe", vmm, reads=[hTk + f"_{bi}a", hTk + f"_{bi}b", wcs[0][1], wcs[1][1]], writes=[pjk])
                    sv, svk = sv_r.next()
                    P.op("act", lambda e, sv=sv, pj=pj: e.copy(out=sv[:], in_=pj[:, 0:256]), reads=[pjk], writes=[svk])
                    P.dma("sp", lambda e, sv=sv, b=b: e.dma_start(out=VV[b * 128:(b + 1) * 128, :], in_=sv[:]), reads=[svk], writes=["VV"], key=svk + "s")
                else:
                    j = p_
                    wc, wck = wcs[0]
                    pj, pjk = pj_r.next()

                    def pmm(e, pj=pj, hT=hT, wc=wc, w=w):
                        ins = None
                        for c in range(16):
                            ins = e.matmul(pj[:, 0:w], lhsT=wc[:, c, :], rhs=hT[:, c, 0:w], start=(c == 0), stop=(c == 15))
                        return ins
                    P.op("pe", pmm, reads=hT_keys + [wck], writes=[pjk])
                    if j < 8:
                        sh, shk = sh_r.next()
                        P.op("act", lambda e, sh=sh, pj=pj, w=w: e.activation(out=sh[:, 0:w], in_=pj[:, 0:w], func=AF.Copy, scale=QSCALE), reads=[pjk], writes=[shk])
                        P.dma("sp", lambda e, sh=sh, j=j, s0=s0, w=w: e.dma_start(out=QT[j, :, s0:s0 + w], in_=sh[:, 0:w]), reads=[shk], writes=["QT"], key=shk + "s")
                    elif j < 10:
                        sh, shk = sh_r.next()
                        P.op("dve", lambda e, sh=sh, pj=pj, w=w: e.tensor_copy(out=sh[:, 0:w], in_=pj[:, 0:w]), reads=[pjk], writes=[shk])
                        P.dma("sp", lambda e, sh=sh, j=j, s0=s0, w=w: e.dma_start(out=KT[j - 8, :, s0:s0 + w], in_=sh[:, 0:w]), reads=[shk], writes=["KT"], key=shk + "s")
                    elif j < 20:
                        sf, sfk = sf_r.next()
                        P.op("dve", lambda e, sf=sf, pj=pj, w=w: e.tensor_copy(out=sf[:, 0:w], in_=pj[:, 0:w]), reads=[pjk], writes=[sfk])
                        P.dma("sp", lambda e, sf=sf, j=j, s0=s0, w=w: e.dma_start(out=XR[j - 12, :, 2 + s0:2 + s0 + w], in_=sf[:, 0:w]), reads=[sfk], writes=["XR"], key=sfk + "s")
                    else:
                        sf, sfk = sf_r.next()
                        P.op("act", lambda e, sf=sf, pj=pj, w=w: e.activation(out=sf[:, 0:w], in_=pj[:, 0:w], func=AF.Gelu), reads=[pjk], writes=[sfk])
                        P.dma("sp", lambda e, sf=sf, j=j, s0=s0, w=w: e.dma_start(out=YG[j - 20, :, s0:s0 + w], in_=sf[:, 0:w]), reads=[sfk], writes=["YG"], key=sfk + "s")
            P.barrier(bar_marker)
            P.flush()
        if stop_after <= 1:
            P.flush(final=True)
            return nc

        for z in (1, 0):
            with ExitStack() as st:
                par = sb(st, f"l{z}_par", [128, 8, 11], F32)
                one_c = sb(st, f"l{z}_one", [128, 1], F32)
                nksp = sb(st, f"l{z}_nksp", [128, 8], F32)
                tmp = [sb(st, f"l{z}_tmp{i}", [128, 8], F32) for i in range(8)]
                wst = sb(st, f"l{z}_wst", [128, 2, 8, 128], F32)
                WA = sb(st, f"l{z}_WA", [128, 8, 128], BF16)
                WI = sb(st, f"l{z}_WI", [128, 8, 128], BF16)
                carry = sb(st, f"l{z}_carry", [128, 8], F32)
                P.dma("sp", lambda e: e.dma_start(out=par[:], in_=lru_par), writes=["l_par"], key="l_par")
                P.op("pool", lambda e: e.memset(one_c[:], 1.0), writes=["l_one"])
                P.op("pool", lambda e: e.memset(carry[:], 0.0), writes=[f"carry{c}" for c in range(8)])
                for (src, dst, nm) in ((lru_wa, WA, "wa"), (lru_wi, WI, "wi")):
                    P.dma("sp", lambda e, src=src: e.dma_start(out=wst[:], in_=src.rearrange("z n c j -> c z n j")), writes=["l_wst"], key="l_wst")
                    P.op("dve", lambda e, dst=dst: e.tensor_copy(out=dst[:], in_=wst[:, z, :, :]), reads=["l_wst"], writes=["l_" + nm])
                lam = par[:, :, 9 + z]
                t_abs, t_e, t_u, t_ln, t_ser, t_msk, t_mx, t_q = [t[:] for t in tmp]
                P.op("dve", lambda e: e.tensor_scalar(out=t_abs, in0=lam, scalar1=-1.0, scalar2=None, op0=ALU.mult), reads=["l_par"], writes=["lt_abs"])
                P.op("dve", lambda e: e.tensor_tensor(out=t_abs, in0=t_abs, in1=lam, op=ALU.max), reads=["l_par", "lt_abs"], writes=["lt_abs"])
                P.op("act", lambda e: e.activation(out=t_e, in_=t_abs, func=AF.Exp, scale=-1.0), reads=["lt_abs"], writes=["lt_e"])
                P.op("dve", lambda e: e.tensor_scalar(out=t_u, in0=t_e, scalar1=1.0, scalar2=None, op0=ALU.add), reads=["lt_e"], writes=["lt_u"])
                P.op("act", lambda e: e.activation(out=t_ln, in_=t_u, func=AF.Ln), reads=["lt_u"], writes=["lt_ln"])
                P.op("dve", lambda e: e.tensor_scalar(out=t_ser, in0=t_e, scalar1=-0.2, scalar2=0.25, op0=ALU.mult, op1=ALU.add), reads=["lt_e"], writes=["lt_ser"])
                for cst in (1.0 / 3.0, 0.5, 1.0):
                    P.op("dve", lambda e: e.tensor_tensor(out=t_ser, in0=t_ser, in1=t_e, op=ALU.mult), reads=["lt_ser", "lt_e"], writes=["lt_ser"])
                    P.op("dve", lambda e, cst=cst: e.tensor_scalar(out=t_ser, in0=t_ser, scalar1=-1.0, scalar2=cst, op0=ALU.mult, op1=ALU.add), reads=["lt_ser"], writes=["lt_ser"])
                P.op("dve", lambda e: e.tensor_tensor(out=t_ser, in0=t_ser, in1=t_e, op=ALU.mult), reads=["lt_ser", "lt_e"], writes=["lt_ser"])
                P.op("dve", lambda e: e.tensor_scalar(out=t_msk, in0=t_e, scalar1=0.1, scalar2=None, op0=ALU.is_lt), reads=["lt_e"], writes=["lt_msk"])
                P.op("dve", lambda e: e.tensor_tensor(out=t_q, in0=t_ser, in1=t_ln, op=ALU.subtract), reads=["lt_ser", "lt_ln"], writes=["lt_q"])
                P.op("dve", lambda e: e.tensor_tensor(out=t_q, in0=t_q, in1=t_msk, op=ALU.mult), reads=["lt_q", "lt_msk"], writes=["lt_q"])
                P.op("dve", lambda e: e.tensor_tensor(out=t_q, in0=t_q, in1=t_ln, op=ALU.add), reads=["lt_q", "lt_ln"], writes=["lt_q"])
                P.op("dve", lambda e: e.tensor_scalar(out=t_mx, in0=lam, scalar1=-1.0, scalar2=0.0, op0=ALU.mult, op1=ALU.max), reads=["l_par"], writes=["lt_mx"])
                P.op("dve", lambda e: e.tensor_tensor(out=t_q, in0=t_q, in1=t_mx, op=ALU.add), reads=["lt_q", "lt_mx"], writes=["lt_q"])
                P.op("dve", lambda e: e.tensor_scalar(out=nksp[:], in0=t_q, scalar1=-8.0, scalar2=None, op0=ALU.mult), reads=["lt_q"], writes=["l_nksp"])

                def ring(name, shape, dt, n):
                    return Ring([(sb(st, f"l{z}_{name}{i}", shape, dt), f"l_{name}{i}") for i in range(n)])
                xr_r = ring("xr", [128, 515], F32, 4)
                vm_r = ring("vm", [128, 512], F32, 3)
                xc_r = ring("xc", [128, 512], F32, 3)
                xcm_r = ring("xcm", [128, 512], F32, 4)
                xcb_r = ring("xcb", [128, 512], BF16, 3)
                r_r = ring("r", [128, 512], F32, 3)
                i_r = ring("i", [128, 512], F32, 4)
                a_r = ring("a", [128, 512], F32, 4)
                s_r = ring("s", [128, 512], F32, 4)
                u_r = ring("u", [128, 512], F32, 3)
                h_r = ring("h", [128, 512], F32, 3)
                hb_r2 = ring("hbt", [128, 512], F32, 4)
                yg_r = ring("ygt", [128, 512], F32, 4)
                rec_r = ring("rec", [128, 512], BF16, 3)
                pg_r = Ring([(ps(st, f"l{z}_pg{i}", [128, 512], F32), f"l_pg{i}") for i in range(6)])
                order = tiles if z == 0 else list(reversed(tiles[1:]))
                litems = [(s0, w, c) for (s0, w) in order for c in range(8)]
                lst_ = {}
                vmst = {}

                def l_load(i):
                    if i >= len(litems):
                        return
                    s0, w, c = litems[i]
                    if c == 0:
                        vm, vmk = vm_r.next()
                        P.dma("sp", lambda e, vm=vm, s0=s0, w=w: e.dma_start(out=vm[:, 0:w], in_=vmask_fm[:, s0:s0 + w]), writes=[vmk], key=vmk)
                        vmst[s0] = (vm, vmk)
                    xr_t, xrk = xr_r.next()
                    P.dma("sp", lambda e, xr_t=xr_t, c=c, s0=s0, w=w: e.dma_start(out=xr_t[:, 0:w + 3], in_=XR[c, :, s0:s0 + w + 3]), writes=[xrk], key=xrk)
                    d_ = {"xr": (xr_t, xrk)}
                    if z == 0 and s0 > 0:
                        hbt, hbk2 = hb_r2.next()
                        ygt, ygk = yg_r.next()
                        P.dma("sp", lambda e, hbt=hbt, c=c, s0=s0, w=w: e.dma_start(out=hbt[:, 0:w], in_=HB[c, :, s0:s0 + w]), writes=[hbk2], key=hbk2)
                        P.dma("sp", lambda e, ygt=ygt, c=c, s0=s0, w=w: e.dma_start(out=ygt[:, 0:w], in_=YG[c, :, s0:s0 + w]), writes=[ygk], key=ygk)
                        d_["hb"] = (hbt, hbk2)
                        d_["yg"] = (ygt, ygk)
                    lst_[i] = d_

                def l_stageA(i):
                    if i >= len(litems):
                        return
                    s0, w, c = litems[i]
                    d_ = lst_[i]
                    xr_t, xrk = d_["xr"]
                    vm, vmk = vmst[s0]
                    xc, xck = xc_r.next()
                    P.op("pool", lambda e, xc=xc, xr_t=xr_t, c=c, w=w: e.tensor_scalar(out=xc[:, 0:w], in0=xr_t[:, 0:w], scalar1=par[:, c, 0:1], scalar2=par[:, c, 4:5], op0=ALU.mult, op1=ALU.add),
                         reads=[xrk, "l_par"], writes=[xck])
                    for j in (1, 2, 3):
                        P.op("dve", lambda e, xc=xc, xr_t=xr_t, c=c, w=w, j=j: e.scalar_tensor_tensor(out=xc[:, 0:w], in0=xr_t[:, j:j + w], scalar=par[:, c, j:j + 1], in1=xc[:, 0:w], op0=ALU.mult, op1=ALU.add),
                             reads=[xrk, xck, "l_par"], writes=[xck])
                    xcm, xcmk = xcm_r.next()
                    P.op("pool", lambda e, xcm=xcm, xc=xc, vm=vm, w=w: e.tensor_tensor(out=xcm[:, 0:w], in0=xc[:, 0:w], in1=vm[:, 0:w], op=ALU.mult), reads=[xck, vmk], writes=[xcmk])
                    xcb, xcbk = xcb_r.next()
                    P.op("act", lambda e, xcb=xcb, xc=xc, w=w: e.copy(out=xcb[:, 0:w], in_=xc[:, 0:w]), reads=[xck], writes=[xcbk])
                    pga, pgak = pg_r.next()
                    pgi, pgik = pg_r.next()
                    P.op("pe", lambda e, pga=pga, xcb=xcb, c=c, w=w: e.matmul(pga[:, 0:w], lhsT=WA[:, c, :], rhs=xcb[:, 0:w], start=True, stop=True), reads=[xcbk, "l_wa"], writes=[pgak])
                    P.op("pe", lambda e, pgi=pgi, xcb=xcb, c=c, w=w: e.matmul(pgi[:, 0:w], lhsT=WI[:, c, :], rhs=xcb[:, 0:w], start=True, stop=True), reads=[xcbk, "l_wi"], writes=[pgik])
                    rt_, rk = r_r.next()
                    it_, ik = i_r.next()
                    P.op("act", lambda e, rt_=rt_, pga=pga, c=c, w=w: e.activation(out=rt_[:, 0:w], in_=pga[:, 0:w], func=AF.Sigmoid, bias=par[:, c, 5 + z:6 + z], scale=1.0), reads=[pgak, "l_par"], writes=[rk])
                    P.op("act", lambda e, it_=it_, pgi=pgi, c=c, w=w: e.activation(out=it_[:, 0:w], in_=pgi[:, 0:w], func=AF.Sigmoid, bias=par[:, c, 7 + z:8 + z], scale=1.0), reads=[pgik, "l_par"], writes=[ik])
                    at_, ak = a_r.next()
                    P.op("act", lambda e, at_=at_, rt_=rt_, c=c, w=w: e.activation(out=at_[:, 0:w], in_=rt_[:, 0:w], func=AF.Exp, scale=nksp[:, c:c + 1]), reads=[rk, "l_nksp"], writes=[ak])
                    st_, sk = s_r.next()
                    P.op("pool", lambda e, st_=st_, at_=at_, w=w: e.tensor_tensor(out=st_[:, 0:w], in0=at_[:, 0:w], in1=at_[:, 0:w], op=ALU.mult), reads=[ak], writes=[sk])
                    P.op("act", lambda e, st_=st_, w=w: e.activation(out=st_[:, 0:w], in_=st_[:, 0:w], func=AF.Sqrt, scale=-1.0, bias=one_c[:, 0:1]), reads=[sk, "l_one"], writes=[sk])
                    d_.update({"xcm": (xcm, xcmk), "i": (it_, ik), "a": (at_, ak), "s": (st_, sk)})

                def l_stageB(i):
                    s0, w, c = litems[i]
                    d_ = lst_.pop(i)
                    xcm, xcmk = d_["xcm"]
                    it_, ik = d_["i"]
                    at_, ak = d_["a"]
                    st_, sk = d_["s"]
                    ut_, uk = u_r.next()
                    P.op("dve", lambda e, ut_=ut_, it_=it_, xcm=xcm, w=w: e.tensor_tensor(out=ut_[:, 0:w], in0=it_[:, 0:w], in1=xcm[:, 0:w], op=ALU.mult), reads=[ik, xcmk], writes=[uk])
                    P.op("dve", lambda e, ut_=ut_, st_=st_, w=w: e.tensor_tensor(out=ut_[:, 0:w], in0=ut_[:, 0:w], in1=st_[:, 0:w], op=ALU.mult), reads=[uk, sk], writes=[uk])
                    ht_, hk = h_r.next()
                    if z == 0:
                        P.op("dve", lambda e, ht_=ht_, at_=at_, ut_=ut_, c=c, w=w: e.tensor_tensor_scan(out=ht_[:, 0:w], data0=at_[:, 0:w], data1=ut_[:, 0:w], initial=carry[:, c:c + 1], op0=ALU.mult, op1=ALU.add),
                             reads=[ak, uk, f"carry{c}"], writes=[hk])
                        P.op("act", lambda e, ht_=ht_, c=c, w=w: e.copy(out=carry[:, c:c + 1], in_=ht_[:, w - 1:w]), reads=[hk], writes=[f"carry{c}"])
                    else:
                        P.op("dve", lambda e, ht_=ht_, at_=at_, ut_=ut_, c=c, w=w: e.tensor_tensor_scan(out=ht_[:, 0:w][:, ::-1], data0=at_[:, 0:w][:, ::-1], data1=ut_[:, 0:w][:, ::-1], initial=carry[:, c:c + 1], op0=ALU.mult, op1=ALU.add),
                             reads=[ak, uk, f"carry{c}"], writes=[hk])
                        P.op("act", lambda e, ht_=ht_, c=c: e.copy(out=carry[:, c:c + 1], in_=ht_[:, 0:1]), reads=[hk], writes=[f"carry{c}"])
                    if z == 1:
                        P.dma("sp", lambda e, ht_=ht_, c=c, s0=s0, w=w: e.dma_start(out=HB[c, :, s0:s0 + w], in_=ht_[:, 0:w]), reads=[hk], writes=["HB"], key=hk + "s")
                    elif s0 > 0:
                        hbt, hbk2 = d_["hb"]
                        ygt, ygk = d_["yg"]
                        P.op("pool", lambda e, hbt=hbt, ht_=ht_, w=w: e.tensor_tensor(out=hbt[:, 0:w], in0=hbt[:, 0:w], in1=ht_[:, 0:w], op=ALU.add), reads=[hbk2, hk], writes=[hbk2])
                        rc, rck = rec_r.next()
                        P.op("dve", lambda e, rc=rc, hbt=hbt, ygt=ygt, w=w: e.tensor_tensor(out=rc[:, 0:w], in0=hbt[:, 0:w], in1=ygt[:, 0:w], op=ALU.mult), reads=[hbk2, ygk], writes=[rck])
                        P.dma("sp", lambda e, rc=rc, c=c, s0=s0, w=w: e.dma_start(out=MIXT[8 + c, :, s0:s0 + w], in_=rc[:, 0:w]), reads=[rck], writes=["MIXT"], key=rck + "s")

                l_load(0)
                l_load(1)
                l_load(2)
                l_stageA(0)
                for i in range(len(litems)):
                    l_load(i + 3)
                    l_stageA(i + 1)
                    l_stageB(i)
                P.barrier(bar_marker)
                P.flush()
        if stop_after <= 2:
            P.flush(final=True)
            return nc

        with ExitStack() as st:
            rb_bc = sb(st, "a_rb", [128, 256], F32)
            sk_bc = sb(st, "a_sk", [128, 8], F32)
            bk_sb = sb(st, "a_bk", [128, 4, 128], F32)
            biasT = sb(st, "a_biasT", [128, 5, 8, 128], F32)
            oh = sb(st, "a_oh", [128, 128], F32)
            sinkt = sb(st, "a_sinkt", [128, 8, 128], F32)
            ones_b = sb(st, "a_ones", [128, 128], BF16)
            kmeta = sb(st, "a_kmeta", [128, 2, 128], BF16)
            vmeta = sb(st, "a_vmeta", [128, 256], BF16)
            P.dma("sp", lambda e: e.dma_start(out=rb_bc[:], in_=rel_bias.rearrange("a b -> (a b)").partition_broadcast(128)), writes=["a_rb"], key="a_c")
            P.dma("sp", lambda e: e.dma_start(out=sk_bc[:], in_=attn_sink.partition_broadcast(128)), writes=["a_sk"], key="a_c")
            P.dma("sp", lambda e: e.dma_start(out=bk_sb[:], in_=bk_tab.rearrange("v p n -> p v n")), writes=["a_bk"], key="a_c")
            for h in range(8):
                P.dma("sp", lambda e, h=h: e.dma_start(out=biasT[:, 0:4, h, :], in_=mk_tab.rearrange("v p n -> p v n")), writes=["a_biasT"], key="a_c")
            P.dma("sp", lambda e: e.dma_start(out=kmeta[:], in_=KT[:, :, 0:128].rearrange("g p n -> p g n")), writes=["a_kmeta"], key="a_c")
            P.dma("sp", lambda e: e.dma_start(out=vmeta[:], in_=VV[0:128, :]), writes=["a_vmeta"], key="a_c")
            P.op("pool", lambda e: e.memset(ones_b[:], 1.0), writes=["a_ones"])
            P.op("act", lambda e: e.activation(out=sk_bc[:], in_=sk_bc[:], func=AF.Exp), reads=["a_sk"], writes=["a_sk"])
            P.op("pool", lambda e: e.memset(sinkt[:], 0.0), writes=["a_sinkt"])
            P.op("pool", lambda e: e.memset(biasT[:, 4, :, :], 0.0), reads=["a_biasT"], writes=["a_biasT"])
            for h in range(8):
                P.op("dve", lambda e, h=h: e.tensor_scalar(out=sinkt[:, h, :], in0=sinkt[:, h, :], scalar1=sk_bc[:, h:h + 1], scalar2=None, op0=ALU.add), reads=["a_sinkt", "a_sk"], writes=["a_sinkt"])
                P.op("dve", lambda e, h=h: e.tensor_scalar(out=biasT[:, 4, h, :], in0=biasT[:, 4, h, :], scalar1=rb_bc[:, 15 * 8 + h:15 * 8 + h + 1], scalar2=None, op0=ALU.add), reads=["a_biasT", "a_rb"], writes=["a_biasT"])
            bk_np, mk_np = static_tables()
            for var in range(4):
                present = sorted(set(int(v) for v in np.unique(bk_np[var][mk_np[var] == 0.0])))
                for bkt in present:
                    P.op("dve", lambda e, var=var, bkt=bkt: e.tensor_scalar(out=oh[:], in0=bk_sb[:, var, :], scalar1=float(bkt), scalar2=None, op0=ALU.is_equal), reads=["a_bk", "a_oh"], writes=["a_oh"])
                    for h in range(8):
                        P.op("dve", lambda e, var=var, bkt=bkt, h=h: e.scalar_tensor_tensor(out=biasT[:, var, h, :], in0=oh[:], scalar=rb_bc[:, bkt * 8 + h:bkt * 8 + h + 1], in1=biasT[:, var, h, :], op0=ALU.mult, op1=ALU.add),
                             reads=["a_oh", "a_rb", "a_biasT"], writes=["a_biasT"])
            q_r = Ring([(sb(st, f"a_q{i}", [128, 8, 512], BF16), f"a_q{i}") for i in range(2)])
            kw_r = Ring([(sb(st, f"a_kw{i}", [128, 2, 768], BF16), f"a_kw{i}") for i in range(2)])
            vw_r = Ring([(sb(st, f"a_vw{i}", [128, 6, 256], BF16), f"a_vw{i}") for i in range(2)])
            ao_r = Ring([(sb(st, f"a_ao{i}", [128, 8, 512], BF16), f"a_ao{i}") for i in range(2)])
            ss_r = Ring([(sb(st, f"a_ss{i}", [128, 512], F32), f"a_ss{i}") for i in range(3)])
            pt_r = Ring([(sb(st, f"a_pt{i}", [128, 512], BF16), f"a_pt{i}") for i in range(8)])
            dn_r = Ring([(sb(st, f"a_dn{i}", [128, 512], F32), f"a_dn{i}") for i in range(2)])
            psS = Ring([(ps(st, f"a_pS{i}", [128, 512], F32), f"a_pS{i}") for i in range(3)])
            psO = Ring([(ps(st, f"a_pO{i}", [128, 512], F32), f"a_pO{i}") for i in range(2)])
            psD = Ring([(ps(st, f"a_pD{i}", [128, 512], F32), f"a_pD{i}") for i in range(2)])
            for (s0, w) in tiles[1:]:
                qt, qk = q_r.next()
                kw, kwk = kw_r.next()
                vw, vwk = vw_r.next()
                ao, aok = ao_r.next()
                hi = min(T, s0 + w + 128)
                ncol = hi - (s0 - 128)
                P.dma("sp", lambda e, qt=qt, s0=s0, w=w: e.dma_start(out=qt[:], in_=QT[:, :, s0:s0 + w].rearrange("c p n -> p c n")), writes=[qk], key=qk)
                P.dma("sp", lambda e, kw=kw, s0=s0, ncol=ncol: e.dma_start(out=kw[:, :, 0:ncol], in_=KT[:, :, s0 - 128:s0 - 128 + ncol].rearrange("g p n -> p g n")), writes=[kwk], key=kwk)
                P.dma("sp", lambda e, vw=vw, s0=s0, ncol=ncol: e.dma_start(out=vw[:, 0:ncol // 128, :], in_=VV[s0 - 128:s0 - 128 + ncol, :].rearrange("(b p) n -> p b n", p=128)), writes=[vwk], key=vwk)
                for bi in range(4):
                    n = s0 // 128 + bi
                    for g in range(2):
                        kbs = [("meta", None, 3 if n == 1 else 4, 0)]
                        for j in (-1, 0, 1):
                            kb = n + j
                            if 1 <= kb <= 64:
                                kbs.append(("band", bi + 1 + j, j + 1, kb))
                        pts = []
                        for (kind, wi_, var, kbcol) in kbs:
                            pS, pSk = psS.next()
                            if kind == "meta":
                                kap = kmeta[:, g, :]
                                kkeys = ["a_kmeta"]
                            else:
                                kap = kw[:, g, wi_ * 128:(wi_ + 1) * 128]
                                kkeys = [kwk]
                            P.op("pe", lambda e, pS=pS, kap=kap, qt=qt, g=g, bi=bi: e.matmul(pS[:], lhsT=kap, rhs=qt[:, 4 * g:4 * g + 4, bi * 128:(bi + 1) * 128], start=True, stop=True),
                                 reads=kkeys + [qk], writes=[pSk])
                            ss, ssk = ss_r.next()
                            P.op("dve", lambda e, ss=ss, pS=pS, var=var, g=g: e.tensor_tensor(out=ss[:].rearrange("p (h n) -> p h n", h=4), in0=pS[:].rearrange("p (h n) -> p h n", h=4), in1=biasT[:, var, 4 * g:4 * g + 4, :], op=ALU.add),
                                 reads=[pSk, "a_biasT"], writes=[ssk])
                            pt, ptk = pt_r.next()
                            P.op("act", lambda e, pt=pt, ss=ss, kbcol=kbcol: e.activation(out=pt[:], in_=ss[:], func=AF.Exp, bias=kbias_sb[:, kbcol:kbcol + 1], scale=1.0), reads=[ssk, "kbias"], writes=[ptk])
                            if kind == "meta":
                                vap = vmeta[:, g * 128:(g + 1) * 128]
                                vkeys = ["a_vmeta"]
                            else:
                                vap = vw[:, wi_, g * 128:(g + 1) * 128]
                                vkeys = [vwk]
                            pts.append((pt, ptk, vap, vkeys))
                        pO, pOk = psO.next()
                        pD, pDk = psD.next()

                        def omm(e, pO=pO, pts=pts):
                            ins = None
                            for ii, (pt, ptk, vap, vkeys) in enumerate(pts):
                                ins = e.matmul(pO[:], lhsT=vap, rhs=pt[:], start=(ii == 0), stop=(ii == len(pts) - 1))
                            return ins

                        def dmm(e, pD=pD, pts=pts):
                            ins = None
                            for ii, (pt, ptk, vap, vkeys) in enumerate(pts):
                                ins = e.matmul(pD[:], lhsT=ones_b[:], rhs=pt[:], start=(ii == 0), stop=(ii == len(pts) - 1))
                            return ins
                        allk = [x[1] for x in pts] + [k for x in pts for k in x[3]]
                        P.op("pe", omm, reads=allk, writes=[pOk])
                        P.op("pe", dmm, reads=allk + ["a_ones"], writes=[pDk])
                        dn, dnk = dn_r.next()
                        P.op("dve", lambda e, dn=dn, pD=pD, g=g: e.tensor_tensor(out=dn[:].rearrange("p (h n) -> p h n", h=4), in0=pD[:].rearrange("p (h n) -> p h n", h=4), in1=sinkt[:, 4 * g:4 * g + 4, :], op=ALU.add),
                             reads=[pDk, "a_sinkt"], writes=[dnk])
                        P.op("dve", lambda e, dn=dn: e.reciprocal(out=dn[:], in_=dn[:]), reads=[dnk], writes=[dnk])
                        P.op("dve", lambda e, ao=ao, pO=pO, dn=dn, g=g, bi=bi: e.tensor_tensor(out=ao[:, 4 * g:4 * g + 4, bi * 128:(bi + 1) * 128], in0=pO[:].rearrange("p (h n) -> p h n", h=4), in1=dn[:].rearrange("p (h n) -> p h n", h=4), op=ALU.mult),
                             reads=[pOk, dnk], writes=[aok + f"_{bi}{g}"])
                P.dma("sp", lambda e, ao=ao, s0=s0, w=w: e.dma_start(out=MIXT[0:8, :, s0:s0 + w].rearrange("c p n -> p c n"), in_=ao[:]),
                      reads=[aok + f"_{bi}{g}" for bi in range(4) for g in range(2)], writes=["MIXT"], key=aok + "s")
            P.barrier(bar_marker)
            P.flush()
        if stop_after <= 3:
            P.flush(final=True)
            return nc

        with ExitStack() as st:
            wo = sb(st, "d_wo", [128, 16, 2048], BF16)
            wst_r = Ring([(sb(st, f"d_wst{i}", [128, 16, 128], F32), f"d_wst{i}") for i in range(1)])
            for j in range(16):
                wstt, wk = wst_r.next()
                P.dma("sp", lambda e, wstt=wstt, j=j: e.dma_start(out=wstt[:], in_=w_out[:, j * 128:(j + 1) * 128].rearrange("(c p) n -> p c n", p=128)), writes=[wk], key=wk)
                if j % 2 == 0:
                    P.op("dve", lambda e, wstt=wstt, j=j: e.tensor_copy(out=wo[:, :, j * 128:(j + 1) * 128], in_=wstt[:]), reads=[wk], writes=[f"d_wo{j}"])
                else:
                    P.op("act", lambda e, wstt=wstt, j=j: e.copy(out=wo[:, :, j * 128:(j + 1) * 128], in_=wstt[:]), reads=[wk], writes=[f"d_wo{j}"])
            wo_keys = [f"d_wo{j}" for j in range(16)]
            G1 = sb(st, "d_G1", [128, D], F32)
            B1 = sb(st, "d_B1", [128, D], F32)
            P.dma("sp", lambda e: e.dma_start(out=G1[:], in_=ln1_g.partition_broadcast(128)), writes=["d_GB"], key="d_c")
            P.dma("sp", lambda e: e.dma_start(out=B1[:], in_=ln1_b.partition_broadcast(128)), writes=["d_GB"], key="d_c")
            rw = sb(st, "d_rw", [128, 16, NE], F32)
            rbb = sb(st, "d_rbb", [128, NE], F32)
            UT = sb(st, "d_UT", [128, 128], F32)
            ones_f = sb(st, "d_ones", [128, 128], F32)
            base = sb(st, "d_base", [128, NE], F32)
            elim = sb(st, "d_elim", [128, NE], F32)
            P.dma("sp", lambda e: e.dma_start(out=rw[:], in_=router_w.rearrange("(c p) n -> p c n", p=128)), writes=["d_rw"], key="d_c")
            P.dma("sp", lambda e: e.dma_start(out=rbb[:], in_=router_b.partition_broadcast(128)), writes=["d_rbb"], key="d_c")
            P.dma("sp", lambda e: e.dma_start(out=UT[:], in_=ut_tab), writes=["d_UT"], key="d_c")
            P.dma("sp", lambda e: e.dma_start(out=base[:], in_=ecoff_tab), writes=["d_base"], key="d_c")
            P.dma("sp", lambda e: e.dma_start(out=elim[:], in_=elim_tab), writes=["d_elim"], key="d_c")
            P.op("pool", lambda e: e.memset(ones_f[:], 1.0), writes=["d_ones"])
            mx_r = Ring([(sb(st, f"d_mx{i}", [128, 16, 512], BF16), f"d_mx{i}") for i in range(2)])
            h0_r = Ring([(sb(st, f"d_h0{i}", [128, D], F32), f"d_h0{i}") for i in range(1)])
            r1_r = Ring([(sb(st, f"d_r1{i}", [128, D], F32), f"d_r1{i}") for i in range(1)])
            h1_r = Ring([(sb(st, f"d_h1{i}", [128, D], F32), f"d_h1{i}") for i in range(2)])
            h1b_r = Ring([(sb(st, f"d_h1b{i}", [128, D], BF16), f"d_h1b{i}") for i in range(2)])
            h1T_r = Ring([(sb(st, f"d_h1T{i}", [128, 16, 128], F32), f"d_h1T{i}") for i in range(1)])
            lnscr = (sb(st, "d_stats", [128, 4, 6], F32), sb(st, "d_mv", [128, 2], F32), sb(st, "d_sd", [128, 1], F32),
                     sb(st, "d_rs", [128, 1], F32), sb(st, "d_nmr", [128, 1], F32))
            lg = sb(st, "d_lg", [128, NE], F32)
            m8 = sb(st, "d_m8", [128, 8], F32)
            sel = sb(st, "d_sel", [128, NE], F32)
            dd = sb(st, "d_dd", [128, NE], F32)
            junk = sb(st, "d_junk", [128, NE], F32)
            nv0 = sb(st, "d_nv0", [128, 1], F32)
            ex4 = sb(st, "d_ex4", [128, 4], F32)
            den1 = sb(st, "d_den1", [128, 1], F32)
            dk = sb(st, "d_dk", [128, 4], F32)
            lk = sb(st, "d_lk", [128, 4], F32)
            ov = sb(st, "d_ov", [128, 4], F32)
            nov = sb(st, "d_nov", [128, 4], F32)
            pw_r = Ring([(ps(st, f"d_pw{i}", [128, 512], F32), f"d_pw{i}") for i in range(4)])
            pt_r = Ring([(ps(st, f"d_pt{i}", [128, 512], F32), f"d_pt{i}") for i in range(2)])
            pl = ps(st, "d_pl", [128, 512], F32)
            pc = ps(st, "d_pc", [128, 512], F32)
            for (s0, w) in tiles[1:]:
                mx, mxk = mx_r.next()
                P.dma("sp", lambda e, mx=mx, s0=s0, w=w: e.dma_start(out=mx[:], in_=MIXT[:, :, s0:s0 + w].rearrange("c p n -> p c n")), writes=[mxk], key=mxk)
                for bi in range(4):
                    n = s0 // 128 + bi
                    h0t, h0k = h0_r.next()
                    P.dma("sp", lambda e, h0t=h0t, n=n: e.dma_start(out=h0t[:], in_=H0[n * 128:(n + 1) * 128, :]), writes=[h0k], key=h0k)
                    r1, r1k = r1_r.next()
                    for ng in range(4):
                        pw, pwk = pw_r.next()

                        def wmm(e, pw=pw, mx=mx, bi=bi, ng=ng):
                            ins = None
                            for c in range(16):
                                ins = e.matmul(pw[:], lhsT=mx[:, c, bi * 128:(bi + 1) * 128], rhs=wo[:, c, ng * 512:(ng + 1) * 512], start=(c == 0), stop=(c == 15))
                            return ins
                        P.op("pe", wmm, reads=[mxk] + wo_keys, writes=[pwk])
                        P.op("dve", lambda e, r1=r1, h0t=h0t, pw=pw, ng=ng: e.scalar_tensor_tensor(out=r1[:, ng * 512:(ng + 1) * 512], in0=h0t[:, ng * 512:(ng + 1) * 512], scalar=ALPHA, in1=pw[:], op0=ALU.mult, op1=ALU.add),
                             reads=[h0k, pwk], writes=[r1k + f"_{ng}"])
                    r1keys = [r1k + f"_{ng}" for ng in range(4)]
                    h1, h1k = h1_r.next()
                    layer_norm_tile(r1keys, r1[:], h1[:], h1k, G1, B1, "d_GB", None, lnscr, "dln")
                    if "DBGM" in dbg:
                        P.dma("sp", lambda e, r1=r1, n=n: e.dma_start(out=DBGM[n * 128:(n + 1) * 128, :], in_=r1[:]), reads=r1keys, key="dbgm")
                    P.dma("sp", lambda e, h1=h1, n=n: e.dma_start(out=H1F[n * 128:(n + 1) * 128, :], in_=h1[:]), reads=[h1k], writes=["H1F"], key=h1k + "s")
                    h1b, h1bk = h1b_r.next()
                    P.op("pool", lambda e, h1b=h1b, h1=h1: e.tensor_copy(out=h1b[:], in_=h1[:]), reads=[h1k], writes=[h1bk])
                    h1T, h1Tk = h1T_r.next()
                    for q4 in range(4):
                        ptp, ptk = pt_r.next()

                        def trf(e, ptp=ptp, h1=h1, q4=q4):
                            ins = None
                            for c4 in range(4):
                                c = q4 * 4 + c4
                                ins = e.transpose(out=ptp[:, c4 * 128:(c4 + 1) * 128], in_=h1[:, c * 128:(c + 1) * 128], identity=ident_f[:])
                            return ins
                        P.op("pe", trf, reads=[h1k, "ident_f"], writes=[ptk])
                        if q4 % 2 == 0:
                            P.op("act", lambda e, ptp=ptp, h1T=h1T, q4=q4: e.copy(out=h1T[:, q4 * 4:(q4 + 1) * 4, :], in_=ptp[:].rearrange("p (c n) -> p c n", c=4)), reads=[ptk], writes=[h1Tk + f"_{q4}"])
                        else:
                            P.op("dve", lambda e, ptp=ptp, h1T=h1T, q4=q4: e.tensor_copy(out=h1T[:, q4 * 4:(q4 + 1) * 4, :], in_=ptp[:].rearrange("p (c n) -> p c n", c=4)), reads=[ptk], writes=[h1Tk + f"_{q4}"])

                    def lmm(e, h1T=h1T):
                        ins = None
                        for c in range(16):
                            ins = e.matmul(pl[:, 0:NE], lhsT=h1T[:, c, :], rhs=rw[:, c, :], start=(c == 0), stop=(c == 15))
                        return ins
                    P.op("pe", lmm, reads=[h1Tk + f"_{q4}" for q4 in range(4)] + ["d_rw"], writes=["d_pl"])
                    P.op("dve", lambda e: e.tensor_tensor(out=lg[:], in0=pl[:, 0:NE], in1=rbb[:], op=ALU.add), reads=["d_pl", "d_rbb"], writes=["d_lg"])
                    P.op("dve", lambda e: e.max(out=m8[:], in_=lg[:]), reads=["d_lg"], writes=["d_m8"])
                    P.op("dve", lambda e, n=n: e.tensor_scalar(out=sel[:], in0=lg[:], scalar1=m8[:, 3:4], scalar2=valid_sb[:, n:n + 1], op0=ALU.is_ge, op1=ALU.mult), reads=["d_lg", "d_m8", "valid"], writes=["d_sel"])
                    P.op("pe", lambda e: e.matmul(pc[:, 0:NE], lhsT=UT[:], rhs=sel[:], start=True, stop=True), reads=["d_sel", "d_UT"], writes=["d_pc0"])
                    P.op("pe", lambda e: e.matmul(pc[:, 64:64 + NE], lhsT=ones_f[:], rhs=sel[:], start=True, stop=True), reads=["d_sel", "d_ones"], writes=["d_pc1"])
                    P.op("dve", lambda e: e.tensor_tensor(out=dd[:], in0=pc[:, 0:NE], in1=base[:], op=ALU.add), reads=["d_pc0", "d_base"], writes=["d_dd"])
                    P.op("dve", lambda e: e.tensor_tensor(out=base[:], in0=pc[:, 64:64 + NE], in1=base[:], op=ALU.add), reads=["d_pc1", "d_base"], writes=["d_base"])
                    P.op("dve", lambda e: e.tensor_scalar(out=nv0[:], in0=m8[:, 0:1], scalar1=-1.0, scalar2=None, op0=ALU.mult), reads=["d_m8"], writes=["d_nv0"])
                    P.op("act", lambda e: e.activation(out=ex4[:], in_=m8[:, 0:4], func=AF.Exp, bias=nv0[:, 0:1], scale=1.0, accum_out=den1[:]), reads=["d_m8", "d_nv0"], writes=["d_ex4", "d_den1"])
                    P.op("dve", lambda e: e.reciprocal(out=den1[:], in_=den1[:]), reads=["d_den1"], writes=["d_den1"])
                    P.op("dve", lambda e: e.tensor_scalar(out=ex4[:], in0=ex4[:], scalar1=den1[:, 0:1], scalar2=None, op0=ALU.mult), reads=["d_ex4", "d_den1"], writes=["d_ex4"])
                    for k in range(4):
                        P.op("dve", lambda e, k=k: e.scalar_tensor_tensor(out=junk[:], in0=lg[:], scalar=m8[:, k:k + 1], in1=dd[:], op0=ALU.is_equal, op1=ALU.mult, accum_out=dk[:, k:k + 1]),
                             reads=["d_lg", "d_m8", "d_dd", "d_junk"], writes=["d_junk", f"d_dk{k}"])
                        P.op("dve", lambda e, k=k: e.scalar_tensor_tensor(out=junk[:], in0=lg[:], scalar=m8[:, k:k + 1], in1=elim[:], op0=ALU.is_equal, op1=ALU.mult, accum_out=lk[:, k:k + 1]),
                             reads=["d_lg", "d_m8", "d_elim", "d_junk"], writes=["d_junk", f"d_lk{k}"])
                    dkk = [f"d_dk{k}" for k in range(4)]
                    lkk = [f"d_lk{k}" for k in range(4)]
                    P.op("dve", lambda e: e.tensor_tensor(out=ov[:], in0=dk[:], in1=lk[:], op=ALU.is_ge), reads=dkk + lkk, writes=["d_ov"])
                    P.op("dve", lambda e: e.tensor_scalar(out=nov[:], in0=ov[:], scalar1=-1.0, scalar2=1.0, op0=ALU.mult, op1=ALU.add), reads=["d_ov"], writes=["d_nov"])
                    P.op("dve", lambda e, n=n: e.tensor_tensor(out=GATE[:, n - 1, :], in0=ex4[:], in1=nov[:], op=ALU.mult), reads=["d_ex4", "d_nov"], writes=[f"GATE{n}"])
                    P.op("dve", lambda e: e.scalar_tensor_tensor(out=ov[:], in0=ov[:], scalar=BIG, in1=dk[:], op0=ALU.mult, op1=ALU.add), reads=["d_ov"] + dkk, writes=["d_ov"])
                    P.op("dve", lambda e, n=n: e.tensor_scalar(out=ov[:], in0=ov[:], scalar1=invbig_sb[:, n:n + 1], scalar2=None, op0=ALU.add), reads=["d_ov", "invbig"], writes=["d_ov"])
                    P.op("dve", lambda e, n=n: e.tensor_copy(out=DEST[:, n - 1, :], in_=ov[:]), reads=["d_ov"], writes=[f"DEST{n}"])
                    if "DBGL" in dbg:
                        P.dma("sp", lambda e, n=n: e.dma_start(out=DBGL[n * 128:(n + 1) * 128, 0:NE], in_=lg[:]), reads=["d_lg"], key="dbgl")
                        P.dma("sp", lambda e, n=n: e.dma_start(out=DBGL[n * 128:(n + 1) * 128, 32:36], in_=ov[:]), reads=["d_ov"], key="dbgl")
                        P.dma("sp", lambda e, n=n: e.dma_start(out=DBGL[n * 128:(n + 1) * 128, 36:40], in_=GATE[:, n - 1, :]), reads=[f"GATE{n}"], key="dbgl")
                    for k in range(4):
                        P.dma("pool", lambda e, h1b=h1b, n=n, k=k: e.indirect_dma_start(out=XE[:, :], out_offset=bass.IndirectOffsetOnAxis(ap=DEST[:, n - 1, k:k + 1], axis=0), in_=h1b[:, :], in_offset=None,
                                                                                   bounds_check=P.bc, oob_is_err=False),
                              reads=[h1bk, f"DEST{n}"], writes=["XE"], key=h1bk + "x")
            P.barrier(bar_marker)
            P.flush()
        if stop_after <= 4:
            P.flush(final=True)
            return nc

        with ExitStack() as st:
            XT = sb(st, "e_XT", [128, 16, CAP], BF16)
            AT = sb(st, "e_AT", [128, 16, CAP], BF16)
            ws_r = Ring([(sb(st, f"e_ws{i}", [128, 16, 256], F32), f"e_ws{i}") for i in range(3)])
            wb_r = Ring([(sb(st, f"e_wb{i}", [128, 16, 256], BF16), f"e_wb{i}") for i in range(3)])
            xr_r = Ring([(sb(st, f"e_xr{i}", [128, D], BF16), f"e_xr{i}") for i in range(2)])
            b1_r = Ring([(sb(st, f"e_b1{i}", [128, 32], F32), f"e_b1{i}") for i in range(2)])
            b2_r = Ring([(sb(st, f"e_b2{i}", [128, D], F32), f"e_b2{i}") for i in range(2)])
            glu_r = Ring([(sb(st, f"e_glu{i}", [128, 512], F32), f"e_glu{i}") for i in range(2)])
            sg_r = Ring([(sb(st, f"e_sg{i}", [128, 512], F32), f"e_sg{i}") for i in range(2)])
            l0_r = Ring([(sb(st, f"e_l0{i}", [128, 512], F32), f"e_l0{i}") for i in range(2)])
            yst_r = Ring([(sb(st, f"e_yst{i}", [128, 256], BF16), f"e_yst{i}") for i in range(3)])
            ptx_r = Ring([(ps(st, f"e_ptx{i}", [128, 1024], BF16), f"e_ptx{i}") for i in range(2)])
            pg_r = Ring([(ps(st, f"e_pg{i}", [128, 512], F32), f"e_pg{i}") for i in range(2)])
            pl_r = Ring([(ps(st, f"e_plin{i}", [128, 512], F32), f"e_plin{i}") for i in range(2)])
            py_r = Ring([(ps(st, f"e_py{i}", [128, 512], F32), f"e_py{i}") for i in range(2)])
            rgs = [(0, 512), (512, 512), (1024, CAP - 1024)] if CAP > 1024 else [(0, 512), (512, CAP - 512)]

            units = []
            for ex in range(NE):
                for j in range(16):
                    units.append((ex, "w1", j))
                for ng in range(8):
                    units.append((ex, "w2", ng))
            ustate = {}
            cast_i = [0]

            def u_load(u):
                if u >= len(units):
                    return
                ex, kind, ix = units[u]
                ws, wsk = ws_r.next()
                if kind == "w1":
                    P.dma("sp", lambda e, ws=ws, ex=ex, ix=ix: e.dma_start(out=ws[:, :, 0:128], in_=exp_w1[ex, :, ix * 128:(ix + 1) * 128].rearrange("(c p) n -> p c n", p=128)), writes=[wsk], key=wsk)
                    P.dma("sp", lambda e, ws=ws, ex=ex, ix=ix: e.dma_start(out=ws[:, :, 128:256], in_=exp_w1[ex, :, D + ix * 128:D + (ix + 1) * 128].rearrange("(c p) n -> p c n", p=128)), writes=[wsk], key=wsk, allow_ww=True)
                else:
                    P.dma("sp", lambda e, ws=ws, ex=ex, ix=ix: e.dma_start(out=ws[:], in_=exp_w2[ex, :, ix * 256:(ix + 1) * 256].rearrange("(c p) n -> p c n", p=128)), writes=[wsk], key=wsk)
                ustate[u] = {"ws": [(ws, wsk)]}

            def u_cast(u):
                if u >= len(units):
                    return
                lst = []
                for (ws, wsk) in ustate[u]["ws"]:
                    wb, wbk = wb_r.next()
                    which = ("dve", "act", "dve", "act", "pool")[cast_i[0] % 5]
                    cast_i[0] += 1
                    if which == "act":
                        P.op("act", lambda e, wb=wb, ws=ws: e.copy(out=wb[:], in_=ws[:]), reads=[wsk], writes=[wbk])
                    else:
                        P.op(which, lambda e, wb=wb, ws=ws: e.tensor_copy(out=wb[:], in_=ws[:]), reads=[wsk], writes=[wbk])
                    lst.append((wb, wbk))
                ustate[u]["wb"] = lst

            bstate = {}

            def b_load(ex):
                if ex >= NE:
                    return
                b1t, b1k = b1_r.next()
                b2t, b2k = b2_r.next()
                P.dma("sp", lambda e, b1t=b1t, ex=ex: e.dma_start(out=b1t[:], in_=exp_b1r[ex]), writes=[b1k], key=b1k)
                P.dma("sp", lambda e, b2t=b2t, ex=ex: e.dma_start(out=b2t[:], in_=exp_b2[ex].partition_broadcast(128)), writes=[b2k], key=b2k)
                bstate[ex] = (b1t, b1k, b2t, b2k)

            xstate = {}

            def x_load(ex, rt):
                if ex >= NE:
                    return
                xrow, xrk = xr_r.next()
                P.dma("sp", lambda e, xrow=xrow, ex=ex, rt=rt: e.dma_start(out=xrow[:], in_=XE[ex * CAP + rt * 128:ex * CAP + (rt + 1) * 128, :]), writes=[xrk], key=xrk)
                xstate[(ex, rt)] = (xrow, xrk)

            def x_trans(ex, rt):
                if ex >= NE:
                    return
                xrow, xrk = xstate.pop((ex, rt))
                for half in range(2):
                    tp, tpk = ptx_r.next()

                    def trx(e, tp=tp, xrow=xrow, half=half):
                        ins = None
                        for c8 in range(8):
                            c = half * 8 + c8
                            ins = e.transpose(out=tp[:, c8 * 128:(c8 + 1) * 128], in_=xrow[:, c * 128:(c + 1) * 128], identity=ident_b[:])
                        return ins
                    P.op("pe", trx, reads=[xrk, "ident_b"], writes=[tpk])
                    if half == 0:
                        P.op("act", lambda e, tp=tp, rt=rt: e.copy(out=XT[:, 0:8, rt * 128:(rt + 1) * 128], in_=tp[:].rearrange("p (c n) -> p c n", c=8)), reads=[tpk], writes=[f"e_XT{rt}a"])
                    else:
                        P.op("dve", lambda e, tp=tp, rt=rt: e.tensor_copy(out=XT[:, 8:16, rt * 128:(rt + 1) * 128], in_=tp[:].rearrange("p (c n) -> p c n", c=8)), reads=[tpk], writes=[f"e_XT{rt}b"])

            xplan = {0: ([0, 1], []), 1: ([2, 3], [0, 1]), 2: ([4, 5], [2, 3]), 3: ([6, 7], [4, 5]), 4: ([8, 9], [6, 7]), 5: ([], [8, 9])}
            if NRT != 10:
                xplan = {}
                rts = list(range(NRT))
                for i in range(0, NRT, 2):
                    xplan.setdefault(i // 2, ([], []))
                    xplan[i // 2] = (rts[i:i + 2], xplan[i // 2][1])
                    xplan.setdefault(i // 2 + 1, ([], []))
                    xplan[i // 2 + 1] = (xplan[i // 2 + 1][0], rts[i:i + 2])

            b_load(0)
            for i in range(0, NRT, 2):
                for rt in range(i, min(i + 2, NRT)):
                    x_load(0, rt)
                for rt in range(i, min(i + 2, NRT)):
                    x_trans(0, rt)
            u_load(0)
            u_load(1)
            u_cast(0)

            at_keys = [f"e_AT{j}_{r0}" for j in range(16) for (r0, _) in rgs]
            for u, (ex, kind, ix) in enumerate(units):
                u_load(u + 2)
                u_cast(u + 1)
                b1t, b1k, b2t, b2k = bstate[ex]
                wbs = ustate[u]["wb"]
                if kind == "w1":
                    (wbg, wbgk), = wbs
                    wbl, wblk = wbg, wbgk
                    for jh in range(1):
                        j = ix
                        for (r0, rw_) in rgs:
                            pg, pgk = pg_r.next()
                            pln, plk = pl_r.next()

                            def m1(e, pg=pg, wb=wbg, jh=jh, r0=r0, rw_=rw_):
                                ins = None
                                for c in range(16):
                                    ins = e.matmul(pg[:, 0:rw_], lhsT=wb[:, c, 0:128], rhs=XT[:, c, r0:r0 + rw_], start=(c == 0), stop=(c == 15))
                                return ins

                            def m1l(e, pg=pln, wb=wbl, jh=jh, r0=r0, rw_=rw_):
                                ins = None
                                for c in range(16):
                                    ins = e.matmul(pg[:, 0:rw_], lhsT=wb[:, c, 128:256], rhs=XT[:, c, r0:r0 + rw_], start=(c == 0), stop=(c == 15))
                                return ins
                            xk_need = [f"e_XT{rt}{x}" for rt in range(r0 // 128, (r0 + rw_) // 128) for x in "ab"]
                            P.op("pe", m1, reads=xk_need + [wbgk], writes=[pgk])
                            P.op("pe", m1l, reads=xk_need + [wblk], writes=[plk])
                            glu, gluk = glu_r.next()
                            sg, sgk = sg_r.next()
                            l0, l0k = l0_r.next()
                            P.op("dve", lambda e, glu=glu, pg=pg, b1t=b1t, j=j, rw_=rw_: e.tensor_scalar(out=glu[:, 0:rw_], in0=pg[:, 0:rw_], scalar1=b1t[:, j:j + 1], scalar2=7.0, op0=ALU.add, op1=ALU.min), reads=[pgk, b1k], writes=[gluk])
                            P.op("act", lambda e, sg=sg, glu=glu, rw_=rw_: e.activation(out=sg[:, 0:rw_], in_=glu[:, 0:rw_], func=AF.Sigmoid, scale=1.702), reads=[gluk], writes=[sgk])
                            P.op("act", lambda e, l0=l0, pln=pln, b1t=b1t, j=j, rw_=rw_: e.activation(out=l0[:, 0:rw_], in_=pln[:, 0:rw_], func=AF.Identity, bias=b1t[:, 16 + j:17 + j], scale=1.0), reads=[plk, b1k], writes=[l0k])
                            P.op("dve", lambda e, l0=l0, rw_=rw_: e.tensor_scalar(out=l0[:, 0:rw_], in0=l0[:, 0:rw_], scalar1=7.0, scalar2=-7.0, op0=ALU.min, op1=ALU.max), reads=[l0k], writes=[l0k])
                            P.op("pool", lambda e, sg=sg, glu=glu, rw_=rw_: e.tensor_tensor(out=sg[:, 0:rw_], in0=sg[:, 0:rw_], in1=glu[:, 0:rw_], op=ALU.mult), reads=[sgk, gluk], writes=[sgk])
                            P.op("dve", lambda e, l0=l0, sg=sg, j=j, r0=r0, rw_=rw_: e.scalar_tensor_tensor(out=AT[:, j, r0:r0 + rw_], in0=l0[:, 0:rw_], scalar=1.0, in1=sg[:, 0:rw_], op0=ALU.add, op1=ALU.mult),
                                 reads=[l0k, sgk], writes=[f"e_AT{j}_{r0}"])
                else:
                    ng = ix
                    (wb2, wb2k), = wbs
                    if ng == 0:
                        b_load(ex + 1)
                    xl, xtr = xplan.get(ng, ([], []))
                    for rt in xtr:
                        x_trans(ex + 1, rt)
                    for rt in xl:
                        x_load(ex + 1, rt)
                    for rt in range(NRT):
                        py, pyk = py_r.next()

                        def m2(e, py=py, wb2=wb2, rt=rt):
                            ins = None
                            for c in range(16):
                                ins = e.matmul(py[:, 0:256], lhsT=AT[:, c, rt * 128:(rt + 1) * 128], rhs=wb2[:, c, :], start=(c == 0), stop=(c == 15))
                            return ins
                        P.op("pe", m2, reads=at_keys + [wb2k], writes=[pyk])
                        yst, ystk = yst_r.next()
                        P.op("dve", lambda e, yst=yst, py=py, b2t=b2t, ng=ng: e.tensor_tensor(out=yst[:], in0=py[:, 0:256], in1=b2t[:, ng * 256:(ng + 1) * 256], op=ALU.add), reads=[pyk, b2k], writes=[ystk])
                        P.dma("sp", lambda e, yst=yst, ex=ex, rt=rt, ng=ng: e.dma_start(out=YE[ex * CAP + rt * 128:ex * CAP + (rt + 1) * 128, ng * 256:(ng + 1) * 256], in_=yst[:]), reads=[ystk], writes=["YE"], key=ystk + "s")
                del ustate[u]
            P.barrier(bar_marker)
            P.flush()
        if stop_after <= 5:
            P.flush(final=True)
            return nc

        with ExitStack() as st:
            G2 = sb(st, "f_G2", [128, D], F32)
            B2 = sb(st, "f_B2", [128, D], F32)
            P.dma("sp", lambda e: e.dma_start(out=G2[:], in_=ln2_g.partition_broadcast(128)), writes=["f_GB"], key="f_c")
            P.dma("sp", lambda e: e.dma_start(out=B2[:], in_=ln2_b.partition_broadcast(128)), writes=["f_GB"], key="f_c")
            yg_r = Ring([(sb(st, f"f_yg{i}", [128, D], BF16), f"f_yg{i}") for i in range(6)])
            h1_r = Ring([(sb(st, f"f_h1{i}", [128, D], F32), f"f_h1{i}") for i in range(2)])
            acc_r = Ring([(sb(st, f"f_acc{i}", [128, D], F32), f"f_acc{i}") for i in range(2)])
            out_r = Ring([(sb(st, f"f_out{i}", [128, D], F32), f"f_out{i}") for i in range(2)])
            lnscr = (sb(st, "f_stats", [128, 4, 6], F32), sb(st, "f_mv", [128, 2], F32), sb(st, "f_sd", [128, 1], F32),
                     sb(st, "f_rs", [128, 1], F32), sb(st, "f_nmr", [128, 1], F32))
            for i in range(6):
                P.op("pool", lambda e, i=i: e.memset(yg_r.items[i][0][:], 0.0), writes=[yg_r.items[i][1]])
            for n in range(1, 65):
                h1, h1k = h1_r.next()
                P.dma("sp", lambda e, h1=h1, n=n: e.dma_start(out=h1[:], in_=H1F[n * 128:(n + 1) * 128, :]), writes=[h1k], key=h1k)
                acc, acck = acc_r.next()
                for k in range(4):
                    yg, ygk = yg_r.next()
                    P.dma("pool", lambda e, yg=yg, n=n, k=k: e.indirect_dma_start(out=yg[:, :], out_offset=None, in_=YE[:, :], in_offset=bass.IndirectOffsetOnAxis(ap=DEST[:, n - 1, k:k + 1], axis=0),
                                                                               bounds_check=P.bc, oob_is_err=False),
                          reads=[f"DEST{n}"], writes=[ygk], key=ygk)
                    if k == 0:
                        P.op("dve", lambda e, acc=acc, h1=h1, yg=yg, n=n: e.tensor_scalar(out=acc[:], in0=yg[:], scalar1=GATE[:, n - 1, 0:1], scalar2=None, op0=ALU.mult), reads=[ygk, f"GATE{n}"], writes=[acck])
                    else:
                        P.op("dve", lambda e, acc=acc, yg=yg, n=n, k=k: e.scalar_tensor_tensor(out=acc[:], in0=yg[:], scalar=GATE[:, n - 1, k:k + 1], in1=acc[:], op0=ALU.mult, op1=ALU.add), reads=[ygk, f"GATE{n}", acck], writes=[acck])
                P.op("dve", lambda e, acc=acc, h1=h1: e.scalar_tensor_tensor(out=acc[:], in0=h1[:], scalar=ALPHA, in1=acc[:], op0=ALU.mult, op1=ALU.add), reads=[h1k, acck], writes=[acck])
                ot, otk = out_r.next()
                layer_norm_tile(acck, acc[:], ot[:], otk, G2, B2, "f_GB", None, lnscr, "fln")
                P.dma("sp", lambda e, ot=ot, n=n: e.dma_start(out=y_out[(n - 1) * 128:n * 128, :], in_=ot[:]), reads=[otk], writes=["y_out"], key=otk + "s")
            P.barrier(bar_marker)
            P.flush()
        P.flush(final=True)
    return nc


def host_prep(x_seq, L, meta_tokens):
    xs = np.zeros((T, D), np.float32)
    xs[112:128] = meta_tokens
    xs[128:128 + L] = x_seq
    valid = np.zeros(T, np.float32)
    valid[112:128 + L] = 1.0
    kb = np.full(T, NEG, np.float32)
    kb[112:128 + L] = 0.0
    tm = lambda a: np.ascontiguousarray(a.reshape(NB, 128).T)
    return {
        "xs": xs,
        "valid_tm": tm(valid),
        "invbig_tm": tm((1.0 - valid) * BIG),
        "kbias_tm": tm(kb),
        "vmask_fm": np.ascontiguousarray(np.broadcast_to(valid[None, :], (128, T))),
    }


def common_inputs(inp):
    bk, mk = static_tables()
    ut = (np.arange(128)[:, None] <= np.arange(128)[None, :]).astype(np.float32)
    ecoff = np.broadcast_to((np.arange(NE) * CAP - 1).astype(np.float32)[None, :], (128, NE))
    elim = np.broadcast_to(((np.arange(NE) + 1) * CAP).astype(np.float32)[None, :], (128, NE))
    c = {
        "bk_tab": bk, "mk_tab": mk, "ut_tab": ut,
        "ecoff_tab": np.ascontiguousarray(ecoff), "elim_tab": np.ascontiguousarray(elim),
        "ident_tab": np.eye(128, dtype=np.float32),
    }
    f = lambda a: np.ascontiguousarray(np.asarray(a, np.float32))
    c["ln_in_g"] = f(inp["ln_in_g"]); c["ln_in_b"] = f(inp["ln_in_b"])
    c["rel_bias"] = f(inp["rel_bias"])
    c["w_in"] = f(inp["w_in"][0])
    rows = [inp["conv_w"][0][j] for j in range(4)] + [inp["conv_b"][0], inp["lru_ba"][0][0], inp["lru_ba"][0][1],
                                                    inp["lru_bi"][0][0], inp["lru_bi"][0][1], inp["lru_lam"][0][0], inp["lru_lam"][0][1]]
    par = np.stack([np.asarray(r, np.float32) for r in rows], axis=0)
    c["lru_par"] = np.ascontiguousarray(par.reshape(11, 8, 128).transpose(2, 1, 0))
    c["lru_wa"] = f(inp["lru_wa"][0])
    c["lru_wi"] = f(inp["lru_wi"][0])
    c["attn_sink"] = f(inp["attn_sink"][0])
    c["w_out"] = f(inp["w_out"][0])
    c["ln1_g"] = f(inp["ln1_g"][0]); c["ln1_b"] = f(inp["ln1_b"][0])
    c["router_w"] = f(inp["router_w"][0]); c["router_b"] = f(inp["router_b"][0])
    c["exp_w1"] = f(inp["exp_w1"][0]); c["exp_b1r"] = np.ascontiguousarray(f(inp["exp_b1"][0]).reshape(NE, 32, 128).transpose(0, 2, 1))
    c["exp_w2"] = f(inp["exp_w2"][0]); c["exp_b2"] = f(inp["exp_b2"][0])
    c["ln2_g"] = f(inp["ln2_g"][0]); c["ln2_b"] = f(inp["ln2_b"][0])
    return c


def kernel(**inp):
    xp = np.asarray(inp["x_prompt"], np.float32)
    xsm = np.asarray(inp["x_sample"], np.float32)
    meta = np.asarray(inp["meta_tokens"], np.float32)
    common = common_inputs(inp)
    seqs = [(xsm[i], 8192) for i in range(4)] + [(xp[i], 4096) for i in range(2)] + [(xp[0], 4096), (xp[1], 4096)]
    in_maps = []
    for (xq, L) in seqs:
        m = dict(common)
        m.update(host_prep(xq, L, meta))
        in_maps.append(m)
    nc = build()
    res = run_bass_kernel_spmd(nc, in_maps, core_ids=list(range(8)))
    ys = [r["y_out"] for r in res.results]
    y_sample = np.stack([ys[i][:8192] for i in range(4)], axis=0).astype(np.float32)
    y_prompt = np.stack([ys[4 + i][:4096] for i in range(2)], axis=0).astype(np.float32)
    return (y_prompt, y_sample)
```

```python
import re
import numpy as np
import concourse.bass as bass
import concourse.mybir as mybir
from concourse.bass_utils import run_bass_kernel_spmd
from contextlib import ExitStack

F32 = mybir.dt.float32
BF16 = mybir.dt.bfloat16
I32 = mybir.dt.int32
AF = mybir.ActivationFunctionType
ALU = mybir.AluOpType

D = 2048
NCH = 16
NB = 65
T = NB * 128
NE = 32
CAP = 1280
NRT = CAP // 128
XROWS = NE * CAP
ALPHA = 2.0 ** 0.25
EPS = 1e-5
NEG = -30000.0
BIG = 1.0e6
ENGS = ["pe", "act", "dve", "pool", "sp"]
SEM_ROT = 30000
DRAM_KEYS = {"H0", "QT", "KT", "VV", "XR", "YG", "HB", "MIXT", "H1F", "XE", "YE", "WIN", "XRhalo", "y_out"}


class Prog:
    def __init__(self, nc, stack):
        self.nc = nc
        self.stack = stack
        self.ops = {e: [] for e in ENGS}
        self.esem = {}
        self.ecnt = {e: 0 for e in ENGS}
        self.waited = {e: {} for e in ENGS}
        self.dsem = {}
        self.lastw = {}
        self.readers = {}
        self.nsem = 0
        self.nbar = 0
        self.bc = None
        self.free_dsems = []
        self.retired = {}

    def _newsem(self, name):
        self.nsem += 1
        return self.stack.enter_context(self.nc.semaphore(f"{name}_{self.nsem}"))

    def _resolve(self, tok):
        if tok[0] == "e":
            return tok[2], tok[3]
        if tok[1] in self.dsem:
            ds = self.dsem[tok[1]]
            return ds[0], ds[1]
        return self.retired[tok[1]]

    def _collect(self, e, reads, writes):
        toks = []
        for k in reads:
            if k in self.lastw:
                toks.append(self.lastw[k])
        for k in writes:
            if k in self.lastw:
                toks.append(self.lastw[k])
            toks.extend(self.readers.get(k, ()))
        waits = []
        for t in toks:
            sem, v = self._resolve(t)
            sid = id(sem)
            if self.waited[e].get(sid, 0) >= v:
                continue
            self.waited[e][sid] = v
            waits.append((sem, v))
        return waits

    def _update(self, tok, reads, writes):
        for k in reads:
            self.readers.setdefault(k, []).append(tok)
        for k in writes:
            self.lastw[k] = tok
            self.readers[k] = []

    def op(self, e, fn, reads=(), writes=()):
        waits = self._collect(e, reads, writes)
        if e not in self.esem or self.ecnt[e] >= SEM_ROT:
            self.esem[e] = self._newsem("e" + e)
            self.ecnt[e] = 0
        sem = self.esem[e]
        self.ecnt[e] += 1
        tok = ("e", e, sem, self.ecnt[e])
        self.ops[e].append((waits, fn, (sem, 1)))
        self._update(tok, reads, writes)

    def dma(self, q, fn, reads=(), writes=(), key=None, allow_ww=False):
        for k in writes:
            if (not allow_ww) and k in RING_KEYS and k in self.lastw and self.lastw[k][0] == "d" and not self.readers.get(k):
                raise RuntimeError(f"ring slot {k} overwritten before any recorded reader")
        waits = self._collect(q, reads, writes)
        if key not in self.dsem:
            self.dsem[key] = self.free_dsems.pop() if self.free_dsems else [self._newsem("d"), 0]
        ds = self.dsem[key]
        ds[1] += 16
        self.ops[q].append((waits, fn, (ds[0], 16)))
        self._update(("d", key), reads, writes)

    def barrier(self, marker_fn):
        self.nbar += 1
        n = self.nbar
        waits = []
        cands = [(s, v) for (s, v) in self.dsem.values()]
        for e in ENGS:
            if e != "sp" and e in self.esem:
                cands.append((self.esem[e], self.ecnt[e]))
        for s, v in cands:
            if self.waited["sp"].get(id(s), 0) < v:
                self.waited["sp"][id(s)] = v
                waits.append((s, v))
        self.ops["sp"].append((waits, None, None))
        for k in list(self.dsem.keys()):
            if k != "bar":
                ent = self.dsem.pop(k)
                self.retired[k] = (ent[0], ent[1])
                self.free_dsems.append(ent)
        self.dma("sp", marker_fn, writes=[f"bar{n}"], key="bar")
        for x in ("act", "dve", "pool", "pe"):
            self.ops[x].append((self._collect(x, [f"bar{n}"], []), None, None))

    def flush(self, final=False):
        nc = self.nc
        fin = []
        if final:
            for k, (s, v) in self.dsem.items():
                fin.append((s, v))
            for e in ENGS:
                if e in self.esem and e != "sp":
                    fin.append((self.esem[e], self.ecnt[e]))
        handles = {"pe": "tensor", "act": "scalar", "dve": "vector", "pool": "gpsimd", "sp": "sync"}
        with nc.Block() as block:
            for e in ENGS:
                ops = self.ops[e]
                last = final and (e == "sp")

                def body(eng, ops=ops, last=last, e=e):
                    if e == "pool" and self.bc is None:
                        r = eng.alloc_register("bcreg")
                        eng.reg_mov(r, XROWS - 1)
                        self.bc = eng.snap(r)
                    for waits, fn, inc in ops:
                        for s, v in waits:
                            eng.wait_ge(s, v)
                        if fn is not None:
                            ins = fn(eng)
                            ins.then_inc(inc[0], inc[1])
                    if last:
                        for s, v in fin:
                            eng.wait_ge(s, v)

                getattr(block, handles[e])(body)
        self.ops = {e: [] for e in ENGS}


RING_KEYS = set()


class Ring:
    def __init__(self, items):
        self.items = items
        self.i = 0
        for it in items:
            RING_KEYS.add(it[1])

    def next(self):
        it = self.items[self.i % len(self.items)]
        self.i += 1
        return it


def t5_bucket_np(rel):
    n = np.abs(rel)
    large = 8 + (np.log(np.maximum(n, 1).astype(np.float32) / 8) / np.log(128 / 8) * 8).astype(np.int32)
    large = np.minimum(large, 15)
    return np.where(rel > 0, 16, 0) + np.where(n < 8, n, large)


def static_tables():
    k = np.arange(128)[:, None]
    q = np.arange(128)[None, :]
    bk = np.zeros((4, 128, 128), np.float32)
    mk = np.zeros((4, 128, 128), np.float32)
    for vi, j in enumerate((-1, 0, 1)):
        rel = 128 * j + k - q
        bk[vi] = t5_bucket_np(rel)
        mk[vi] = np.where(np.abs(rel) <= 128, 0.0, NEG)
    rel = k - (128 + q)
    bk[3] = t5_bucket_np(rel)
    mk[3] = 0.0
    return bk, mk


def build(stop_after=99, dbg=()):
    nc = bass.Bass("TRN2", target_bir_lowering=False)

    def din(name, shape, dt=F32):
        return nc.dram_tensor(name, list(shape), dt, kind="ExternalInput").ap()

    def dscr(name, shape, dt):
        kind = "ExternalOutput" if name in dbg else "Internal"
        return nc.dram_tensor(name, list(shape), dt, kind=kind).ap()

    xs = din("xs", [T, D])
    valid_tm = din("valid_tm", [128, NB])
    invbig_tm = din("invbig_tm", [128, NB])
    kbias_tm = din("kbias_tm", [128, NB])
    vmask_fm = din("vmask_fm", [128, T])
    bk_tab = din("bk_tab", [4, 128, 128])
    mk_tab = din("mk_tab", [4, 128, 128])
    ut_tab = din("ut_tab", [128, 128])
    ecoff_tab = din("ecoff_tab", [128, NE])
    elim_tab = din("elim_tab", [128, NE])
    ident_tab = din("ident_tab", [128, 128])
    ln_in_g = din("ln_in_g", [D]); ln_in_b = din("ln_in_b", [D])
    rel_bias = din("rel_bias", [32, 8])
    w_in = din("w_in", [D, 3584])
    lru_par = din("lru_par", [128, 8, 11])
    lru_wa = din("lru_wa", [2, 8, 128, 128])
    lru_wi = din("lru_wi", [2, 8, 128, 128])
    attn_sink = din("attn_sink", [8])
    w_out = din("w_out", [D, D])
    ln1_g = din("ln1_g", [D]); ln1_b = din("ln1_b", [D])
    router_w = din("router_w", [D, NE]); router_b = din("router_b", [NE])
    exp_w1 = din("exp_w1", [NE, D, 2 * D]); exp_b1r = din("exp_b1r", [NE, 128, 32])
    exp_w2 = din("exp_w2", [NE, D, D]); exp_b2 = din("exp_b2", [NE, D])
    ln2_g = din("ln2_g", [D]); ln2_b = din("ln2_b", [D])
    y_out = nc.dram_tensor("y_out", [64 * 128, D], F32, kind="ExternalOutput").ap()

    WIN = dscr("WIN", [28, 128, 2048], BF16)
    H0 = dscr("H0", [T, D], F32)
    QT = dscr("QT", [8, 128, T], BF16)
    KT = dscr("KT", [2, 128, T], BF16)
    VV = dscr("VV", [T, 256], BF16)
    XR = dscr("XR", [8, 128, T + 4], F32)
    YG = dscr("YG", [8, 128, T], F32)
    HB = dscr("HB", [8, 128, T], F32)
    MIXT = dscr("MIXT", [16, 128, T], BF16)
    H1F = dscr("H1F", [T, D], F32)
    XE = dscr("XE", [XROWS, D], BF16)
    YE = dscr("YE", [XROWS, D], BF16)
    DBGM = dscr("DBGM", [T, D], F32)
    DBGL = dscr("DBGL", [T, 64], F32)

    with ExitStack() as gst:
        P = Prog(nc, gst)

        def sb(st, name, shape, dt):
            return st.enter_context(nc.sbuf_tensor(name, list(shape), dt))

        def ps(st, name, shape, dt):
            return st.enter_context(nc.psum_tensor(name, list(shape), dt))

        barsb = sb(gst, "barsb", [128, 16], F32)
        ident_b = sb(gst, "ident_b", [128, 128], BF16)
        ident_f = sb(gst, "ident_f", [128, 128], F32)
        valid_sb = sb(gst, "valid_sb", [128, NB], F32)
        invbig_sb = sb(gst, "invbig_sb", [128, NB], F32)
        kbias_sb = sb(gst, "kbias_sb", [128, NB], F32)
        DEST = sb(gst, "DEST", [128, 64, 4], I32)
        GATE = sb(gst, "GATE", [128, 64, 4], F32)
        eps_sb = sb(gst, "eps_sb", [128, 1], F32)

        bar_marker = lambda e: e.dma_start(out=barsb[0:1, 0:16], in_=valid_tm[0:1, 0:16])
        P.op("pool", lambda e: e.memset(eps_sb[:], EPS), writes=["eps"])
        P.dma("sp", lambda e: e.dma_start(out=ident_f[:], in_=ident_tab), writes=["ident_f"], key="g0")
        P.dma("sp", lambda e: e.dma_start(out=valid_sb[:], in_=valid_tm), writes=["valid"], key="g0")
        P.dma("sp", lambda e: e.dma_start(out=invbig_sb[:], in_=invbig_tm), writes=["invbig"], key="g0")
        P.dma("sp", lambda e: e.dma_start(out=kbias_sb[:], in_=kbias_tm), writes=["kbias"], key="g0")
        P.op("dve", lambda e: e.tensor_copy(out=ident_b[:], in_=ident_f[:]), reads=["ident_f"], writes=["ident_b"])

        def layer_norm_tile(st_key, src, dst, dkey, Gt, Bt, gkey, valid_col, scr, tag):
            stats, mv, sd, rs, nmr = scr
            skeys = st_key if isinstance(st_key, list) else [st_key]
            for i in range(4):
                P.op("dve", lambda e, i=i: e.bn_stats(out=stats[:, i, :], in_=src[:, i * 512:(i + 1) * 512]),
                     reads=skeys, writes=[f"{tag}_stats{i}"])
            P.op("dve", lambda e: e.bn_aggr(out=mv[:], in_=stats[:].rearrange("p a b -> p (a b)")),
                 reads=[f"{tag}_stats{i}" for i in range(4)], writes=[f"{tag}_mv"])
            P.op("act", lambda e: e.activation(out=sd[:], in_=mv[:, 1:2], func=AF.Sqrt, bias=eps_sb[:, 0:1], scale=1.0),
                 reads=[f"{tag}_mv", "eps"], writes=[f"{tag}_sd"])
            P.op("dve", lambda e: e.reciprocal(out=rs[:], in_=sd[:]), reads=[f"{tag}_sd"], writes=[f"{tag}_rs"])
            if valid_col is not None:
                P.op("dve", lambda e: e.tensor_tensor(out=rs[:], in0=rs[:], in1=valid_col, op=ALU.mult),
                     reads=[f"{tag}_rs", "valid"], writes=[f"{tag}_rs"])
            P.op("dve", lambda e: e.scalar_tensor_tensor(out=nmr[:], in0=mv[:, 0:1], scalar=-1.0, in1=rs[:], op0=ALU.mult, op1=ALU.mult),
                 reads=[f"{tag}_mv", f"{tag}_rs"], writes=[f"{tag}_nmr"])
            P.op("act", lambda e: e.activation(out=dst, in_=src, func=AF.Identity, scale=rs[:, 0:1], bias=nmr[:, 0:1]),
                 reads=skeys + [f"{tag}_rs", f"{tag}_nmr"], writes=[dkey])
            P.op("dve", lambda e: e.tensor_tensor(out=dst, in0=dst, in1=Gt[:], op=ALU.mult),
                 reads=[dkey, gkey], writes=[dkey])
            if valid_col is not None:
                P.op("dve", lambda e: e.scalar_tensor_tensor(out=dst, in0=Bt[:], scalar=valid_col, in1=dst, op0=ALU.mult, op1=ALU.add),
                     reads=[dkey, gkey, "valid"], writes=[dkey])
            else:
                P.op("dve", lambda e: e.tensor_tensor(out=dst, in0=dst, in1=Bt[:], op=ALU.add),
                     reads=[dkey, gkey], writes=[dkey])

        with ExitStack() as st:
            wst = [sb(st, f"p0_wst{i}", [128, 16, 256], F32) for i in range(2)]
            wbf = [sb(st, f"p0_wbf{i}", [128, 16, 256], BF16) for i in range(2)]
            for j2 in range(14):
                i = j2 % 2
                P.dma("sp", lambda e, i=i, j2=j2: e.dma_start(out=wst[i][:], in_=w_in[:, j2 * 256:(j2 + 1) * 256].rearrange("(c p) n -> p c n", p=128)),
                      writes=[f"p0_wst{i}"], key=f"p0_l{i}")
                eng = "dve" if i == 0 else "act"
                if eng == "dve":
                    P.op("dve", lambda e, i=i: e.tensor_copy(out=wbf[i][:], in_=wst[i][:]), reads=[f"p0_wst{i}"], writes=[f"p0_wbf{i}"])
                else:
                    P.op("act", lambda e, i=i: e.copy(out=wbf[i][:], in_=wst[i][:]), reads=[f"p0_wst{i}"], writes=[f"p0_wbf{i}"])
                for h in range(2):
                    P.dma("sp", lambda e, i=i, j2=j2, h=h: e.dma_start(out=WIN[2 * j2 + h].rearrange("p (c n) -> p c n", c=16), in_=wbf[i][:, :, h * 128:(h + 1) * 128]),
                          reads=[f"p0_wbf{i}"], writes=["WIN"], key=f"p0_s{i}")
            zt = sb(st, "p0_zt", [128, 8, 2], F32)
            P.op("dve", lambda e: e.memset(zt[:], 0.0), writes=["p0_zt"])
            P.dma("sp", lambda e: e.dma_start(out=XR[:, :, 0:2].rearrange("c p n -> p c n"), in_=zt[:]), reads=["p0_zt"], writes=["XRhalo"], key="p0_z")
            P.dma("sp", lambda e: e.dma_start(out=XR[:, :, T + 2:T + 4].rearrange("c p n -> p c n"), in_=zt[:]), reads=["p0_zt"], writes=["XRhalo"], key="p0_z")
            P.barrier(bar_marker)
            P.flush()

        tiles = [(0, 128)] + [(128 + 512 * i, 512) for i in range(16)]
        with ExitStack() as st:
            Gt = sb(st, "p1_G", [128, D], F32)
            Bt = sb(st, "p1_B", [128, D], F32)
            P.dma("sp", lambda e: e.dma_start(out=Gt[:], in_=ln_in_g.partition_broadcast(128)), writes=["p1_GB"], key="p1_gb")
            P.dma("sp", lambda e: e.dma_start(out=Bt[:], in_=ln_in_b.partition_broadcast(128)), writes=["p1_GB"], key="p1_gb")
            xt_r = Ring([(sb(st, f"p1_xt{i}", [128, D], F32), f"p1_xt{i}") for i in range(4)])
            h0_r = Ring([(sb(st, f"p1_h0{i}", [128, D], F32), f"p1_h0{i}") for i in range(2)])
            hb_r = Ring([(sb(st, f"p1_hb{i}", [128, D], BF16), f"p1_hb{i}") for i in range(2)])
            hT_r = Ring([(sb(st, f"p1_hT{i}", [128, 16, 512], BF16), f"p1_hT{i}") for i in range(2)])
            wc_r = Ring([(sb(st, f"p1_wc{i}", [128, 16, 128], BF16), f"p1_wc{i}") for i in range(6)])
            sf_r = Ring([(sb(st, f"p1_sf{i}", [128, 512], F32), f"p1_sf{i}") for i in range(3)])
            sh_r = Ring([(sb(st, f"p1_sh{i}", [128, 512], BF16), f"p1_sh{i}") for i in range(3)])
            sv_r = Ring([(sb(st, f"p1_sv{i}", [128, 256], BF16), f"p1_sv{i}") for i in range(2)])
            lnscr = (sb(st, "p1_stats", [128, 4, 6], F32), sb(st, "p1_mv", [128, 2], F32), sb(st, "p1_sd", [128, 1], F32),
                     sb(st, "p1_rs", [128, 1], F32), sb(st, "p1_nmr", [128, 1], F32))
            tp_r = Ring([(ps(st, f"p1_tp{i}", [128, 1024], BF16), f"p1_tp{i}") for i in range(2)])
            pj_r = Ring([(ps(st, f"p1_pj{i}", [128, 512], F32), f"p1_pj{i}") for i in range(4)])
            QSCALE = 128.0 ** -0.5

            tstate = {}

            def t_xload(ti):
                if ti >= len(tiles):
                    return
                s0, w = tiles[ti]
                hT, hTk = hT_r.next()
                xs_l = []
                for bi in range(w // 128):
                    b = s0 // 128 + bi
                    xt, xk = xt_r.next()
                    P.dma("sp", lambda e, xt=xt, b=b: e.dma_start(out=xt[:], in_=xs[b * 128:(b + 1) * 128, :]), writes=[xk], key=xk)
                    xs_l.append((xt, xk))
                tstate[ti] = {"hT": hT, "hTk": hTk, "x": xs_l, "keys": []}

            def t_ln(ti, bi):
                if ti >= len(tiles):
                    return
                s0, w = tiles[ti]
                if bi >= w // 128:
                    return
                stt = tstate[ti]
                hT, hTk = stt["hT"], stt["hTk"]
                xt, xk = stt["x"][bi]
                b = s0 // 128 + bi
                h0t, h0k = h0_r.next()
                hbt, hbk = hb_r.next()
                layer_norm_tile(xk, xt[:], h0t[:], h0k, Gt, Bt, "p1_GB", valid_sb[:, b:b + 1], lnscr, "p1ln")
                P.op("pool", lambda e, hbt=hbt, h0t=h0t: e.tensor_copy(out=hbt[:], in_=h0t[:]), reads=[h0k], writes=[hbk])
                P.dma("sp", lambda e, h0t=h0t, b=b: e.dma_start(out=H0[b * 128:(b + 1) * 128, :], in_=h0t[:]), reads=[h0k], writes=["H0"], key=h0k + "s")
                stt["hb"] = stt.get("hb", {})
                stt["hb"][bi] = (hbt, hbk)

            def t_lnb(ti, bi):
                if ti >= len(tiles):
                    return
                s0, w = tiles[ti]
                if bi >= w // 128:
                    return
                stt = tstate[ti]
                hT, hTk = stt["hT"], stt["hTk"]
                hbt, hbk = stt["hb"].pop(bi)
                for half in range(2):
                    tp, tpk = tp_r.next()

                    def tr(e, tp=tp, hbt=hbt, half=half):
                        ins = None
                        for c8 in range(8):
                            c = half * 8 + c8
                            ins = e.transpose(out=tp[:, c8 * 128:(c8 + 1) * 128], in_=hbt[:, c * 128:(c + 1) * 128], identity=ident_b[:])
                        return ins
                    P.op("pe", tr, reads=[hbk, "ident_b"], writes=[tpk])
                    if half == 0:
                        P.op("act", lambda e, tp=tp, hT=hT, bi=bi: e.copy(out=hT[:, 0:8, bi * 128:(bi + 1) * 128], in_=tp[:].rearrange("p (c n) -> p c n", c=8)),
                             reads=[tpk], writes=[hTk + f"_{bi}a"])
                    else:
                        P.op("dve", lambda e, tp=tp, hT=hT, bi=bi: e.tensor_copy(out=hT[:, 8:16, bi * 128:(bi + 1) * 128], in_=tp[:].rearrange("p (c n) -> p c n", c=8)),
                             reads=[tpk], writes=[hTk + f"_{bi}b"])
                    stt["keys"].append(hTk + f"_{bi}{'ab'[half]}")

            items = []
            for ti, (s0, w) in enumerate(tiles):
                for bi in range(w // 128):
                    items.append((ti, "v", bi))
                chunks = (list(range(8, 10)) + list(range(12, 20))) if s0 == 0 else (list(range(0, 10)) + list(range(12, 28)))
                for j in chunks:
                    items.append((ti, "f", j))
            istate = {}

            def i_load(i):
                if i >= len(items):
                    return
                ti, kind, p_ = items[i]
                ids = [10, 11] if kind == "v" else [p_]
                lst = []
                for cid in ids:
                    wc, wck = wc_r.next()
                    P.dma("sp", lambda e, wc=wc, cid=cid: e.dma_start(out=wc[:], in_=WIN[cid].rearrange("p (c n) -> p c n", c=16)), writes=[wck], key=wck)
                    lst.append((wc, wck))
                istate[i] = lst

            t_xload(0)
            t_ln(0, 0)
            t_lnb(0, 0)
            t_xload(1)
            i_load(0)
            i_load(1)
            per_tile_count = {}
            for (ti, kind, p_) in items:
                per_tile_count[ti] = per_tile_count.get(ti, 0) + 1
            seen_in_tile = {}
            for i, (ti, kind, p_) in enumerate(items):
                i_load(i + 2)
                s0, w = tiles[ti]
                stt = tstate[ti]
                hT, hTk = stt["hT"], stt["hTk"]
                nbk = w // 128
                hT_keys = [hTk + f"_{bi}{x}" for bi in range(nbk) for x in "ab"]
                k_in = seen_in_tile.get(ti, 0)
                seen_in_tile[ti] = k_in + 1
                n_it = per_tile_count[ti]
                for bi_n in range(4):
                    if k_in == (bi_n * n_it) // 4:
                        t_ln(ti + 1, bi_n)
                    if k_in == min(n_it - 1, (bi_n * n_it) // 4 + 5) and not (bi_n < 3 and ((bi_n + 1) * n_it) // 4 <= (bi_n * n_it) // 4 + 5 - 100):
                        t_lnb(ti + 1, bi_n)
                if k_in == n_it - 1:
                    t_xload(ti + 2)
                wcs = istate.pop(i)
                if kind == "v":
                    bi = p_
                    b = s0 // 128 + bi
                    pj, pjk = pj_r.next()

                    def vmm(e, pj=pj, hT=hT, bi=bi, wcs=wcs):
                        ins = None
                        for h in range(2):
                            for c in range(16):
                                ins = e.matmul(pj[:, h * 128:(h + 1) * 128], lhsT=hT[:, c, bi * 128:(bi + 1) * 128], rhs=wcs[h][0][:, c, :], start=(c == 0), stop=(c == 15))
                        return ins
                    P.op("pe", vmm, reads=[hTk + f"_{bi}a", hTk + f"_{bi}b", wcs[0][1], wcs[1][1]], writes=[pjk])
                    sv, svk = sv_r.next()
                    P.op("act", lambda e, sv=sv, pj=pj: e.copy(out=sv[:], in_=pj[:, 0:256]), reads=[pjk], writes=[svk])
                    P.dma("sp", lambda e, sv=sv, b=b: e.dma_start(out=VV[b * 128:(b + 1) * 128, :], in_=sv[:]), reads=[svk], writes=["VV"], key=svk + "s")
                else:
                    j = p_
                    wc, wck = wcs[0]
                    pj, pjk = pj_r.next()

                    def pmm(e, pj=pj, hT=hT, wc=wc, w=w):
                        ins = None
                        for c in range(16):
                            ins = e.matmul(pj[:, 0:w], lhsT=wc[:, c, :], rhs=hT[:, c, 0:w], start=(c == 0), stop=(c == 15))
                        return ins
                    P.op("pe", pmm, reads=hT_keys + [wck], writes=[pjk])
                    if j < 8:
                        sh, shk = sh_r.next()
                        P.op("act", lambda e, sh=sh, pj=pj, w=w: e.activation(out=sh[:, 0:w], in_=pj[:, 0:w], func=AF.Copy, scale=QSCALE), reads=[pjk], writes=[shk])
                        P.dma("sp", lambda e, sh=sh, j=j, s0=s0, w=w: e.dma_start(out=QT[j, :, s0:s0 + w], in_=sh[:, 0:w]), reads=[shk], writes=["QT"], key=shk + "s")
                    elif j < 10:
                        sh, shk = sh_r.next()
                        P.op("dve", lambda e, sh=sh, pj=pj, w=w: e.tensor_copy(out=sh[:, 0:w], in_=pj[:, 0:w]), reads=[pjk], writes=[shk])
                        P.dma("sp", lambda e, sh=sh, j=j, s0=s0, w=w: e.dma_start(out=KT[j - 8, :, s0:s0 + w], in_=sh[:, 0:w]), reads=[shk], writes=["KT"], key=shk + "s")
                    elif j < 20:
                        sf, sfk = sf_r.next()
                        P.op("dve", lambda e, sf=sf, pj=pj, w=w: e.tensor_copy(out=sf[:, 0:w], in_=pj[:, 0:w]), reads=[pjk], writes=[sfk])
                        P.dma("sp", lambda e, sf=sf, j=j, s0=s0, w=w: e.dma_start(out=XR[j - 12, :, 2 + s0:2 + s0 + w], in_=sf[:, 0:w]), reads=[sfk], writes=["XR"], key=sfk + "s")
                    else:
                        sf, sfk = sf_r.next()
                        P.op("act", lambda e, sf=sf, pj=pj, w=w: e.activation(out=sf[:, 0:w], in_=pj[:, 0:w], func=AF.Gelu), reads=[pjk], writes=[sfk])
                        P.dma("sp", lambda e, sf=sf, j=j, s0=s0, w=w: e.dma_start(out=YG[j - 20, :, s0:s0 + w], in_=sf[:, 0:w]), reads=[sfk], writes=["YG"], key=sfk + "s")
            P.barrier(bar_marker)
            P.flush()
        if stop_after <= 1:
            P.flush(final=True)
            return nc

        for z in (1, 0):
            with ExitStack() as st:
                par = sb(st, f"l{z}_par", [128, 8, 11], F32)
                one_c = sb(st, f"l{z}_one", [128, 1], F32)
                nksp = sb(st, f"l{z}_nksp", [128, 8], F32)
                tmp = [sb(st, f"l{z}_tmp{i}", [128, 8], F32) for i in range(8)]
                wst = sb(st, f"l{z}_wst", [128, 2, 8, 128], F32)
                WA = sb(st, f"l{z}_WA", [128, 8, 128], BF16)
                WI = sb(st, f"l{z}_WI", [128, 8, 128], BF16)
                carry = sb(st, f"l{z}_carry", [128, 8], F32)
                P.dma("sp", lambda e: e.dma_start(out=par[:], in_=lru_par), writes=["l_par"], key="l_par")
                P.op("pool", lambda e: e.memset(one_c[:], 1.0), writes=["l_one"])
                P.op("pool", lambda e: e.memset(carry[:], 0.0), writes=[f"carry{c}" for c in range(8)])
                for (src, dst, nm) in ((lru_wa, WA, "wa"), (lru_wi, WI, "wi")):
                    P.dma("sp", lambda e, src=src: e.dma_start(out=wst[:], in_=src.rearrange("z n c j -> c z n j")), writes=["l_wst"], key="l_wst")
                    P.op("dve", lambda e, dst=dst: e.tensor_copy(out=dst[:], in_=wst[:, z, :, :]), reads=["l_wst"], writes=["l_" + nm])
                lam = par[:, :, 9 + z]
                t_abs, t_e, t_u, t_ln, t_ser, t_msk, t_mx, t_q = [t[:] for t in tmp]
                P.op("dve", lambda e: e.tensor_scalar(out=t_abs, in0=lam, scalar1=-1.0, scalar2=None, op0=ALU.mult), reads=["l_par"], writes=["lt_abs"])
                P.op("dve", lambda e: e.tensor_tensor(out=t_abs, in0=t_abs, in1=lam, op=ALU.max), reads=["l_par", "lt_abs"], writes=["lt_abs"])
                P.op("act", lambda e: e.activation(out=t_e, in_=t_abs, func=AF.Exp, scale=-1.0), reads=["lt_abs"], writes=["lt_e"])
                P.op("dve", lambda e: e.tensor_scalar(out=t_u, in0=t_e, scalar1=1.0, scalar2=None, op0=ALU.add), reads=["lt_e"], writes=["lt_u"])
                P.op("act", lambda e: e.activation(out=t_ln, in_=t_u, func=AF.Ln), reads=["lt_u"], writes=["lt_ln"])
                P.op("dve", lambda e: e.tensor_scalar(out=t_ser, in0=t_e, scalar1=-0.2, scalar2=0.25, op0=ALU.mult, op1=ALU.add), reads=["lt_e"], writes=["lt_ser"])
                for cst in (1.0 / 3.0, 0.5, 1.0):
                    P.op("dve", lambda e: e.tensor_tensor(out=t_ser, in0=t_ser, in1=t_e, op=ALU.mult), reads=["lt_ser", "lt_e"], writes=["lt_ser"])
                    P.op("dve", lambda e, cst=cst: e.tensor_scalar(out=t_ser, in0=t_ser, scalar1=-1.0, scalar2=cst, op0=ALU.mult, op1=ALU.add), reads=["lt_ser"], writes=["lt_ser"])
                P.op("dve", lambda e: e.tensor_tensor(out=t_ser, in0=t_ser, in1=t_e, op=ALU.mult), reads=["lt_ser", "lt_e"], writes=["lt_ser"])
                P.op("dve", lambda e: e.tensor_scalar(out=t_msk, in0=t_e, scalar1=0.1, scalar2=None, op0=ALU.is_lt), reads=["lt_e"], writes=["lt_msk"])
                P.op("dve", lambda e: e.tensor_tensor(out=t_q, in0=t_ser, in1=t_ln, op=ALU.subtract), reads=["lt_ser", "lt_ln"], writes=["lt_q"])
                P.op("dve", lambda e: e.tensor_tensor(out=t_q, in0=t_q, in1=t_msk, op=ALU.mult), reads=["lt_q", "lt_msk"], writes=["lt_q"])
                P.op("dve", lambda e: e.tensor_tensor(out=t_q, in0=t_q, in1=t_ln, op=ALU.add), reads=["lt_q", "lt_ln"], writes=["lt_q"])
                P.op("dve", lambda e: e.tensor_scalar(out=t_mx, in0=lam, scalar1=-1.0, scalar2=0.0, op0=ALU.mult, op1=ALU.max), reads=["l_par"], writes=["lt_mx"])
                P.op("dve", lambda e: e.tensor_tensor(out=t_q, in0=t_q, in1=t_mx, op=ALU.add), reads=["lt_q", "lt_mx"], writes=["lt_q"])
                P.op("dve", lambda e: e.tensor_scalar(out=nksp[:], in0=t_q, scalar1=-8.0, scalar2=None, op0=ALU.mult), reads=["lt_q"], writes=["l_nksp"])

                def ring(name, shape, dt, n):
                    return Ring([(sb(st, f"l{z}_{name}{i}", shape, dt), f"l_{name}{i}") for i in range(n)])
                xr_r = ring("xr", [128, 515], F32, 4)
                vm_r = ring("vm", [128, 512], F32, 3)
                xc_r = ring("xc", [128, 512], F32, 3)
                xcm_r = ring("xcm", [128, 512], F32, 4)
                xcb_r = ring("xcb", [128, 512], BF16, 3)
                r_r = ring("r", [128, 512], F32, 3)
                i_r = ring("i", [128, 512], F32, 4)
                a_r = ring("a", [128, 512], F32, 4)
                s_r = ring("s", [128, 512], F32, 4)
                u_r = ring("u", [128, 512], F32, 3)
                h_r = ring("h", [128, 512], F32, 3)
                hb_r2 = ring("hbt", [128, 512], F32, 4)
                yg_r = ring("ygt", [128, 512], F32, 4)
                rec_r = ring("rec", [128, 512], BF16, 3)
                pg_r = Ring([(ps(st, f"l{z}_pg{i}", [128, 512], F32), f"l_pg{i}") for i in range(6)])
                order = tiles if z == 0 else list(reversed(tiles[1:]))
                litems = [(s0, w, c) for (s0, w) in order for c in range(8)]
                lst_ = {}
                vmst = {}

                def l_load(i):
                    if i >= len(litems):
                        return
                    s0, w, c = litems[i]
                    if c == 0:
                        vm, vmk = vm_r.next()
                        P.dma("sp", lambda e, vm=vm, s0=s0, w=w: e.dma_start(out=vm[:, 0:w], in_=vmask_fm[:, s0:s0 + w]), writes=[vmk], key=vmk)
                        vmst[s0] = (vm, vmk)
                    xr_t, xrk = xr_r.next()
                    P.dma("sp", lambda e, xr_t=xr_t, c=c, s0=s0, w=w: e.dma_start(out=xr_t[:, 0:w + 3], in_=XR[c, :, s0:s0 + w + 3]), writes=[xrk], key=xrk)
                    d_ = {"xr": (xr_t, xrk)}
                    if z == 0 and s0 > 0:
                        hbt, hbk2 = hb_r2.next()
                        ygt, ygk = yg_r.next()
                        P.dma("sp", lambda e, hbt=hbt, c=c, s0=s0, w=w: e.dma_start(out=hbt[:, 0:w], in_=HB[c, :, s0:s0 + w]), writes=[hbk2], key=hbk2)
                        P.dma("sp", lambda e, ygt=ygt, c=c, s0=s0, w=w: e.dma_start(out=ygt[:, 0:w], in_=YG[c, :, s0:s0 + w]), writes=[ygk], key=ygk)
                        d_["hb"] = (hbt, hbk2)
                        d_["yg"] = (ygt, ygk)
                    lst_[i] = d_

                def l_stageA(i):
                    if i >= len(litems):
                        return
                    s0, w, c = litems[i]
                    d_ = lst_[i]
                    xr_t, xrk = d_["xr"]
                    vm, vmk = vmst[s0]
                    xc, xck = xc_r.next()
                    P.op("pool", lambda e, xc=xc, xr_t=xr_t, c=c, w=w: e.tensor_scalar(out=xc[:, 0:w], in0=xr_t[:, 0:w], scalar1=par[:, c, 0:1], scalar2=par[:, c, 4:5], op0=ALU.mult, op1=ALU.add),
                         reads=[xrk, "l_par"], writes=[xck])
                    for j in (1, 2, 3):
                        P.op("dve", lambda e, xc=xc, xr_t=xr_t, c=c, w=w, j=j: e.scalar_tensor_tensor(out=xc[:, 0:w], in0=xr_t[:, j:j + w], scalar=par[:, c, j:j + 1], in1=xc[:, 0:w], op0=ALU.mult, op1=ALU.add),
                             reads=[xrk, xck, "l_par"], writes=[xck])
                    xcm, xcmk = xcm_r.next()
                    P.op("pool", lambda e, xcm=xcm, xc=xc, vm=vm, w=w: e.tensor_tensor(out=xcm[:, 0:w], in0=xc[:, 0:w], in1=vm[:, 0:w], op=ALU.mult), reads=[xck, vmk], writes=[xcmk])
                    xcb, xcbk = xcb_r.next()
                    P.op("act", lambda e, xcb=xcb, xc=xc, w=w: e.copy(out=xcb[:, 0:w], in_=xc[:, 0:w]), reads=[xck], writes=[xcbk])
                    pga, pgak = pg_r.next()
                    pgi, pgik = pg_r.next()
                    P.op("pe", lambda e, pga=pga, xcb=xcb, c=c, w=w: e.matmul(pga[:, 0:w], lhsT=WA[:, c, :], rhs=xcb[:, 0:w], start=True, stop=True), reads=[xcbk, "l_wa"], writes=[pgak])
                    P.op("pe", lambda e, pgi=pgi, xcb=xcb, c=c, w=w: e.matmul(pgi[:, 0:w], lhsT=WI[:, c, :], rhs=xcb[:, 0:w], start=True, stop=True), reads=[xcbk, "l_wi"], writes=[pgik])
                    rt_, rk = r_r.next()
                    it_, ik = i_r.next()
                    P.op("act", lambda e, rt_=rt_, pga=pga, c=c, w=w: e.activation(out=rt_[:, 0:w], in_=pga[:, 0:w], func=AF.Sigmoid, bias=par[:, c, 5 + z:6 + z], scale=1.0), reads=[pgak, "l_par"], writes=[rk])
                    P.op("act", lambda e, it_=it_, pgi=pgi, c=c, w=w: e.activation(out=it_[:, 0:w], in_=pgi[:, 0:w], func=AF.Sigmoid, bias=par[:, c, 7 + z:8 + z], scale=1.0), reads=[pgik, "l_par"], writes=[ik])
                    at_, ak = a_r.next()
                    P.op("act", lambda e, at_=at_, rt_=rt_, c=c, w=w: e.activation(out=at_[:, 0:w], in_=rt_[:, 0:w], func=AF.Exp, scale=nksp[:, c:c + 1]), reads=[rk, "l_nksp"], writes=[ak])
                    st_, sk = s_r.next()
                    P.op("pool", lambda e, st_=st_, at_=at_, w=w: e.tensor_tensor(out=st_[:, 0:w], in0=at_[:, 0:w], in1=at_[:, 0:w], op=ALU.mult), reads=[ak], writes=[sk])
                    P.op("act", lambda e, st_=st_, w=w: e.activation(out=st_[:, 0:w], in_=st_[:, 0:w], func=AF.Sqrt, scale=-1.0, bias=one_c[:, 0:1]), reads=[sk, "l_one"], writes=[sk])
                    d_.update({"xcm": (xcm, xcmk), "i": (it_, ik), "a": (at_, ak), "s": (st_, sk)})

                def l_stageB(i):
                    s0, w, c = litems[i]
                    d_ = lst_.pop(i)
                    xcm, xcmk = d_["xcm"]
                    it_, ik = d_["i"]
                    at_, ak = d_["a"]
                    st_, sk = d_["s"]
                    ut_, uk = u_r.next()
                    P.op("dve", lambda e, ut_=ut_, it_=it_, xcm=xcm, w=w: e.tensor_tensor(out=ut_[:, 0:w], in0=it_[:, 0:w], in1=xcm[:, 0:w], op=ALU.mult), reads=[ik, xcmk], writes=[uk])
                    P.op("dve", lambda e, ut_=ut_, st_=st_, w=w: e.tensor_tensor(out=ut_[:, 0:w], in0=ut_[:, 0:w], in1=st_[:, 0:w], op=ALU.mult), reads=[uk, sk], writes=[uk])
                    ht_, hk = h_r.next()
                    if z == 0:
                        P.op("dve", lambda e, ht_=ht_, at_=at_, ut_=ut_, c=c, w=w: e.tensor_tensor_scan(out=ht_[:, 0:w], data0=at_[:, 0:w], data1=ut_[:, 0:w], initial=carry[:, c:c + 1], op0=ALU.mult, op1=ALU.add),
                             reads=[ak, uk, f"carry{c}"], writes=[hk])
                        P.op("act", lambda e, ht_=ht_, c=c, w=w: e.copy(out=carry[:, c:c + 1], in_=ht_[:, w - 1:w]), reads=[hk], writes=[f"carry{c}"])
                    else:
                        P.op("dve", lambda e, ht_=ht_, at_=at_, ut_=ut_, c=c, w=w: e.tensor_tensor_scan(out=ht_[:, 0:w][:, ::-1], data0=at_[:, 0:w][:, ::-1], data1=ut_[:, 0:w][:, ::-1], initial=carry[:, c:c + 1], op0=ALU.mult, op1=ALU.add),
                             reads=[ak, uk, f"carry{c}"], writes=[hk])
                        P.op("act", lambda e, ht_=ht_, c=c: e.copy(out=carry[:, c:c + 1], in_=ht_[:, 0:1]), reads=[hk], writes=[f"carry{c}"])
                    if z == 1:
                        P.dma("sp", lambda e, ht_=ht_, c=c, s0=s0, w=w: e.dma_start(out=HB[c, :, s0:s0 + w], in_=ht_[:, 0:w]), reads=[hk], writes=["HB"], key=hk + "s")
                    elif s0 > 0:
                        hbt, hbk2 = d_["hb"]
                        ygt, ygk = d_["yg"]
                        P.op("pool", lambda e, hbt=hbt, ht_=ht_, w=w: e.tensor_tensor(out=hbt[:, 0:w], in0=hbt[:, 0:w], in1=ht_[:, 0:w], op=ALU.add), reads=[hbk2, hk], writes=[hbk2])
                        rc, rck = rec_r.next()
                        P.op("dve", lambda e, rc=rc, hbt=hbt, ygt=ygt, w=w: e.tensor_tensor(out=rc[:, 0:w], in0=hbt[:, 0:w], in1=ygt[:, 0:w], op=ALU.mult), reads=[hbk2, ygk], writes=[rck])
                        P.dma("sp", lambda e, rc=rc, c=c, s0=s0, w=w: e.dma_start(out=MIXT[8 + c, :, s0:s0 + w], in_=rc[:, 0:w]), reads=[rck], writes=["MIXT"], key=rck + "s")

                l_load(0)
                l_load(1)
                l_load(2)
                l_stageA(0)
                for i in range(len(litems)):
                    l_load(i + 3)
                    l_stageA(i + 1)
                    l_stageB(i)
                P.barrier(bar_marker)
                P.flush()
        if stop_after <= 2:
            P.flush(final=True)
            return nc

        with ExitStack() as st:
            rb_bc = sb(st, "a_rb", [128, 256], F32)
            sk_bc = sb(st, "a_sk", [128, 8], F32)
            bk_sb = sb(st, "a_bk", [128, 4, 128], F32)
            biasT = sb(st, "a_biasT", [128, 5, 8, 128], F32)
            oh = sb(st, "a_oh", [128, 128], F32)
            sinkt = sb(st, "a_sinkt", [128, 8, 128], F32)
            ones_b = sb(st, "a_ones", [128, 128], BF16)
            kmeta = sb(st, "a_kmeta", [128, 2, 128], BF16)
            vmeta = sb(st, "a_vmeta", [128, 256], BF16)
            P.dma("sp", lambda e: e.dma_start(out=rb_bc[:], in_=rel_bias.rearrange("a b -> (a b)").partition_broadcast(128)), writes=["a_rb"], key="a_c")
            P.dma("sp", lambda e: e.dma_start(out=sk_bc[:], in_=attn_sink.partition_broadcast(128)), writes=["a_sk"], key="a_c")
            P.dma("sp", lambda e: e.dma_start(out=bk_sb[:], in_=bk_tab.rearrange("v p n -> p v n")), writes=["a_bk"], key="a_c")
            for h in range(8):
                P.dma("sp", lambda e, h=h: e.dma_start(out=biasT[:, 0:4, h, :], in_=mk_tab.rearrange("v p n -> p v n")), writes=["a_biasT"], key="a_c")
            P.dma("sp", lambda e: e.dma_start(out=kmeta[:], in_=KT[:, :, 0:128].rearrange("g p n -> p g n")), writes=["a_kmeta"], key="a_c")
            P.dma("sp", lambda e: e.dma_start(out=vmeta[:], in_=VV[0:128, :]), writes=["a_vmeta"], key="a_c")
            P.op("pool", lambda e: e.memset(ones_b[:], 1.0), writes=["a_ones"])
            P.op("act", lambda e: e.activation(out=sk_bc[:], in_=sk_bc[:], func=AF.Exp), reads=["a_sk"], writes=["a_sk"])
            P.op("pool", lambda e: e.memset(sinkt[:], 0.0), writes=["a_sinkt"])
            P.op("pool", lambda e: e.memset(biasT[:, 4, :, :], 0.0), reads=["a_biasT"], writes=["a_biasT"])
            for h in range(8):
                P.op("dve", lambda e, h=h: e.tensor_scalar(out=sinkt[:, h, :], in0=sinkt[:, h, :], scalar1=sk_bc[:, h:h + 1], scalar2=None, op0=ALU.add), reads=["a_sinkt", "a_sk"], writes=["a_sinkt"])
                P.op("dve", lambda e, h=h: e.tensor_scalar(out=biasT[:, 4, h, :], in0=biasT[:, 4, h, :], scalar1=rb_bc[:, 15 * 8 + h:15 * 8 + h + 1], scalar2=None, op0=ALU.add), reads=["a_biasT", "a_rb"], writes=["a_biasT"])
            bk_np, mk_np = static_tables()
            for var in range(4):
                present = sorted(set(int(v) for v in np.unique(bk_np[var][mk_np[var] == 0.0])))
                for bkt in present:
                    P.op("dve", lambda e, var=var, bkt=bkt: e.tensor_scalar(out=oh[:], in0=bk_sb[:, var, :], scalar1=float(bkt), scalar2=None, op0=ALU.is_equal), reads=["a_bk", "a_oh"], writes=["a_oh"])
                    for h in range(8):
                        P.op("dve", lambda e, var=var, bkt=bkt, h=h: e.scalar_tensor_tensor(out=biasT[:, var, h, :], in0=oh[:], scalar=rb_bc[:, bkt * 8 + h:bkt * 8 + h + 1], in1=biasT[:, var, h, :], op0=ALU.mult, op1=ALU.add),
                             reads=["a_oh", "a_rb", "a_biasT"], writes=["a_biasT"])
            q_r = Ring([(sb(st, f"a_q{i}", [128, 8, 512], BF16), f"a_q{i}") for i in range(3)])
            kw_r = Ring([(sb(st, f"a_kw{i}", [128, 2, 768], BF16), f"a_kw{i}") for i in range(3)])
            vw_r = Ring([(sb(st, f"a_vw{i}", [128, 6, 256], BF16), f"a_vw{i}") for i in range(3)])
            ao_r = Ring([(sb(st, f"a_ao{i}", [128, 8, 512], BF16), f"a_ao{i}") for i in range(3)])
            ss_r = Ring([(sb(st, f"a_ss{i}", [128, 512], F32), f"a_ss{i}") for i in range(4)])
            pt_r = Ring([(sb(st, f"a_pt{i}", [128, 512], BF16), f"a_pt{i}") for i in range(12)])
            dn_r = Ring([(sb(st, f"a_dn{i}", [128, 512], F32), f"a_dn{i}") for i in range(2)])
            psS = Ring([(ps(st, f"a_pS{i}", [128, 512], F32), f"a_pS{i}") for i in range(4)])
            psO = Ring([(ps(st, f"a_pO{i}", [128, 512], F32), f"a_pO{i}") for i in range(2)])
            psD = Ring([(ps(st, f"a_pD{i}", [128, 512], F32), f"a_pD{i}") for i in range(2)])
            atiles = tiles[1:]
            aitems = [(ti, bi, g) for ti in range(len(atiles)) for bi in range(4) for g in range(2)]
            atst = {}
            aist = {}

            def a_tload(ti):
                if ti >= len(atiles):
                    return
                s0, w = atiles[ti]
                qt, qk = q_r.next()
                kw, kwk = kw_r.next()
                vw, vwk = vw_r.next()
                ao, aok = ao_r.next()
                hi = min(T, s0 + w + 128)
                ncol = hi - (s0 - 128)
                P.dma("sp", lambda e, qt=qt, s0=s0, w=w: e.dma_start(out=qt[:], in_=QT[:, :, s0:s0 + w].rearrange("c p n -> p c n")), writes=[qk], key=qk)
                P.dma("sp", lambda e, kw=kw, s0=s0, ncol=ncol: e.dma_start(out=kw[:, :, 0:ncol], in_=KT[:, :, s0 - 128:s0 - 128 + ncol].rearrange("g p n -> p g n")), writes=[kwk], key=kwk)
                P.dma("sp", lambda e, vw=vw, s0=s0, ncol=ncol: e.dma_start(out=vw[:, 0:ncol // 128, :], in_=VV[s0 - 128:s0 - 128 + ncol, :].rearrange("(b p) n -> p b n", p=128)), writes=[vwk], key=vwk)
                atst[ti] = (qt, qk, kw, kwk, vw, vwk, ao, aok)

            def a_stageS(k):
                if k >= len(aitems):
                    return
                ti, bi, g = aitems[k]
                s0, w = atiles[ti]
                qt, qk, kw, kwk, vw, vwk, ao, aok = atst[ti]
                n = s0 // 128 + bi
                kbs = [("meta", None, 3 if n == 1 else 4, 0)]
                for j in (-1, 0, 1):
                    kb = n + j
                    if 1 <= kb <= 64:
                        kbs.append(("band", bi + 1 + j, j + 1, kb))
                pts = []
                for (kind, wi_, var, kbcol) in kbs:
                    pS, pSk = psS.next()
                    if kind == "meta":
                        kap = kmeta[:, g, :]
                        kkeys = ["a_kmeta"]
                    else:
                        kap = kw[:, g, wi_ * 128:(wi_ + 1) * 128]
                        kkeys = [kwk]
                    P.op("pe", lambda e, pS=pS, kap=kap, qt=qt, g=g, bi=bi: e.matmul(pS[:], lhsT=kap, rhs=qt[:, 4 * g:4 * g + 4, bi * 128:(bi + 1) * 128], start=True, stop=True),
                         reads=kkeys + [qk], writes=[pSk])
                    ss, ssk = ss_r.next()
                    P.op("dve", lambda e, ss=ss, pS=pS, var=var, g=g: e.tensor_tensor(out=ss[:].rearrange("p (h n) -> p h n", h=4), in0=pS[:].rearrange("p (h n) -> p h n", h=4), in1=biasT[:, var, 4 * g:4 * g + 4, :], op=ALU.add),
                         reads=[pSk, "a_biasT"], writes=[ssk])
                    pt, ptk = pt_r.next()
                    P.op("act", lambda e, pt=pt, ss=ss, kbcol=kbcol: e.activation(out=pt[:], in_=ss[:], func=AF.Exp, bias=kbias_sb[:, kbcol:kbcol + 1], scale=1.0), reads=[ssk, "kbias"], writes=[ptk])
                    if kind == "meta":
                        vap = vmeta[:, g * 128:(g + 1) * 128]
                        vkeys = ["a_vmeta"]
                    else:
                        vap = vw[:, wi_, g * 128:(g + 1) * 128]
                        vkeys = [vwk]
                    pts.append((pt, ptk, vap, vkeys))
                aist[k] = pts

            def a_stageO(k):
                ti, bi, g = aitems[k]
                s0, w = atiles[ti]
                qt, qk, kw, kwk, vw, vwk, ao, aok = atst[ti]
                pts = aist.pop(k)
                pO, pOk = psO.next()
                pD, pDk = psD.next()

                def omm(e, pO=pO, pts=pts):
                    ins = None
                    for ii, (pt, ptk, vap, vkeys) in enumerate(pts):
                        ins = e.matmul(pO[:], lhsT=vap, rhs=pt[:], start=(ii == 0), stop=(ii == len(pts) - 1))
                    return ins

                def dmm(e, pD=pD, pts=pts):
                    ins = None
                    for ii, (pt, ptk, vap, vkeys) in enumerate(pts):
                        ins = e.matmul(pD[:], lhsT=ones_b[:], rhs=pt[:], start=(ii == 0), stop=(ii == len(pts) - 1))
                    return ins
                allk = [x[1] for x in pts] + [kk for x in pts for kk in x[3]]
                P.op("pe", omm, reads=allk, writes=[pOk])
                P.op("pe", dmm, reads=allk + ["a_ones"], writes=[pDk])
                dn, dnk = dn_r.next()
                P.op("dve", lambda e, dn=dn, pD=pD, g=g: e.tensor_tensor(out=dn[:].rearrange("p (h n) -> p h n", h=4), in0=pD[:].rearrange("p (h n) -> p h n", h=4), in1=sinkt[:, 4 * g:4 * g + 4, :], op=ALU.add),
                     reads=[pDk, "a_sinkt"], writes=[dnk])
                P.op("dve", lambda e, dn=dn: e.reciprocal(out=dn[:], in_=dn[:]), reads=[dnk], writes=[dnk])
                P.op("dve", lambda e, ao=ao, pO=pO, dn=dn, g=g, bi=bi: e.tensor_tensor(out=ao[:, 4 * g:4 * g + 4, bi * 128:(bi + 1) * 128], in0=pO[:].rearrange("p (h n) -> p h n", h=4), in1=dn[:].rearrange("p (h n) -> p h n", h=4), op=ALU.mult),
                     reads=[pOk, dnk], writes=[aok + f"_{bi}{g}"])
                if bi == 3 and g == 1:
                    P.dma("sp", lambda e, ao=ao, s0=s0, w=w: e.dma_start(out=MIXT[0:8, :, s0:s0 + w].rearrange("c p n -> p c n"), in_=ao[:]),
                          reads=[aok + f"_{b2_}{g2_}" for b2_ in range(4) for g2_ in range(2)], writes=["MIXT"], key=aok + "s")

            a_tload(0)
            a_tload(1)
            a_stageS(0)
            for k in range(len(aitems)):
                ti, bi, g = aitems[k]
                if bi == 0 and g == 0:
                    a_tload(ti + 2)
                a_stageS(k + 1)
                a_stageO(k)
            P.barrier(bar_marker)
            P.flush()
        if stop_after <= 3:
            P.flush(final=True)
            return nc

        with ExitStack() as st:
            wo = sb(st, "d_wo", [128, 16, 2048], BF16)
            wst_r = Ring([(sb(st, f"d_wst{i}", [128, 16, 128], F32), f"d_wst{i}") for i in range(1)])
            for j in range(16):
                wstt, wk = wst_r.next()
                P.dma("sp", lambda e, wstt=wstt, j=j: e.dma_start(out=wstt[:], in_=w_out[:, j * 128:(j + 1) * 128].rearrange("(c p) n -> p c n", p=128)), writes=[wk], key=wk)
                if j % 2 == 0:
                    P.op("dve", lambda e, wstt=wstt, j=j: e.tensor_copy(out=wo[:, :, j * 128:(j + 1) * 128], in_=wstt[:]), reads=[wk], writes=[f"d_wo{j}"])
                else:
                    P.op("act", lambda e, wstt=wstt, j=j: e.copy(out=wo[:, :, j * 128:(j + 1) * 128], in_=wstt[:]), reads=[wk], writes=[f"d_wo{j}"])
            wo_keys = [f"d_wo{j}" for j in range(16)]
            G1 = sb(st, "d_G1", [128, D], F32)
            B1 = sb(st, "d_B1", [128, D], F32)
            P.dma("sp", lambda e: e.dma_start(out=G1[:], in_=ln1_g.partition_broadcast(128)), writes=["d_GB"], key="d_c")
            P.dma("sp", lambda e: e.dma_start(out=B1[:], in_=ln1_b.partition_broadcast(128)), writes=["d_GB"], key="d_c")
            rw = sb(st, "d_rw", [128, 16, NE], F32)
            rbb = sb(st, "d_rbb", [128, NE], F32)
            UT = sb(st, "d_UT", [128, 128], F32)
            ones_f = sb(st, "d_ones", [128, 128], F32)
            base = sb(st, "d_base", [128, NE], F32)
            elim = sb(st, "d_elim", [128, NE], F32)
            P.dma("sp", lambda e: e.dma_start(out=rw[:], in_=router_w.rearrange("(c p) n -> p c n", p=128)), writes=["d_rw"], key="d_c")
            P.dma("sp", lambda e: e.dma_start(out=rbb[:], in_=router_b.partition_broadcast(128)), writes=["d_rbb"], key="d_c")
            P.dma("sp", lambda e: e.dma_start(out=UT[:], in_=ut_tab), writes=["d_UT"], key="d_c")
            P.dma("sp", lambda e: e.dma_start(out=base[:], in_=ecoff_tab), writes=["d_base"], key="d_c")
            P.dma("sp", lambda e: e.dma_start(out=elim[:], in_=elim_tab), writes=["d_elim"], key="d_c")
            P.op("pool", lambda e: e.memset(ones_f[:], 1.0), writes=["d_ones"])
            mx_r = Ring([(sb(st, f"d_mx{i}", [128, 16, 512], BF16), f"d_mx{i}") for i in range(2)])
            h0_r = Ring([(sb(st, f"d_h0{i}", [128, D], F32), f"d_h0{i}") for i in range(2)])
            r1_r = Ring([(sb(st, f"d_r1{i}", [128, D], F32), f"d_r1{i}") for i in range(2)])
            h1_r = Ring([(sb(st, f"d_h1{i}", [128, D], F32), f"d_h1{i}") for i in range(2)])
            h1b_r = Ring([(sb(st, f"d_h1b{i}", [128, D], BF16), f"d_h1b{i}") for i in range(2)])
            h1T_r = Ring([(sb(st, f"d_h1T{i}", [128, 16, 128], F32), f"d_h1T{i}") for i in range(1)])
            lnscr = (sb(st, "d_stats", [128, 4, 6], F32), sb(st, "d_mv", [128, 2], F32), sb(st, "d_sd", [128, 1], F32),
                     sb(st, "d_rs", [128, 1], F32), sb(st, "d_nmr", [128, 1], F32))
            lg = sb(st, "d_lg", [128, NE], F32)
            m8 = sb(st, "d_m8", [128, 8], F32)
            sel = sb(st, "d_sel", [128, NE], F32)
            dd = sb(st, "d_dd", [128, NE], F32)
            junk = sb(st, "d_junk", [128, NE], F32)
            nv0 = sb(st, "d_nv0", [128, 1], F32)
            ex4 = sb(st, "d_ex4", [128, 4], F32)
            den1 = sb(st, "d_den1", [128, 1], F32)
            dk = sb(st, "d_dk", [128, 4], F32)
            lk = sb(st, "d_lk", [128, 4], F32)
            ov = sb(st, "d_ov", [128, 4], F32)
            nov = sb(st, "d_nov", [128, 4], F32)
            pw_r = Ring([(ps(st, f"d_pw{i}", [128, 512], F32), f"d_pw{i}") for i in range(4)])
            pt_r = Ring([(ps(st, f"d_pt{i}", [128, 512], F32), f"d_pt{i}") for i in range(2)])
            pl = ps(st, "d_pl", [128, 512], F32)
            pc = ps(st, "d_pc", [128, 512], F32)
            dblocks = [(ti, bi) for ti in range(len(tiles) - 1) for bi in range(4)]
            dst_ = {}
            dmx = {}

            def d_mxload(ti):
                if ti >= len(tiles) - 1:
                    return
                s0, w = tiles[1 + ti]
                mx, mxk = mx_r.next()
                P.dma("sp", lambda e, mx=mx, s0=s0, w=w: e.dma_start(out=mx[:], in_=MIXT[:, :, s0:s0 + w].rearrange("c p n -> p c n")), writes=[mxk], key=mxk)
                dmx[ti] = (mx, mxk)

            def d_stageA(k):
                if k >= len(dblocks):
                    return
                ti, bi = dblocks[k]
                s0, w = tiles[1 + ti]
                mx, mxk = dmx[ti]
                n = s0 // 128 + bi
                h0t, h0k = h0_r.next()
                P.dma("sp", lambda e, h0t=h0t, n=n: e.dma_start(out=h0t[:], in_=H0[n * 128:(n + 1) * 128, :]), writes=[h0k], key=h0k)
                r1, r1k = r1_r.next()
                for ng in range(4):
                    pw, pwk = pw_r.next()

                    def wmm(e, pw=pw, mx=mx, bi=bi, ng=ng):
                        ins = None
                        for c in range(16):
                            ins = e.matmul(pw[:], lhsT=mx[:, c, bi * 128:(bi + 1) * 128], rhs=wo[:, c, ng * 512:(ng + 1) * 512], start=(c == 0), stop=(c == 15))
                        return ins
                    P.op("pe", wmm, reads=[mxk] + wo_keys, writes=[pwk])
                    P.op("dve", lambda e, r1=r1, h0t=h0t, pw=pw, ng=ng: e.scalar_tensor_tensor(out=r1[:, ng * 512:(ng + 1) * 512], in0=h0t[:, ng * 512:(ng + 1) * 512], scalar=ALPHA, in1=pw[:], op0=ALU.mult, op1=ALU.add),
                         reads=[h0k, pwk], writes=[r1k + f"_{ng}"])
                dst_[k] = (n, r1, r1k)

            def d_stageB(k):
                n, r1, r1k = dst_.pop(k)
                r1keys = [r1k + f"_{ng}" for ng in range(4)]
                h1, h1k = h1_r.next()
                layer_norm_tile(r1keys, r1[:], h1[:], h1k, G1, B1, "d_GB", None, lnscr, "dln")
                if "DBGM" in dbg:
                    P.dma("sp", lambda e, r1=r1, n=n: e.dma_start(out=DBGM[n * 128:(n + 1) * 128, :], in_=r1[:]), reads=r1keys, key="dbgm")
                P.dma("sp", lambda e, h1=h1, n=n: e.dma_start(out=H1F[n * 128:(n + 1) * 128, :], in_=h1[:]), reads=[h1k], writes=["H1F"], key=h1k + "s")
                h1b, h1bk = h1b_r.next()
                P.op("pool", lambda e, h1b=h1b, h1=h1: e.tensor_copy(out=h1b[:], in_=h1[:]), reads=[h1k], writes=[h1bk])
                h1T, h1Tk = h1T_r.next()
                for q4 in range(4):
                    ptp, ptk = pt_r.next()

                    def trf(e, ptp=ptp, h1=h1, q4=q4):
                        ins = None
                        for c4 in range(4):
                            c = q4 * 4 + c4
                            ins = e.transpose(out=ptp[:, c4 * 128:(c4 + 1) * 128], in_=h1[:, c * 128:(c + 1) * 128], identity=ident_f[:])
                        return ins
                    P.op("pe", trf, reads=[h1k, "ident_f"], writes=[ptk])
                    if q4 % 2 == 0:
                        P.op("act", lambda e, ptp=ptp, h1T=h1T, q4=q4: e.copy(out=h1T[:, q4 * 4:(q4 + 1) * 4, :], in_=ptp[:].rearrange("p (c n) -> p c n", c=4)), reads=[ptk], writes=[h1Tk + f"_{q4}"])
                    else:
                        P.op("dve", lambda e, ptp=ptp, h1T=h1T, q4=q4: e.tensor_copy(out=h1T[:, q4 * 4:(q4 + 1) * 4, :], in_=ptp[:].rearrange("p (c n) -> p c n", c=4)), reads=[ptk], writes=[h1Tk + f"_{q4}"])

                def lmm(e, h1T=h1T):
                    ins = None
                    for c in range(16):
                        ins = e.matmul(pl[:, 0:NE], lhsT=h1T[:, c, :], rhs=rw[:, c, :], start=(c == 0), stop=(c == 15))
                    return ins
                P.op("pe", lmm, reads=[h1Tk + f"_{q4}" for q4 in range(4)] + ["d_rw"], writes=["d_pl"])
                P.op("dve", lambda e: e.tensor_tensor(out=lg[:], in0=pl[:, 0:NE], in1=rbb[:], op=ALU.add), reads=["d_pl", "d_rbb"], writes=["d_lg"])
                P.op("dve", lambda e: e.max(out=m8[:], in_=lg[:]), reads=["d_lg"], writes=["d_m8"])
                P.op("dve", lambda e, n=n: e.tensor_scalar(out=sel[:], in0=lg[:], scalar1=m8[:, 3:4], scalar2=valid_sb[:, n:n + 1], op0=ALU.is_ge, op1=ALU.mult), reads=["d_lg", "d_m8", "valid"], writes=["d_sel"])
                P.op("pe", lambda e: e.matmul(pc[:, 0:NE], lhsT=UT[:], rhs=sel[:], start=True, stop=True), reads=["d_sel", "d_UT"], writes=["d_pc0"])
                P.op("pe", lambda e: e.matmul(pc[:, 64:64 + NE], lhsT=ones_f[:], rhs=sel[:], start=True, stop=True), reads=["d_sel", "d_ones"], writes=["d_pc1"])
                P.op("dve", lambda e: e.tensor_tensor(out=dd[:], in0=pc[:, 0:NE], in1=base[:], op=ALU.add), reads=["d_pc0", "d_base"], writes=["d_dd"])
                P.op("dve", lambda e: e.tensor_tensor(out=base[:], in0=pc[:, 64:64 + NE], in1=base[:], op=ALU.add), reads=["d_pc1", "d_base"], writes=["d_base"])
                P.op("dve", lambda e: e.tensor_scalar(out=nv0[:], in0=m8[:, 0:1], scalar1=-1.0, scalar2=None, op0=ALU.mult), reads=["d_m8"], writes=["d_nv0"])
                P.op("act", lambda e: e.activation(out=ex4[:], in_=m8[:, 0:4], func=AF.Exp, bias=nv0[:, 0:1], scale=1.0, accum_out=den1[:]), reads=["d_m8", "d_nv0"], writes=["d_ex4", "d_den1"])
                P.op("dve", lambda e: e.reciprocal(out=den1[:], in_=den1[:]), reads=["d_den1"], writes=["d_den1"])
                P.op("dve", lambda e: e.tensor_scalar(out=ex4[:], in0=ex4[:], scalar1=den1[:, 0:1], scalar2=None, op0=ALU.mult), reads=["d_ex4", "d_den1"], writes=["d_ex4"])
                for k in range(4):
                    P.op("dve", lambda e, k=k: e.scalar_tensor_tensor(out=junk[:], in0=lg[:], scalar=m8[:, k:k + 1], in1=dd[:], op0=ALU.is_equal, op1=ALU.mult, accum_out=dk[:, k:k + 1]),
                         reads=["d_lg", "d_m8", "d_dd", "d_junk"], writes=["d_junk", f"d_dk{k}"])
                    P.op("dve", lambda e, k=k: e.scalar_tensor_tensor(out=junk[:], in0=lg[:], scalar=m8[:, k:k + 1], in1=elim[:], op0=ALU.is_equal, op1=ALU.mult, accum_out=lk[:, k:k + 1]),
                         reads=["d_lg", "d_m8", "d_elim", "d_junk"], writes=["d_junk", f"d_lk{k}"])
                dkk = [f"d_dk{k}" for k in range(4)]
                lkk = [f"d_lk{k}" for k in range(4)]
                P.op("dve", lambda e: e.tensor_tensor(out=ov[:], in0=dk[:], in1=lk[:], op=ALU.is_ge), reads=dkk + lkk, writes=["d_ov"])
                P.op("dve", lambda e: e.tensor_scalar(out=nov[:], in0=ov[:], scalar1=-1.0, scalar2=1.0, op0=ALU.mult, op1=ALU.add), reads=["d_ov"], writes=["d_nov"])
                P.op("dve", lambda e, n=n: e.tensor_tensor(out=GATE[:, n - 1, :], in0=ex4[:], in1=nov[:], op=ALU.mult), reads=["d_ex4", "d_nov"], writes=[f"GATE{n}"])
                P.op("dve", lambda e: e.scalar_tensor_tensor(out=ov[:], in0=ov[:], scalar=BIG, in1=dk[:], op0=ALU.mult, op1=ALU.add), reads=["d_ov"] + dkk, writes=["d_ov"])
                P.op("dve", lambda e, n=n: e.tensor_scalar(out=ov[:], in0=ov[:], scalar1=invbig_sb[:, n:n + 1], scalar2=None, op0=ALU.add), reads=["d_ov", "invbig"], writes=["d_ov"])
                P.op("dve", lambda e, n=n: e.tensor_copy(out=DEST[:, n - 1, :], in_=ov[:]), reads=["d_ov"], writes=[f"DEST{n}"])
                if "DBGL" in dbg:
                    P.dma("sp", lambda e, n=n: e.dma_start(out=DBGL[n * 128:(n + 1) * 128, 0:NE], in_=lg[:]), reads=["d_lg"], key="dbgl")
                    P.dma("sp", lambda e, n=n: e.dma_start(out=DBGL[n * 128:(n + 1) * 128, 32:36], in_=ov[:]), reads=["d_ov"], key="dbgl")
                    P.dma("sp", lambda e, n=n: e.dma_start(out=DBGL[n * 128:(n + 1) * 128, 36:40], in_=GATE[:, n - 1, :]), reads=[f"GATE{n}"], key="dbgl")
                for k in range(4):
                    P.dma("pool", lambda e, h1b=h1b, n=n, k=k: e.indirect_dma_start(out=XE[:, :], out_offset=bass.IndirectOffsetOnAxis(ap=DEST[:, n - 1, k:k + 1], axis=0), in_=h1b[:, :], in_offset=None,
                                                                               bounds_check=P.bc, oob_is_err=False),
                          reads=[h1bk, f"DEST{n}"], writes=["XE"], key=h1bk + "x")

            d_mxload(0)
            d_stageA(0)
            for k in range(len(dblocks)):
                ti, bi = dblocks[k]
                if bi == 0:
                    d_mxload(ti + 1)
                d_stageA(k + 1)
                d_stageB(k)
            P.barrier(bar_marker)
            P.flush()
        if stop_after <= 4:
            P.flush(final=True)
            return nc

        with ExitStack() as st:
            XT = sb(st, "e_XT", [128, 16, CAP], BF16)
            AT = sb(st, "e_AT", [128, 16, CAP], BF16)
            ws_r = Ring([(sb(st, f"e_ws{i}", [128, 16, 256], F32), f"e_ws{i}") for i in range(3)])
            wb_r = Ring([(sb(st, f"e_wb{i}", [128, 16, 256], BF16), f"e_wb{i}") for i in range(3)])
            xr_r = Ring([(sb(st, f"e_xr{i}", [128, D], BF16), f"e_xr{i}") for i in range(2)])
            b1_r = Ring([(sb(st, f"e_b1{i}", [128, 32], F32), f"e_b1{i}") for i in range(2)])
            b2_r = Ring([(sb(st, f"e_b2{i}", [128, D], F32), f"e_b2{i}") for i in range(2)])
            glu_r = Ring([(sb(st, f"e_glu{i}", [128, 512], F32), f"e_glu{i}") for i in range(2)])
            sg_r = Ring([(sb(st, f"e_sg{i}", [128, 512], F32), f"e_sg{i}") for i in range(2)])
            l0_r = Ring([(sb(st, f"e_l0{i}", [128, 512], F32), f"e_l0{i}") for i in range(2)])
            yst_r = Ring([(sb(st, f"e_yst{i}", [128, 256], BF16), f"e_yst{i}") for i in range(4)])
            ptx_r = Ring([(ps(st, f"e_ptx{i}", [128, 1024], BF16), f"e_ptx{i}") for i in range(2)])
            pg_r = Ring([(ps(st, f"e_pg{i}", [128, 512], F32), f"e_pg{i}") for i in range(2)])
            pl_r = Ring([(ps(st, f"e_plin{i}", [128, 512], F32), f"e_plin{i}") for i in range(2)])
            py_r = Ring([(ps(st, f"e_py{i}", [128, 512], F32), f"e_py{i}") for i in range(2)])
            rgs = [(0, 512), (512, 512), (1024, CAP - 1024)] if CAP > 1024 else [(0, 512), (512, CAP - 512)]

            units = []
            for ex in range(NE):
                for j in range(16):
                    units.append((ex, "w1", j))
                for ng in range(8):
                    units.append((ex, "w2", ng))
            ustate = {}
            cast_i = [0]

            def u_load(u):
                if u >= len(units):
                    return
                ex, kind, ix = units[u]
                ws, wsk = ws_r.next()
                if kind == "w1":
                    P.dma("sp", lambda e, ws=ws, ex=ex, ix=ix: e.dma_start(out=ws[:, :, 0:128], in_=exp_w1[ex, :, ix * 128:(ix + 1) * 128].rearrange("(c p) n -> p c n", p=128)), writes=[wsk], key=wsk)
                    P.dma("sp", lambda e, ws=ws, ex=ex, ix=ix: e.dma_start(out=ws[:, :, 128:256], in_=exp_w1[ex, :, D + ix * 128:D + (ix + 1) * 128].rearrange("(c p) n -> p c n", p=128)), writes=[wsk], key=wsk, allow_ww=True)
                else:
                    P.dma("sp", lambda e, ws=ws, ex=ex, ix=ix: e.dma_start(out=ws[:], in_=exp_w2[ex, :, ix * 256:(ix + 1) * 256].rearrange("(c p) n -> p c n", p=128)), writes=[wsk], key=wsk)
                ustate[u] = {"ws": [(ws, wsk)]}

            def u_cast(u):
                if u >= len(units):
                    return
                lst = []
                for (ws, wsk) in ustate[u]["ws"]:
                    wb, wbk = wb_r.next()
                    which = ("dve", "act", "dve", "act", "pool")[cast_i[0] % 5]
                    cast_i[0] += 1
                    if which == "act":
                        P.op("act", lambda e, wb=wb, ws=ws: e.copy(out=wb[:], in_=ws[:]), reads=[wsk], writes=[wbk])
                    else:
                        P.op(which, lambda e, wb=wb, ws=ws: e.tensor_copy(out=wb[:], in_=ws[:]), reads=[wsk], writes=[wbk])
                    lst.append((wb, wbk))
                ustate[u]["wb"] = lst

            bstate = {}

            def b_load(ex):
                if ex >= NE:
                    return
                b1t, b1k = b1_r.next()
                b2t, b2k = b2_r.next()
                P.dma("sp", lambda e, b1t=b1t, ex=ex: e.dma_start(out=b1t[:], in_=exp_b1r[ex]), writes=[b1k], key=b1k)
                P.dma("sp", lambda e, b2t=b2t, ex=ex: e.dma_start(out=b2t[:], in_=exp_b2[ex].partition_broadcast(128)), writes=[b2k], key=b2k)
                bstate[ex] = (b1t, b1k, b2t, b2k)

            xstate = {}

            def x_load(ex, rt):
                if ex >= NE:
                    return
                xrow, xrk = xr_r.next()
                P.dma("sp", lambda e, xrow=xrow, ex=ex, rt=rt: e.dma_start(out=xrow[:], in_=XE[ex * CAP + rt * 128:ex * CAP + (rt + 1) * 128, :]), writes=[xrk], key=xrk)
                xstate[(ex, rt)] = (xrow, xrk)

            def x_trans(ex, rt):
                if ex >= NE:
                    return
                xrow, xrk = xstate.pop((ex, rt))
                for half in range(2):
                    tp, tpk = ptx_r.next()

                    def trx(e, tp=tp, xrow=xrow, half=half):
                        ins = None
                        for c8 in range(8):
                            c = half * 8 + c8
                            ins = e.transpose(out=tp[:, c8 * 128:(c8 + 1) * 128], in_=xrow[:, c * 128:(c + 1) * 128], identity=ident_b[:])
                        return ins
                    P.op("pe", trx, reads=[xrk, "ident_b"], writes=[tpk])
                    if half == 0:
                        P.op("act", lambda e, tp=tp, rt=rt: e.copy(out=XT[:, 0:8, rt * 128:(rt + 1) * 128], in_=tp[:].rearrange("p (c n) -> p c n", c=8)), reads=[tpk], writes=[f"e_XT{rt}a"])
                    else:
                        P.op("dve", lambda e, tp=tp, rt=rt: e.tensor_copy(out=XT[:, 8:16, rt * 128:(rt + 1) * 128], in_=tp[:].rearrange("p (c n) -> p c n", c=8)), reads=[tpk], writes=[f"e_XT{rt}b"])

            xplan = {0: ([0, 1], []), 1: ([2, 3], [0, 1]), 2: ([4, 5], [2, 3]), 3: ([6, 7], [4, 5]), 4: ([8, 9], [6, 7]), 5: ([], [8, 9])}
            if NRT != 10:
                xplan = {}
                rts = list(range(NRT))
                for i in range(0, NRT, 2):
                    xplan.setdefault(i // 2, ([], []))
                    xplan[i // 2] = (rts[i:i + 2], xplan[i // 2][1])
                    xplan.setdefault(i // 2 + 1, ([], []))
                    xplan[i // 2 + 1] = (xplan[i // 2 + 1][0], rts[i:i + 2])

            b_load(0)
            for i in range(0, NRT, 2):
                for rt in range(i, min(i + 2, NRT)):
                    x_load(0, rt)
                for rt in range(i, min(i + 2, NRT)):
                    x_trans(0, rt)
            u_load(0)
            u_load(1)
            u_cast(0)

            at_keys = [f"e_AT{j}_{r0}" for j in range(16) for (r0, _) in rgs]
            for u, (ex, kind, ix) in enumerate(units):
                u_load(u + 2)
                u_cast(u + 1)
                b1t, b1k, b2t, b2k = bstate[ex]
                wbs = ustate[u]["wb"]
                if kind == "w1":
                    (wbg, wbgk), = wbs
                    wbl, wblk = wbg, wbgk
                    for jh in range(1):
                        j = ix
                        for (r0, rw_) in rgs:
                            pg, pgk = pg_r.next()
                            pln, plk = pl_r.next()

                            def m1(e, pg=pg, wb=wbg, jh=jh, r0=r0, rw_=rw_):
                                ins = None
                                for c in range(16):
                                    ins = e.matmul(pg[:, 0:rw_], lhsT=wb[:, c, 0:128], rhs=XT[:, c, r0:r0 + rw_], start=(c == 0), stop=(c == 15))
                                return ins

                            def m1l(e, pg=pln, wb=wbl, jh=jh, r0=r0, rw_=rw_):
                                ins = None
                                for c in range(16):
                                    ins = e.matmul(pg[:, 0:rw_], lhsT=wb[:, c, 128:256], rhs=XT[:, c, r0:r0 + rw_], start=(c == 0), stop=(c == 15))
                                return ins
                            xk_need = [f"e_XT{rt}{x}" for rt in range(r0 // 128, (r0 + rw_) // 128) for x in "ab"]
                            P.op("pe", m1, reads=xk_need + [wbgk], writes=[pgk])
                            P.op("pe", m1l, reads=xk_need + [wblk], writes=[plk])
                            glu, gluk = glu_r.next()
                            sg, sgk = sg_r.next()
                            l0, l0k = l0_r.next()
                            P.op("dve", lambda e, glu=glu, pg=pg, b1t=b1t, j=j, rw_=rw_: e.tensor_scalar(out=glu[:, 0:rw_], in0=pg[:, 0:rw_], scalar1=b1t[:, j:j + 1], scalar2=7.0, op0=ALU.add, op1=ALU.min), reads=[pgk, b1k], writes=[gluk])
                            P.op("act", lambda e, sg=sg, glu=glu, rw_=rw_: e.activation(out=sg[:, 0:rw_], in_=glu[:, 0:rw_], func=AF.Sigmoid, scale=1.702), reads=[gluk], writes=[sgk])
                            P.op("act", lambda e, l0=l0, pln=pln, b1t=b1t, j=j, rw_=rw_: e.activation(out=l0[:, 0:rw_], in_=pln[:, 0:rw_], func=AF.Identity, bias=b1t[:, 16 + j:17 + j], scale=1.0), reads=[plk, b1k], writes=[l0k])
                            P.op("dve", lambda e, l0=l0, rw_=rw_: e.tensor_scalar(out=l0[:, 0:rw_], in0=l0[:, 0:rw_], scalar1=7.0, scalar2=-7.0, op0=ALU.min, op1=ALU.max), reads=[l0k], writes=[l0k])
                            P.op("pool", lambda e, sg=sg, glu=glu, rw_=rw_: e.tensor_tensor(out=sg[:, 0:rw_], in0=sg[:, 0:rw_], in1=glu[:, 0:rw_], op=ALU.mult), reads=[sgk, gluk], writes=[sgk])
                            P.op("dve", lambda e, l0=l0, sg=sg, j=j, r0=r0, rw_=rw_: e.scalar_tensor_tensor(out=AT[:, j, r0:r0 + rw_], in0=l0[:, 0:rw_], scalar=1.0, in1=sg[:, 0:rw_], op0=ALU.add, op1=ALU.mult),
                                 reads=[l0k, sgk], writes=[f"e_AT{j}_{r0}"])
                else:
                    ng = ix
                    (wb2, wb2k), = wbs
                    if ng == 0:
                        b_load(ex + 1)
                    xl, xtr = xplan.get(ng, ([], []))
                    for rt in xtr:
                        x_trans(ex + 1, rt)
                    for rt in xl:
                        x_load(ex + 1, rt)
                    for rt in range(NRT):
                        py, pyk = py_r.next()

                        def m2(e, py=py, wb2=wb2, rt=rt):
                            ins = None
                            for c in range(16):
                                ins = e.matmul(py[:, 0:256], lhsT=AT[:, c, rt * 128:(rt + 1) * 128], rhs=wb2[:, c, :], start=(c == 0), stop=(c == 15))
                            return ins
                        P.op("pe", m2, reads=at_keys + [wb2k], writes=[pyk])
                        yst, ystk = yst_r.next()
                        P.op("dve", lambda e, yst=yst, py=py, b2t=b2t, ng=ng: e.tensor_tensor(out=yst[:], in0=py[:, 0:256], in1=b2t[:, ng * 256:(ng + 1) * 256], op=ALU.add), reads=[pyk, b2k], writes=[ystk])
                        P.dma("sp", lambda e, yst=yst, ex=ex, rt=rt, ng=ng: e.dma_start(out=YE[ex * CAP + rt * 128:ex * CAP + (rt + 1) * 128, ng * 256:(ng + 1) * 256], in_=yst[:]), reads=[ystk], writes=["YE"], key=ystk + "s")
                del ustate[u]
            P.barrier(bar_marker)
            P.flush()
        if stop_after <= 5:
            P.flush(final=True)
            return nc

        with ExitStack() as st:
            G2 = sb(st, "f_G2", [128, D], F32)
            B2 = sb(st, "f_B2", [128, D], F32)
            P.dma("sp", lambda e: e.dma_start(out=G2[:], in_=ln2_g.partition_broadcast(128)), writes=["f_GB"], key="f_c")
            P.dma("sp", lambda e: e.dma_start(out=B2[:], in_=ln2_b.partition_broadcast(128)), writes=["f_GB"], key="f_c")
            yg_r = Ring([(sb(st, f"f_yg{i}", [128, D], BF16), f"f_yg{i}") for i in range(8)])
            h1_r = Ring([(sb(st, f"f_h1{i}", [128, D], F32), f"f_h1{i}") for i in range(3)])
            acc_r = Ring([(sb(st, f"f_acc{i}", [128, D], F32), f"f_acc{i}") for i in range(2)])
            out_r = Ring([(sb(st, f"f_out{i}", [128, D], F32), f"f_out{i}") for i in range(2)])
            lnscr = (sb(st, "f_stats", [128, 4, 6], F32), sb(st, "f_mv", [128, 2], F32), sb(st, "f_sd", [128, 1], F32),
                     sb(st, "f_rs", [128, 1], F32), sb(st, "f_nmr", [128, 1], F32))
            for i in range(8):
                P.op("pool", lambda e, i=i: e.memset(yg_r.items[i][0][:], 0.0), writes=[yg_r.items[i][1]])
            fst = {}

            def f_load(n):
                if n > 64:
                    return
                h1, h1k = h1_r.next()
                P.dma("sp", lambda e, h1=h1, n=n: e.dma_start(out=h1[:], in_=H1F[n * 128:(n + 1) * 128, :]), writes=[h1k], key=h1k)
                ygs = []
                for k in range(4):
                    yg, ygk = yg_r.next()
                    P.dma("pool", lambda e, yg=yg, n=n, k=k: e.indirect_dma_start(out=yg[:, :], out_offset=None, in_=YE[:, :], in_offset=bass.IndirectOffsetOnAxis(ap=DEST[:, n - 1, k:k + 1], axis=0),
                                                                               bounds_check=P.bc, oob_is_err=False),
                          reads=[f"DEST{n}"], writes=[ygk], key=ygk, allow_ww=True)
                    ygs.append((yg, ygk))
                fst[n] = (h1, h1k, ygs)

            def f_comp(n):
                h1, h1k, ygs = fst.pop(n)
                acc, acck = acc_r.next()
                for k in range(4):
                    yg, ygk = ygs[k]
                    if k == 0:
                        P.op("dve", lambda e, acc=acc, yg=yg, n=n: e.tensor_scalar(out=acc[:], in0=yg[:], scalar1=GATE[:, n - 1, 0:1], scalar2=None, op0=ALU.mult), reads=[ygk, f"GATE{n}"], writes=[acck])
                    else:
                        P.op("dve", lambda e, acc=acc, yg=yg, n=n, k=k: e.scalar_tensor_tensor(out=acc[:], in0=yg[:], scalar=GATE[:, n - 1, k:k + 1], in1=acc[:], op0=ALU.mult, op1=ALU.add), reads=[ygk, f"GATE{n}", acck], writes=[acck])
                P.op("dve", lambda e, acc=acc, h1=h1: e.scalar_tensor_tensor(out=acc[:], in0=h1[:], scalar=ALPHA, in1=acc[:], op0=ALU.mult, op1=ALU.add), reads=[h1k, acck], writes=[acck])
                ot, otk = out_r.next()
                layer_norm_tile(acck, acc[:], ot[:], otk, G2, B2, "f_GB", None, lnscr, "fln")
                P.dma("sp", lambda e, ot=ot, n=n: e.dma_start(out=y_out[(n - 1) * 128:n * 128, :], in_=ot[:]), reads=[otk], writes=["y_out"], key=otk + "s")

            f_load(1)
            for n in range(1, 65):
                f_load(n + 1)
                f_comp(n)
            P.barrier(bar_marker)
            P.flush()
        P.flush(final=True)
    return nc


def host_prep(x_seq, L, meta_tokens):
    xs = np.zeros((T, D), np.float32)
    xs[112:128] = meta_tokens
    xs[128:128 + L] = x_seq
    valid = np.zeros(T, np.float32)
    valid[112:128 + L] = 1.0
    kb = np.full(T, NEG, np.float32)
    kb[112:128 + L] = 0.0
    tm = lambda a: np.ascontiguousarray(a.reshape(NB, 128).T)
    return {
        "xs": xs,
        "valid_tm": tm(valid),
        "invbig_tm": tm((1.0 - valid) * BIG),
        "kbias_tm": tm(kb),
        "vmask_fm": np.ascontiguousarray(np.broadcast_to(valid[None, :], (128, T))),
    }


def common_inputs(inp):
    bk, mk = static_tables()
    ut = (np.arange(128)[:, None] <= np.arange(128)[None, :]).astype(np.float32)
    ecoff = np.broadcast_to((np.arange(NE) * CAP - 1).astype(np.float32)[None, :], (128, NE))
    elim = np.broadcast_to(((np.arange(NE) + 1) * CAP).astype(np.float32)[None, :], (128, NE))
    c = {
        "bk_tab": bk, "mk_tab": mk, "ut_tab": ut,
        "ecoff_tab": np.ascontiguousarray(ecoff), "elim_tab": np.ascontiguousarray(elim),
        "ident_tab": np.eye(128, dtype=np.float32),
    }
    f = lambda a: np.ascontiguousarray(np.asarray(a, np.float32))
    c["ln_in_g"] = f(inp["ln_in_g"]); c["ln_in_b"] = f(inp["ln_in_b"])
    c["rel_bias"] = f(inp["rel_bias"])
    c["w_in"] = f(inp["w_in"][0])
    rows = [inp["conv_w"][0][j] for j in range(4)] + [inp["conv_b"][0], inp["lru_ba"][0][0], inp["lru_ba"][0][1],
                                                    inp["lru_bi"][0][0], inp["lru_bi"][0][1], inp["lru_lam"][0][0], inp["lru_lam"][0][1]]
    par = np.stack([np.asarray(r, np.float32) for r in rows], axis=0)
    c["lru_par"] = np.ascontiguousarray(par.reshape(11, 8, 128).transpose(2, 1, 0))
    c["lru_wa"] = f(inp["lru_wa"][0])
    c["lru_wi"] = f(inp["lru_wi"][0])
    c["attn_sink"] = f(inp["attn_sink"][0])
    c["w_out"] = f(inp["w_out"][0])
    c["ln1_g"] = f(inp["ln1_g"][0]); c["ln1_b"] = f(inp["ln1_b"][0])
    c["router_w"] = f(inp["router_w"][0]); c["router_b"] = f(inp["router_b"][0])
    c["exp_w1"] = f(inp["exp_w1"][0]); c["exp_b1r"] = np.ascontiguousarray(f(inp["exp_b1"][0]).reshape(NE, 32, 128).transpose(0, 2, 1))
    c["exp_w2"] = f(inp["exp_w2"][0]); c["exp_b2"] = f(inp["exp_b2"][0])
    c["ln2_g"] = f(inp["ln2_g"][0]); c["ln2_b"] = f(inp["ln2_b"][0])
    return c


def kernel(**inp):
    xp = np.asarray(inp["x_prompt"], np.float32)
    xsm = np.asarray(inp["x_sample"], np.float32)
    meta = np.asarray(inp["meta_tokens"], np.float32)
    common = common_inputs(inp)
    seqs = [(xsm[i], 8192) for i in range(4)] + [(xp[i], 4096) for i in range(2)] + [(xp[0], 4096), (xp[1], 4096)]
    in_maps = []
    for (xq, L) in seqs:
        m = dict(common)
        m.update(host_prep(xq, L, meta))
        in_maps.append(m)
    nc = build()
    res = run_bass_kernel_spmd(nc, in_maps, core_ids=list(range(8)))
    ys = [r["y_out"] for r in res.results]
    y_sample = np.stack([ys[i][:8192] for i in range(4)], axis=0).astype(np.float32)
    y_prompt = np.stack([ys[4 + i][:4096] for i in range(2)], axis=0).astype(np.float32)
    return (y_prompt, y_sample)
```
